# Optimizing a Trainium2 kernel written in Bass

```python
import math
import jax, jax.numpy as jnp
from jax import lax
import numpy as np

D_MODEL = 2048
BATCH = 4
SEQ = 2048
DEPTH = 2

GRID_W = 64
CTX_LEN = 256
MIX_WIDTH = D_MODEL
RET_WIDTH = MIX_WIDTH // 2
S5_WIDTH = MIX_WIDTH - RET_WIDTH
RET_HEADS = 8
RET_HEAD_DIM = RET_WIDTH // RET_HEADS
RET_CHUNK = 128
S5_GROUP = 16
S5_GROUPS = S5_WIDTH // S5_GROUP
S5_STATE = 64
IN_WIDTH = 4 * RET_WIDTH + S5_WIDTH
N_EXPERT_GROUPS = 4
EXPERTS_PER_GROUP = 8
N_EXPERTS = N_EXPERT_GROUPS * EXPERTS_PER_GROUP
FINE_TOP_K = 2
EXPERT_FF = D_MODEL // 4
MOE_BLOCK = 128
ROPE_BASE = 10000.0
NORM_EPS = 1e-6
GN_EPS = 1e-5
N_MOD = 6

kernel_name = 'hymba_retnet_s5_hmoe_prefix_dit'


def rms_norm(x, g):
    xf = x.astype(jnp.float32)
    y = xf * lax.rsqrt(jnp.mean(xf * xf, axis=-1, keepdims=True) + NORM_EPS)
    return (y * g.astype(jnp.float32)).astype(x.dtype)


def adaln(cond, w, b):
    return jax.nn.silu(cond) @ w + b


def modulate(h, shift, scale):
    return h * (1.0 + scale) + shift


def rope_2d(n_tok):
    rows = n_tok // GRID_W
    row = jnp.repeat(jnp.arange(rows, dtype=jnp.float32), GRID_W)
    col = jnp.tile(jnp.arange(GRID_W, dtype=jnp.float32), rows)
    n_freq = RET_HEAD_DIM // 4
    inv = ROPE_BASE ** (-jnp.arange(n_freq, dtype=jnp.float32) / n_freq)
    ang = jnp.concatenate([row[:, None] * inv, col[:, None] * inv], axis=-1)
    return jnp.cos(ang), jnp.sin(ang)


def apply_rope(t, cos, sin):
    tp = t.reshape(*t.shape[:-1], -1, 2)
    t0, t1 = tp[..., 0], tp[..., 1]
    return jnp.stack([t0 * cos - t1 * sin, t0 * sin + t1 * cos], axis=-1).reshape(t.shape)


def split_heads(t):
    b, l, _ = t.shape
    return t.astype(jnp.float32).reshape(b, l, RET_HEADS, -1).transpose(0, 2, 1, 3)


def head_group_norm(y):
    mu = jnp.mean(y, axis=-1, keepdims=True)
    var = jnp.mean(jnp.square(y - mu), axis=-1, keepdims=True)
    y = (y - mu) * lax.rsqrt(var + GN_EPS)
    b, h, l, dv = y.shape
    return y.transpose(0, 2, 1, 3).reshape(b, l, h * dv)


def retention_direction(q, k, v, log_g, state0, inclusive, need_out):
    b, h, l, dk = q.shape
    dv = v.shape[-1]
    n = l // RET_CHUNK
    kc = k.reshape(b, h, n, RET_CHUNK, dk)
    vc = v.reshape(b, h, n, RET_CHUNK, dv)
    pos = jnp.arange(RET_CHUNK, dtype=jnp.float32)
    k_w = jnp.exp(log_g[:, None] * (RET_CHUNK - 1.0 - pos)[None])
    chunk_kv = jnp.einsum('bhncd,bhnce->nbhde', kc * k_w[None, :, None, :, None], vc)
    chunk_decay = jnp.exp(log_g * RET_CHUNK)[None, :, None, None]

    def step(s, kv):
        return chunk_decay * s + kv, s

    final, prev = lax.scan(step, state0, chunk_kv)
    if not need_out:
        return None, final
    qc = q.reshape(b, h, n, RET_CHUNK, dk)
    diff = pos[:, None] - pos[None, :]
    mask = (diff >= 0) if inclusive else (diff > 0)
    decay = jnp.where(mask[None], jnp.exp(log_g[:, None, None] * jnp.where(mask, diff, 0.0)[None]), 0.0)
    scores = jnp.einsum('bhnid,bhnjd->bhnij', qc, kc) * decay[None, :, None]
    inner = jnp.einsum('bhnij,bhnje->bhnie', scores, vc)
    q_w = jnp.exp(log_g[:, None] * (pos + 1.0)[None])
    cross = jnp.einsum('bhnid,nbhde->bhnie', qc * q_w[None, :, None, :, None], prev)
    return (inner + cross).reshape(b, h, l, dv), final


def retention_mixer(q_c, k_c, v_c, q_l, k_l, v_l, decay_param, need_ctx_out):
    log_g = -jnp.exp(decay_param.astype(jnp.float32))
    b, h, _, dk = q_c.shape
    dv = v_c.shape[-1]
    zero = jnp.zeros((b, h, dk, dv), jnp.float32)
    rev = lambda t: jnp.flip(t, axis=2)
    ctx_f, s_f = retention_direction(q_c, k_c, v_c, log_g[0], zero, True, need_ctx_out)
    lat_f, _ = retention_direction(q_l, k_l, v_l, log_g[0], s_f, True, True)
    ctx_b, s_b = retention_direction(rev(q_c), rev(k_c), rev(v_c), log_g[1], zero, False, need_ctx_out)
    lat_b, _ = retention_direction(rev(q_l), rev(k_l), rev(v_l), log_g[1], s_b, False, True)
    lat = head_group_norm(lat_f + rev(lat_b))
    ctx_o = head_group_norm(ctx_f + rev(ctx_b)) if need_ctx_out else None
    return ctx_o, lat


def s5_discretize(a_re, a_im, log_step, b_re, b_im):
    dt = jnp.exp(log_step)[:, None]
    mag = jnp.exp(a_re * dt)
    ang = a_im * dt
    abar_re = mag * jnp.cos(ang)
    abar_im = mag * jnp.sin(ang)
    den = a_re * a_re + a_im * a_im
    nr = abar_re - 1.0
    fr = (nr * a_re + abar_im * a_im) / den
    fi = (abar_im * a_re - nr * a_im) / den
    bbar_re = fr[..., None] * b_re - fi[..., None] * b_im
    bbar_im = fr[..., None] * b_im + fi[..., None] * b_re
    return abar_re, abar_im, bbar_re, bbar_im


def complex_linear_combine(left, right):
    a1r, a1i, b1r, b1i = left
    a2r, a2i, b2r, b2i = right
    return (a1r * a2r - a1i * a2i,
            a1r * a2i + a1i * a2r,
            a2r * b1r - a2i * b1i + b2r,
            a2r * b1i + a2i * b1r + b2i)


def s5_direction(u, disc, c_re, c_im, h0, reverse, need_out):
    abar_re, abar_im, bbar_re, bbar_im = disc
    l = u.shape[0]
    bu_re = jnp.einsum('lbgc,gpc->lbgp', u, bbar_re)
    bu_im = jnp.einsum('lbgc,gpc->lbgp', u, bbar_im)
    shape = (l, 1) + abar_re.shape
    a_re = jnp.broadcast_to(abar_re, shape)
    a_im = jnp.broadcast_to(abar_im, shape)
    acum_re, acum_im, h_re, h_im = lax.associative_scan(
        complex_linear_combine, (a_re, a_im, bu_re, bu_im), reverse=reverse, axis=0)
    if h0 is not None:
        h0_re, h0_im = h0
        h_re, h_im = (h_re + acum_re * h0_re - acum_im * h0_im,
                      h_im + acum_re * h0_im + acum_im * h0_re)
    end = 0 if reverse else l - 1
    final = (h_re[end], h_im[end])
    if not need_out:
        return None, final
    y = jnp.einsum('lbgp,gcp->lbgc', h_re, c_re) - jnp.einsum('lbgp,gcp->lbgc', h_im, c_im)
    return y, final


def s5_mixer(u_ctx, u_lat, a_re, a_im, log_step, b_re, b_im, c_re, c_im, d_skip, glu_w, glu_b, need_ctx_out):
    f32 = jnp.float32

    def to_scan(u):
        b, l, _ = u.shape
        return u.astype(f32).reshape(b, l, S5_GROUPS, S5_GROUP).transpose(1, 0, 2, 3)

    uc, ul = to_scan(u_ctx), to_scan(u_lat)
    d = d_skip.astype(f32)
    y_lat = d * ul
    y_ctx = d * uc if need_ctx_out else None
    for direction, reverse in ((0, False), (1, True)):
        disc = s5_discretize(a_re[direction].astype(f32), a_im[direction].astype(f32),
                             log_step[direction].astype(f32), b_re[direction].astype(f32),
                             b_im[direction].astype(f32))
        cr, ci = c_re[direction].astype(f32), c_im[direction].astype(f32)
        yc, h_end = s5_direction(uc, disc, cr, ci, None, reverse, need_ctx_out)
        yl, _ = s5_direction(ul, disc, cr, ci, h_end, reverse, True)
        y_lat = y_lat + yl
        if need_ctx_out:
            y_ctx = y_ctx + yc

    def glu_out(y):
        l, b = y.shape[0], y.shape[1]
        z = jax.nn.gelu(y.transpose(1, 0, 2, 3).reshape(b, l, S5_WIDTH))
        return z * jax.nn.sigmoid(z @ glu_w + glu_b)

    return (glu_out(y_ctx) if need_ctx_out else None), glu_out(y_lat)


def hierarchical_moe(h, grp_w, grp_b, exp_w, exp_b, w_gate, w_up, w_down):
    n, d = h.shape
    f32 = jnp.float32
    hf = h.astype(f32)
    grp_logits = hf @ grp_w.astype(f32) + grp_b.astype(f32)
    grp_prob = jax.nn.softmax(grp_logits, axis=-1)
    grp = jnp.argmax(grp_logits, axis=-1).astype(jnp.int32)
    grp_gate = jnp.take_along_axis(grp_prob, grp[:, None], axis=-1)
    exp_logits = (hf @ exp_w.astype(f32) + exp_b.astype(f32)).reshape(n, N_EXPERT_GROUPS, EXPERTS_PER_GROUP)
    in_group = jnp.take_along_axis(exp_logits, grp[:, None, None], axis=1)[:, 0]
    top_logit, top_idx = lax.top_k(in_group, FINE_TOP_K)
    gate = grp_gate * jax.nn.softmax(top_logit, axis=-1)
    expert = grp[:, None] * EXPERTS_PER_GROUP + top_idx

    n_slots = n * FINE_TOP_K
    slot_expert = expert.reshape(-1)
    slot_token = jnp.repeat(jnp.arange(n, dtype=jnp.int32), FINE_TOP_K)
    slot_gate = gate.reshape(-1)
    order = jnp.argsort(slot_expert)
    sorted_expert = slot_expert[order]
    counts = jnp.bincount(slot_expert, length=N_EXPERTS)
    padded = (counts + MOE_BLOCK - 1) // MOE_BLOCK * MOE_BLOCK
    starts = jnp.cumsum(counts) - counts
    padded_end = jnp.cumsum(padded)
    padded_start = padded_end - padded
    dest = padded_start[sorted_expert] + jnp.arange(n_slots) - starts[sorted_expert]
    n_blocks = (n_slots + N_EXPERTS * (MOE_BLOCK - 1)) // MOE_BLOCK
    cap = n_blocks * MOE_BLOCK
    buf_token = jnp.full((cap,), n, jnp.int32).at[dest].set(slot_token[order])
    buf_gate = jnp.zeros((cap,), f32).at[dest].set(slot_gate[order])
    block_expert = jnp.minimum(
        jnp.searchsorted(padded_end, jnp.arange(n_blocks) * MOE_BLOCK, side='right'), N_EXPERTS - 1)
    h_pad = jnp.concatenate([h, jnp.zeros((1, d), h.dtype)], axis=0)

    def expert_block(args):
        tok, e = args
        xb = h_pad[tok]
        return (jax.nn.silu(xb @ w_gate[e]) * (xb @ w_up[e])) @ w_down[e]

    y = lax.map(expert_block, (buf_token.reshape(n_blocks, MOE_BLOCK), block_expert))
    y = y.reshape(cap, d).astype(f32) * buf_gate[:, None]
    out = jnp.zeros((n + 1, d), f32).at[buf_token].add(y)
    return out[:n]


def setup_inputs(seed: int = 0) -> dict:
    key = jax.random.key(seed)
    ks = jax.random.split(key, 32)
    f32 = jnp.float32
    d = D_MODEL

    def nrm(i, shape, std):
        return jax.random.normal(ks[i], shape, f32) * std

    h_idx = jnp.arange(RET_HEADS, dtype=f32)
    decay_init = jnp.log(-jnp.log1p(-(2.0 ** (-5.0 - h_idx))))
    n_idx = jnp.arange(S5_STATE, dtype=f32)
    return {
        'x': nrm(0, (BATCH, SEQ, d), 1.0),
        'c': nrm(1, (BATCH, d), 1.0),
        'ctx': nrm(2, (BATCH, CTX_LEN, d), 1.0),
        'c_ctx': nrm(3, (d,), 1.0),
        'ada_w': nrm(4, (DEPTH, d, N_MOD * d), 0.5 * d ** -0.5),
        'ada_b': nrm(5, (DEPTH, N_MOD * d), 0.02),
        'norm1_g': 1.0 + nrm(6, (DEPTH, d), 0.02),
        'w_in': nrm(7, (DEPTH, d, IN_WIDTH), d ** -0.5),
        'ret_decay': decay_init + nrm(8, (DEPTH, 2, RET_HEADS), 0.01),
        's5_a_re': -0.5 + nrm(9, (DEPTH, 2, S5_GROUPS, S5_STATE), 0.01),
        's5_a_im': math.pi * n_idx + nrm(10, (DEPTH, 2, S5_GROUPS, S5_STATE), 0.01),
        's5_log_step': jax.random.uniform(ks[11], (DEPTH, 2, S5_GROUPS), f32, math.log(1e-3), math.log(1e-1)),
        's5_b_re': nrm(12, (DEPTH, 2, S5_GROUPS, S5_STATE, S5_GROUP), (2 * S5_GROUP) ** -0.5),
        's5_b_im': nrm(13, (DEPTH, 2, S5_GROUPS, S5_STATE, S5_GROUP), (2 * S5_GROUP) ** -0.5),
        's5_c_re': nrm(14, (DEPTH, 2, S5_GROUPS, S5_GROUP, S5_STATE), 0.5),
        's5_c_im': nrm(15, (DEPTH, 2, S5_GROUPS, S5_GROUP, S5_STATE), 0.5),
        's5_d': nrm(16, (DEPTH, S5_GROUPS, S5_GROUP), 1.0),
        's5_glu_w': nrm(17, (DEPTH, S5_WIDTH, S5_WIDTH), S5_WIDTH ** -0.5),
        's5_glu_b': nrm(18, (DEPTH, S5_WIDTH), 0.02),
        'w_out': nrm(19, (DEPTH, MIX_WIDTH, d), MIX_WIDTH ** -0.5),
        'norm2_g': 1.0 + nrm(20, (DEPTH, d), 0.02),
        'router_grp_w': nrm(21, (DEPTH, d, N_EXPERT_GROUPS), d ** -0.5),
        'router_grp_b': nrm(22, (DEPTH, N_EXPERT_GROUPS), 0.01),
        'router_exp_w': nrm(23, (DEPTH, d, N_EXPERTS), d ** -0.5),
        'router_exp_b': nrm(24, (DEPTH, N_EXPERTS), 0.01),
        'exp_w_gate': nrm(25, (DEPTH, N_EXPERTS, d, EXPERT_FF), d ** -0.5),
        'exp_w_up': nrm(26, (DEPTH, N_EXPERTS, d, EXPERT_FF), d ** -0.5),
        'exp_w_down': nrm(27, (DEPTH, N_EXPERTS, EXPERT_FF, d), EXPERT_FF ** -0.5),
        'final_g': 1.0 + nrm(28, (d,), 0.02),
    }


def reference(x, c, ctx, c_ctx, ada_w, ada_b, norm1_g, w_in, ret_decay,
              s5_a_re, s5_a_im, s5_log_step, s5_b_re, s5_b_im, s5_c_re, s5_c_im,
              s5_d, s5_glu_w, s5_glu_b, w_out, norm2_g,
              router_grp_w, router_grp_b, router_exp_w, router_exp_b,
              exp_w_gate, exp_w_up, exp_w_down, final_g):
    bsz, n_lat, d = x.shape
    n_ctx = ctx.shape[1]
    cos, sin = rope_2d(n_lat)
    k_scale = RET_HEAD_DIM ** -0.5
    split_at = [RET_WIDTH, 2 * RET_WIDTH, 3 * RET_WIDTH, 4 * RET_WIDTH]
    x_lat, x_ctx = x, ctx
    for layer in range(DEPTH):
        ctx_out = layer < DEPTH - 1
        mod_lat = jnp.split(adaln(c, ada_w[layer], ada_b[layer])[:, None, :], N_MOD, axis=-1)
        mod_ctx = jnp.split(adaln(c_ctx, ada_w[layer], ada_b[layer])[None, None, :], N_MOD, axis=-1)

        h_lat = modulate(rms_norm(x_lat, norm1_g[layer]), mod_lat[0], mod_lat[1])
        h_ctx = modulate(rms_norm(x_ctx, norm1_g[layer]), mod_ctx[0], mod_ctx[1])
        q_l, k_l, v_l, gate_l, u_l = jnp.split(h_lat @ w_in[layer], split_at, axis=-1)
        q_c, k_c, v_c, gate_c, u_c = jnp.split(h_ctx @ w_in[layer], split_at, axis=-1)
        ret_ctx, ret_lat = retention_mixer(
            split_heads(q_c), split_heads(k_c) * k_scale, split_heads(v_c),
            apply_rope(split_heads(q_l), cos, sin), apply_rope(split_heads(k_l), cos, sin) * k_scale,
            split_heads(v_l), ret_decay[layer], ctx_out)
        s5_ctx, s5_lat = s5_mixer(u_c, u_l, s5_a_re[layer], s5_a_im[layer], s5_log_step[layer],
                                  s5_b_re[layer], s5_b_im[layer], s5_c_re[layer], s5_c_im[layer],
                                  s5_d[layer], s5_glu_w[layer], s5_glu_b[layer], ctx_out)
        mix_lat = jnp.concatenate([ret_lat * jax.nn.silu(gate_l), s5_lat], axis=-1) @ w_out[layer]
        x_lat = x_lat + mod_lat[2] * mix_lat
        if ctx_out:
            mix_ctx = jnp.concatenate([ret_ctx * jax.nn.silu(gate_c), s5_ctx], axis=-1) @ w_out[layer]
            x_ctx = x_ctx + mod_ctx[2] * mix_ctx

        h2_lat = modulate(rms_norm(x_lat, norm2_g[layer]), mod_lat[3], mod_lat[4])
        if ctx_out:
            h2_ctx = modulate(rms_norm(x_ctx, norm2_g[layer]), mod_ctx[3], mod_ctx[4])
            tokens = jnp.concatenate([h2_ctx.reshape(-1, d), h2_lat.reshape(-1, d)], axis=0)
        else:
            tokens = h2_lat.reshape(-1, d)
        ffn = hierarchical_moe(tokens, router_grp_w[layer], router_grp_b[layer],
                               router_exp_w[layer], router_exp_b[layer],
                               exp_w_gate[layer], exp_w_up[layer], exp_w_down[layer])
        x_lat = x_lat + mod_lat[5] * ffn[ffn.shape[0] - bsz * n_lat:].reshape(bsz, n_lat, d)
        if ctx_out:
            x_ctx = x_ctx + mod_ctx[5] * ffn[:bsz * n_ctx].reshape(bsz, n_ctx, d)
    return rms_norm(x_lat, final_g)
```

```python
import numpy as np
import concourse.bass as bass
import concourse.mybir as mybir
from concourse.bass_utils import run_bass_kernel_spmd

F32 = mybir.dt.float32
BF16 = mybir.dt.bfloat16
AF = mybir.ActivationFunctionType
ALU = mybir.AluOpType
AX = mybir.AxisListType

D = 2048
NCTX = 256
NLAT = 2048
NTOK = NCTX + NLAT
NT = NTOK // 128
KT = D // 128
INW = 5120
DEPTH = 2
import os
RLEVEL = int(os.environ.get("RLEVEL", "9"))
RFLAGS = os.environ.get("RFLAGS", "WBE")
EPOCH = 30000
NDMASEM = 8


class Sched:
    def __init__(self, nc, same_engine_sync=("dve", "act", "pool")):
        self.nc = nc
        self.E = {"pe": nc.tensor, "dve": nc.vector, "act": nc.scalar,
                  "pool": nc.gpsimd, "sp": nc.sync}
        self.same = set(same_engine_sync)
        self.dmaq = {"qsp": "sp", "qpool": "pool", "qact": "act"}
        self.sems = {}
        self.count = {}
        self.waited = {}
        self.res = {}
        self.nwaits = 0

    def _sem(self, stream, idx):
        if stream in self.dmaq:
            key = (stream, (idx - 1) % NDMASEM)
            val = 16 * ((idx - 1) // NDMASEM + 1)
        else:
            key = (stream, (idx - 1) // EPOCH)
            val = (idx - 1) % EPOCH + 1
        if key not in self.sems:
            self.sems[key] = self.nc.alloc_semaphore(f"s_{key[0]}_{key[1]}")
        return self.sems[key], val

    def _is_waited(self, eng, stream, idx):
        w = self.waited.setdefault(eng, {})
        if stream in self.dmaq:
            return idx in w.get(stream, ())
        return w.get(stream, 0) >= idx

    def _wait(self, eng, stream, idx):
        if self._is_waited(eng, stream, idx):
            return
        if stream == eng and eng not in self.same:
            return
        h, v = self._sem(stream, idx)
        self.E[eng].wait_ge(h, v)
        self.nwaits += 1
        w = self.waited[eng]
        if stream in self.dmaq:
            w.setdefault(stream, set()).add(idx)
        else:
            w[stream] = idx

    def _deps(self, eng, reads, writes):
        need = set()
        for k in reads:
            st = self.res.get(k)
            if st and st[0]:
                need.add(st[0])
        for k in writes:
            st = self.res.get(k)
            if st:
                if st[0]:
                    need.add(st[0])
                need.update(st[1])
        mx = {}
        for s, i in need:
            if s in self.dmaq:
                self._wait(eng, s, i)
            else:
                mx[s] = max(mx.get(s, 0), i)
        for s, i in mx.items():
            self._wait(eng, s, i)

    def _record(self, stream, idx, reads, writes):
        for k in reads:
            st = self.res.setdefault(k, [None, []])
            if stream not in self.dmaq:
                st[1] = [r for r in st[1] if r[0] != stream]
            st[1].append((stream, idx))
        for k in writes:
            self.res[k] = [(stream, idx), []]

    def op(self, eng, fn, reads=(), writes=()):
        self._deps(eng, reads, writes)
        idx = self.count.get(eng, 0) + 1
        self.count[eng] = idx
        h, v = self._sem(eng, idx)
        fn(self.E[eng]).then_inc(h, 1)
        self._record(eng, idx, reads, writes)
        return idx

    def dma(self, q, out, in_, reads=(), writes=(), **kw):
        eng = self.dmaq[q]
        self._deps(eng, reads, writes)
        idx = self.count.get(q, 0) + 1
        self.count[q] = idx
        if idx > NDMASEM:
            self._wait(eng, q, idx - NDMASEM)
        h, v = self._sem(q, idx)
        self.E[eng].dma_start(out=out, in_=in_, **kw).then_inc(h, 16)
        self._record(q, idx, reads, writes)
        return idx

    def finish(self, keys, eng="sp"):
        self._deps(eng, list(keys), [])

    def barrier(self):
        for eng in self.E:
            for s_, n in list(self.count.items()):
                if n == 0:
                    continue
                if s_ in self.dmaq:
                    for i in range(max(1, n - NDMASEM + 1), n + 1):
                        self._wait(eng, s_, i)
                elif s_ != eng or eng in self.same:
                    self._wait(eng, s_, n)
        self.res = {}


class Arena:
    def __init__(self, nc, base, limit):
        self.nc, self.off, self.limit = nc, base, limit
        self.n = 0

    def __call__(self, name, shape, dt=F32):
        esz = 2 if dt == BF16 else 4
        size = esz
        for d_ in shape[1:]:
            size *= d_
        off = (self.off + 31) // 32 * 32
        self.off = off + size
        assert self.off <= self.limit, (name, self.off, self.limit)
        return self.nc.alloc_sbuf_tensor_at(name, list(shape), dt, offset=off).ap()


class K:
    pass


def build(stop=None, dbg=()):
    nc = bass.Bass("TRN2", target_bir_lowering=False)
    S = Sched(nc)
    k = K()
    k.nc, k.S = nc, S

    def din(name, shape, dt=F32):
        return nc.dram_tensor(name, list(shape), dt, kind="ExternalInput").ap()

    def dscr(name, shape, dt=F32):
        return nc.dram_tensor(name, list(shape), dt, kind="Internal").ap()

    def dout(name, shape, dt=F32):
        return nc.dram_tensor(name, list(shape), dt, kind="ExternalOutput").ap()

    def sb(name, shape, dt=F32):
        return nc.alloc_sbuf_tensor(name, list(shape), dt).ap()

    I = {}
    I["xin"] = din("xin", [NLAT, D])
    I["ctxin"] = din("ctxin", [NCTX, D])
    I["ccT"] = din("ccT", [128, KT, 2])
    I["ada_w"] = din("ada_w", [DEPTH, D, 6 * D])
    I["ada_b"] = din("ada_b", [DEPTH, 6 * D])
    I["norm1_g"] = din("norm1_g", [DEPTH, D])
    I["w_in"] = din("w_in", [DEPTH, D, INW])
    I["ident"] = din("ident", [128, 128])
    I["rope_cos"] = din("rope_cos", [NLAT, 256])
    I["rope_sin"] = din("rope_sin", [NLAT, 256])
    I["ret_decay"] = din("ret_decay", [DEPTH, 16])
    I["pos"] = din("pos", [128, 2, 4])
    I["maskF"] = din("maskF", [128, 128])
    I["maskB"] = din("maskB", [128, 128])
    for nm in ("s5_ar", "s5_ai", "s5_ls"):
        I[nm] = din(nm, [DEPTH, 2, 128, 32])
    for nm in ("s5_bzr", "s5_bzi", "s5_czr", "s5_czi"):
        I[nm] = din(nm, [DEPTH, 2, 128, 32, 32])
    I["s5_d"] = din("s5_d", [DEPTH, 1024])
    I["s5_glu_w"] = din("s5_glu_w", [DEPTH, 1024, 1024])
    I["s5_glu_b"] = din("s5_glu_b", [DEPTH, 1024])
    I["w_out"] = din("w_out", [DEPTH, D, D])
    I["norm2_g"] = din("norm2_g", [DEPTH, D])
    I["final_g"] = din("final_g", [D])
    I["rw"] = din("rw", [DEPTH, 128, KT, 36])
    I["rb"] = din("rb", [DEPTH, 36])
    I["exp_w_gate"] = din("exp_w_gate", [DEPTH, 32, D, 512])
    I["exp_w_up"] = din("exp_w_up", [DEPTH, 32, D, 512])
    I["exp_w_down"] = din("exp_w_down", [DEPTH, 32, 512, D])
    out_final = dout("out", [NLAT, D])

    XR = dscr("XR", [NTOK, D])
    MOD = dscr("MOD", [DEPTH, 2, 6 * D])
    PROJ = dscr("PROJ", [NTOK, INW])
    MIX = dscr("MIX", [NTOK, D])
    Y5 = dscr("Y5", [NTOK, 1024])
    Z5 = dscr("Z5", [NTOK, 1024])
    GATE = dscr("GATE", [NTOK, 32])
    ATd = dscr("ATd", [32, 128, 4, NTOK], BF16)
    dbg_out = {}
    for name, shape in dbg:
        dbg_out[name] = dout("dbg_" + name, shape)

    SLAB = 204 * 1024
    slab = nc.alloc_sbuf_tensor("slab", [128, SLAB // 4], F32)
    SB0 = nc.lookup_mloc(slab).addr
    SB_LIMIT = SB0 + SLAB
    pers = Arena(nc, SB0, SB0 + 12 * 1024)
    ident = pers("ident_sb", [128, 128])
    small = pers("small", [128, 64])
    eps_t = pers("eps_t", [128, 2])
    S.op("dve", lambda e: e.memset(eps_t[:, 0:1], 1e-6), [], ["eps"])
    S.op("dve", lambda e: e.memset(eps_t[:, 1:2], 1e-5), [], ["eps"])
    S.dma("qsp", ident, I["ident"], writes=["ident"])

    ps = [nc.alloc_psum_tensor(f"ps{i}", [128, 512], F32).ap() for i in range(8)]

    S.dma("qsp", XR[0:NCTX, :], I["ctxin"], writes=["XR"])
    S.dma("qpool", XR[NCTX:NTOK, :], I["xin"], writes=["XR"])

    ar = Arena(nc, SB0 + 12 * 1024, SB_LIMIT)
    hT = ar("hT", [128, KT, NTOK], BF16)
    xt = [ar(f"xt{i}", [128, D]) for i in range(2)]
    big = [ar(f"big{i}", [128, KT, 512]) for i in range(2)]
    wbf = [ar(f"wbf{i}", [128, KT, 512], BF16) for i in range(1)]
    wbf.append(wbf[0])
    junk = ar("junk", [128, D])
    bc = [big[0][:, 4 * j:4 * j + 4, :].rearrange("p a b -> p (a b)") for j in range(4)]
    sb = ar

    ccT = sb("ccT_sb", [128, KT, 2])
    sT = sb("sT", [128, KT, 2])
    S.dma("qsp", ccT, I["ccT"], writes=["ccT"])
    S.op("act", lambda e: e.activation(out=sT, in_=ccT, func=AF.Silu), ["ccT"], ["sT"])
    ab = sb("ab", [2, 512])
    mo = sb("mo", [2, 512])
    for l in range(DEPTH):
        for cb in range(24):
            W = big[cb % 2]
            wk = f"big{cb % 2}"
            S.dma("qsp" if cb % 2 == 0 else "qpool", W,
                  I["ada_w"][l, :, cb * 512:(cb + 1) * 512].rearrange("(k p) c -> p k c", p=128),
                  writes=[wk])
            S.dma("qsp", ab, I["ada_b"][l, cb * 512:(cb + 1) * 512].partition_broadcast(2), writes=["ab"])
            for kk in range(KT):
                S.op("pe", lambda e, kk=kk, W=W: e.matmul(ps[0][0:2, :], lhsT=sT[:, kk, :], rhs=W[:, kk, :],
                                                           start=(kk == 0), stop=(kk == KT - 1)),
                     ["sT", wk], ["ps0"])
            S.op("dve", lambda e: e.tensor_tensor(out=mo, in0=ps[0][0:2, :], in1=ab, op=ALU.add),
                 ["ps0", "ab"], ["mo"])
            S.dma("qsp", MOD[l, :, cb * 512:(cb + 1) * 512], mo, reads=["mo"], writes=["MOD"])
    if "MOD" in dbg_out:
        S.dma("qsp", dbg_out["MOD"], MOD, reads=["MOD"], writes=["dbg"])
    if stop == "A":
        S.finish(["dbg"])
        return nc

    def load_bc(l, j, tt, which, qn="qsp"):
        S.dma(qn, bc[j], MOD[l, tt, which * D:(which + 1) * D].partition_broadcast(128),
              reads=["MOD"], writes=[f"bc{j}"])

    small2 = pers("small2", [128, 160])
    rbt = pers("rbt", [128, 36])

    def router_tile(l, t, pr):
        L = small2[:, 0:36]; M = small2[:, 40:72]; m8 = small2[:, 72:80]; G1 = small2[:, 80:112]; G2 = small2[:, 112:144]
        sc = small2[:, 144:160]
        k_ = "rt_"
        S.op("dve", lambda e: e.tensor_tensor(out=L, in0=pr, in1=rbt, op=ALU.add), ["ps7", "rbt"], [k_ + "L"])
        S.op("dve", lambda e: e.tensor_reduce(out=sc[:, 0:1], in_=L[:, 0:4], axis=AX.X, op=ALU.max), [k_ + "L"], [k_ + "gmax"])
        S.op("dve", lambda e: e.tensor_scalar(out=sc[:, 1:2], in0=sc[:, 0:1], scalar1=-1.0, scalar2=None, op0=ALU.mult), [k_ + "gmax"], [k_ + "ngmax"])
        S.op("act", lambda e: e.activation(out=sc[:, 4:8], in_=L[:, 0:4], func=AF.Exp, bias=sc[:, 1:2], accum_out=sc[:, 2:3]),
             [k_ + "L", k_ + "ngmax"], [k_ + "gsum", k_ + "e4"])
        S.op("dve", lambda e: e.reciprocal(out=sc[:, 3:4], in_=sc[:, 2:3]), [k_ + "gsum"], [k_ + "ggate"])
        S.op("dve", lambda e: e.tensor_scalar(out=sc[:, 8:12], in0=L[:, 0:4], scalar1=sc[:, 0:1], scalar2=None, op0=ALU.is_equal), [k_ + "L", k_ + "gmax"], [k_ + "oh"])
        S.op("dve", lambda e: e.tensor_scalar(out=sc[:, 8:12], in0=sc[:, 8:12], scalar1=-1.0, scalar2=1e30, op0=ALU.add, op1=ALU.mult), [k_ + "oh"], [k_ + "oh"])
        for g in range(4):
            S.op("dve", lambda e, g=g: e.tensor_scalar(out=M[:, g * 8:(g + 1) * 8], in0=L[:, 4 + g * 8:12 + g * 8], scalar1=sc[:, 8 + g:9 + g], scalar2=None, op0=ALU.add),
                 [k_ + "L", k_ + "oh"], [k_ + f"M{g}"])
        MALL = [k_ + f"M{g}" for g in range(4)]
        if RLEVEL < 3:
            return
        S.op("dve", lambda e: e.max(out=m8, in_=M), MALL, [k_ + "m8"])
        if RLEVEL < 4:
            return
        S.op("dve", lambda e: e.tensor_tensor(out=sc[:, 12:13], in0=m8[:, 1:2], in1=m8[:, 0:1], op=ALU.subtract), [k_ + "m8"], [k_ + "diff"])
        S.op("act", lambda e: e.activation(out=sc[:, 13:14], in_=sc[:, 12:13], func=AF.Exp), [k_ + "diff"], [k_ + "ed"])
        S.op("dve", lambda e: e.tensor_scalar(out=sc[:, 14:15], in0=sc[:, 13:14], scalar1=1.0, scalar2=None, op0=ALU.add), [k_ + "ed"], [k_ + "w1"])
        S.op("dve", lambda e: e.reciprocal(out=sc[:, 14:15], in_=sc[:, 14:15]), [k_ + "w1"], [k_ + "w1"])
        S.op("dve", lambda e: e.tensor_tensor(out=sc[:, 15:16], in0=sc[:, 13:14], in1=sc[:, 14:15], op=ALU.mult), [k_ + "ed", k_ + "w1"], [k_ + "w2"])
        S.op("dve", lambda e: e.tensor_tensor(out=sc[:, 14:15], in0=sc[:, 14:15], in1=sc[:, 3:4], op=ALU.mult), [k_ + "w1", k_ + "ggate"], [k_ + "w1"])
        S.op("dve", lambda e: e.tensor_tensor(out=sc[:, 15:16], in0=sc[:, 15:16], in1=sc[:, 3:4], op=ALU.mult), [k_ + "w2", k_ + "ggate"], [k_ + "w2"])
        S.op("dve", lambda e: e.tensor_scalar(out=G1, in0=M, scalar1=m8[:, 0:1], scalar2=sc[:, 14:15], op0=ALU.is_equal, op1=ALU.mult), MALL + [k_ + "m8", k_ + "w1"], [k_ + "G1"])
        S.op("dve", lambda e: e.tensor_scalar(out=G2, in0=M, scalar1=m8[:, 1:2], scalar2=sc[:, 15:16], op0=ALU.is_equal, op1=ALU.mult), MALL + [k_ + "m8", k_ + "w2"], [k_ + "G2"])
        S.op("dve", lambda e: e.tensor_tensor(out=G1, in0=G1, in1=G2, op=ALU.add), [k_ + "G1", k_ + "G2"], [k_ + "G1"])
        S.dma("qsp", GATE[t * 128:(t + 1) * 128, :], G1, reads=[k_ + "G1"], writes=["GATE"])

    def norm_mod_T(l, gname, sh_idx, sc_idx, router=False, t_start=0):
        S.dma("qsp", junk, I[gname][l].partition_broadcast(128), writes=["junk"])
        for tt in range(2):
            load_bc(l, 2 * tt, tt, sc_idx)
            load_bc(l, 2 * tt + 1, tt, sh_idx, "qpool")
            S.op("dve", lambda e, tt=tt: e.scalar_tensor_tensor(out=bc[2 * tt], in0=bc[2 * tt], scalar=1.0,
                                                                in1=junk, op0=ALU.add, op1=ALU.mult),
                 [f"bc{2 * tt}", "junk"], [f"bc{2 * tt}"])
        if router:
            h32 = big[1][:, :, 0:128]
            RW = big[1][:, :, 128:164]
            if "W" in RFLAGS:
                S.dma("qsp", RW, I["rw"][l], writes=["RW"])
            if "B" in RFLAGS:
                S.dma("qsp", rbt, I["rb"][l].partition_broadcast(128), writes=["rbt"])
        for t in range(t_start, NT):
            tt = 0 if t < 2 else 1
            x_ = xt[t % 2]
            xk = f"xt{t % 2}"
            S.dma("qsp" if t % 2 == 0 else "qpool", x_, XR[t * 128:(t + 1) * 128, :], reads=["XR"], writes=[xk])
            ss = small[:, 0:1]
            S.op("act", lambda e, x_=x_: e.activation(out=junk, in_=x_, func=AF.Square, accum_out=ss),
                 [xk], ["junk", "ss"])
            S.op("act", lambda e: e.activation(out=small[:, 1:2], in_=ss, func=AF.Sqrt, scale=1.0 / D, bias=eps_t[:, 0:1]),
                 ["ss"], ["rs"])
            S.op("dve", lambda e: e.reciprocal(out=small[:, 2:3], in_=small[:, 1:2]), ["rs"], ["rstd"])
            S.op("dve", lambda e, x_=x_, tt=tt: e.scalar_tensor_tensor(out=x_, in0=x_, scalar=small[:, 2:3],
                                                                       in1=bc[2 * tt], op0=ALU.mult, op1=ALU.mult),
                 [xk, "rstd", f"bc{2 * tt}"], [xk])
            S.op("pool", lambda e, x_=x_, tt=tt: e.tensor_tensor(out=x_, in0=x_, in1=bc[2 * tt + 1], op=ALU.add),
                 [xk, f"bc{2 * tt + 1}"], [xk])
            for kq in range(4):
                p_ = ps[1 + kq % 2]
                pk = f"ps{1 + kq % 2}"
                for j in range(4):
                    kk = kq * 4 + j
                    S.op("pe", lambda e, p_=p_, j=j, kk=kk, x_=x_: e.transpose(p_[:, j * 128:(j + 1) * 128],
                                                                               x_[:, kk * 128:(kk + 1) * 128], ident),
                         [xk, "ident"], [pk])
                S.op("act" if kq % 2 == 0 else "dve",
                     lambda e, p_=p_, kq=kq, t=t: e.activation(out=hT[:, kq * 4:(kq + 1) * 4, t * 128:(t + 1) * 128],
                                                               in_=p_.rearrange("p (j c) -> p j c", j=4), func=AF.Copy)
                     if kq % 2 == 0 else
                     e.tensor_copy(out=hT[:, kq * 4:(kq + 1) * 4, t * 128:(t + 1) * 128],
                                   in_=p_.rearrange("p (j c) -> p j c", j=4)),
                     [pk], [("hT", t)])
                if router and "E" in RFLAGS:
                    S.op("act" if kq % 2 == 0 else "dve",
                         (lambda e, p_=p_, kq=kq: e.tensor_copy(out=h32[:, kq * 4:(kq + 1) * 4, :], in_=p_.rearrange("p (j c) -> p j c", j=4)))
                         if kq % 2 == 1 else
                         (lambda e, p_=p_, kq=kq: e.activation(out=h32[:, kq * 4:(kq + 1) * 4, :], in_=p_.rearrange("p (j c) -> p j c", j=4), func=AF.Copy)),
                         [pk], [("h32", kq)])
            if router and RLEVEL >= 1:
                for kk in range(KT):
                    S.op("pe", lambda e, kk=kk: e.matmul(ps[7][:, 0:36], lhsT=h32[:, kk, :], rhs=RW[:, kk, :], start=(kk == 0), stop=(kk == KT - 1)),
                         [("h32", kk // 4), "RW"], ["ps7"])
                if RLEVEL >= 2:
                    router_tile(l, t, ps[7][:, 0:36])

    HT_ALL = [("hT", t) for t in range(NT)]

    for l in range(DEPTH):
        S.barrier()
        norm_mod_T(l, "norm1_g", 0, 1)
        S.barrier()
        if l == 0 and "hT" in dbg_out:
            S.dma("qsp", dbg_out["hT"].rearrange("(k p) t -> p k t", p=128), hT, reads=HT_ALL, writes=["dbg"])
        if stop == "P1":
            S.finish(["dbg"])
            return nc

        cs = sb("cs", [128, 2, 256]) if l == 0 else cs
        po = [sb(f"po{i}", [128, 512]) for i in range(2)] if l == 0 else po
        rt = [sb(f"rt{i}", [128, 512]) for i in range(2)] if l == 0 else rt
        n_evac = 0
        for cb in range(INW // 512):
            W = big[cb % 2]
            wk = f"big{cb % 2}"
            Wb = wbf[cb % 2]
            wbk = f"wbf{cb % 2}"
            S.dma("qsp" if cb % 2 == 0 else "qpool", W,
                  I["w_in"][l, :, cb * 512:(cb + 1) * 512].rearrange("(k p) c -> p k c", p=128), writes=[wk])
            S.op("act", lambda e, W=W, Wb=Wb: e.activation(out=Wb[:, 0:8, :], in_=W[:, 0:8, :], func=AF.Copy), [wk], [wbk])
            S.op("pool", lambda e, W=W, Wb=Wb: e.tensor_copy(out=Wb[:, 8:16, :], in_=W[:, 8:16, :]), [wk], [wbk])
            for t in range(NT):
                p_ = ps[3 + t % 2]
                pk = f"ps{3 + t % 2}"
                for kk in range(KT):
                    S.op("pe", lambda e, p_=p_, kk=kk, t=t, Wb=Wb: e.matmul(p_, lhsT=hT[:, kk, t * 128:(t + 1) * 128],
                                                                            rhs=Wb[:, kk, :], start=(kk == 0), stop=(kk == KT - 1)),
                         [("hT", t), wbk], [pk])
                o_ = po[n_evac % 2]
                ok = f"po{n_evac % 2}"
                n_evac += 1
                is_qk = cb < 4
                is_k = cb in (2, 3)
                if is_qk and t >= 2:
                    lt = t - 2
                    S.dma("qsp", cs[:, 0, :], I["rope_cos"][lt * 128:(lt + 1) * 128, :], writes=["cs0"])
                    S.dma("qpool", cs[:, 1, :], I["rope_sin"][lt * 128:(lt + 1) * 128, :], writes=["cs1"])
                    pv = p_.rearrange("p (m two) -> p m two", two=2)
                    ov = o_.rearrange("p (m two) -> p m two", two=2)
                    r0 = rt[0].rearrange("p (m two) -> p m two", two=2)
                    r1 = rt[1].rearrange("p (m two) -> p m two", two=2)
                    sc = (128 ** -0.5) if is_k else 1.0
                    S.op("dve", lambda e, pv=pv, r0=r0: e.tensor_tensor(out=r0[:, :, 0], in0=pv[:, :, 0], in1=cs[:, 0, :], op=ALU.mult),
                         [pk, "cs0"], ["rt0a"])
                    S.op("dve", lambda e, pv=pv, r0=r0: e.tensor_tensor(out=r0[:, :, 1], in0=pv[:, :, 1], in1=cs[:, 1, :], op=ALU.mult),
                         [pk, "cs1"], ["rt0b"])
                    S.op("dve", lambda e, pv=pv, r1=r1: e.tensor_tensor(out=r1[:, :, 0], in0=pv[:, :, 0], in1=cs[:, 1, :], op=ALU.mult),
                         [pk, "cs1"], ["rt1a"])
                    S.op("dve", lambda e, pv=pv, r1=r1: e.tensor_tensor(out=r1[:, :, 1], in0=pv[:, :, 1], in1=cs[:, 0, :], op=ALU.mult),
                         [pk, "cs0"], ["rt1b"])
                    S.op("pool", lambda e, ov=ov, r0=r0: e.tensor_tensor(out=ov[:, :, 0], in0=r0[:, :, 0], in1=r0[:, :, 1], op=ALU.subtract),
                         ["rt0a", "rt0b"], [ok])
                    S.op("pool", lambda e, ov=ov, r1=r1: e.tensor_tensor(out=ov[:, :, 1], in0=r1[:, :, 0], in1=r1[:, :, 1], op=ALU.add),
                         ["rt1a", "rt1b"], [ok])
                    if is_k:
                        S.op("act", lambda e, o_=o_, sc=sc: e.activation(out=o_, in_=o_, func=AF.Copy, scale=sc), [ok], [ok])
                elif is_k:
                    S.op("act", lambda e, o_=o_, p_=p_: e.activation(out=o_, in_=p_, func=AF.Copy, scale=128 ** -0.5), [pk], [ok])
                else:
                    S.op("act", lambda e, o_=o_, p_=p_: e.activation(out=o_, in_=p_, func=AF.Copy), [pk], [ok])
                S.dma("qsp" if n_evac % 2 else "qpool", PROJ[t * 128:(t + 1) * 128, cb * 512:(cb + 1) * 512], o_,
                      reads=[ok], writes=["PROJ"])
        if l == 0 and "PROJ" in dbg_out:
            S.dma("qsp", dbg_out["PROJ"], PROJ, reads=["PROJ"], writes=["dbg"])
        if stop == "P2":
            S.finish(["dbg"])
            return nc

        S.barrier()
        a3 = Arena(nc, SB0 + 12 * 1024, SB_LIMIT)
        tg = f"L{l}"
        qf = a3("qf" + tg, [128, NT, 128]); kf = a3("kf" + tg, [128, NT, 128])
        vf = a3("vf" + tg, [128, NT, 128]); gf = a3("gf" + tg, [128, NT, 128])
        qs = a3("qs" + tg, [128, NT, 128]); ks = a3("ks" + tg, [128, NT, 128])
        qsT = a3("qsT" + tg, [128, NT * 128], BF16); ksT = a3("ksT" + tg, [128, NT * 128], BF16)
        kst = a3("kst" + tg, [128, NT, 128], BF16); vb = a3("vb" + tg, [128, NT, 128], BF16)
        RET = a3("RET" + tg, [128, NT, 128]); sq = a3("sq" + tg, [128, NT, 128])
        mF = a3("mF" + tg, [128, 128]); mB = a3("mB" + tg, [128, 128])
        scb = [a3(f"scb{i}" + tg, [128, 128], BF16) for i in range(2)]
        Sf = a3("Sf" + tg, [128, 128]); Sb = a3("Sb" + tg, [128, 128], BF16)
        DEC = a3("DEC" + tg, [128, 2, 8, 4]); lg = a3("lg" + tg, [128, 16]); POS = a3("POS" + tg, [128, 2, 4])
        stat = a3("stat" + tg, [128, 4, NT])
        S.dma("qsp", lg, I["ret_decay"][l].partition_broadcast(128), writes=["lg"])
        S.dma("qsp", POS, I["pos"], writes=["POS"])
        S.dma("qpool", mF, I["maskF"], writes=["mF"])
        S.dma("qpool", mB, I["maskB"], writes=["mB"])
        S.op("act", lambda e: e.activation(out=lg, in_=lg, func=AF.Exp), ["lg"], ["lg"])
        S.op("dve", lambda e: e.tensor_scalar(out=lg, in0=lg, scalar1=-1.0, scalar2=None, op0=ALU.mult), ["lg"], ["lg"])
        for d_ in range(2):
            for h in range(8):
                S.op("act", lambda e, d_=d_, h=h: e.activation(out=DEC[:, d_, h, :], in_=POS[:, d_, :], func=AF.Exp,
                                                               scale=lg[:, d_ * 8 + h:d_ * 8 + h + 1]),
                     ["lg", "POS"], ["DEC"])
        order_f = list(range(NT))
        order_b = [1, 0] + list(range(NT - 1, 1, -1))
        for h in range(8):
            def hv(off):
                return PROJ[:, off + h * 128: off + (h + 1) * 128].rearrange("(t p) c -> p t c", p=128)
            S.dma("qsp", qf, hv(0), reads=["PROJ"], writes=["qf"])
            S.dma("qpool", kf, hv(1024), reads=["PROJ"], writes=["kf"])
            S.dma("qsp", vf, hv(2048), reads=["PROJ"], writes=["vf"])
            S.dma("qpool", gf, hv(3072), reads=["PROJ"], writes=["gf"])
            S.op("pool", lambda e: e.tensor_copy(out=vb, in_=vf), ["vf"], ["vb"])
            for d_ in range(2):
                mk, mkk = (mF, "mF") if d_ == 0 else (mB, "mB")
                S.op("dve", lambda e, d_=d_, h=h: e.tensor_scalar(out=qs, in0=qf, scalar1=DEC[:, d_, h, 0:1], scalar2=None, op0=ALU.mult),
                     ["qf", "DEC"], ["qs"])
                S.op("pool", lambda e, d_=d_, h=h: e.tensor_scalar(out=ks, in0=kf, scalar1=DEC[:, d_, h, 1:2], scalar2=None, op0=ALU.mult),
                     ["kf", "DEC"], ["ks"])
                S.op("act", lambda e, d_=d_, h=h: e.activation(out=kst, in_=kf, func=AF.Copy, scale=DEC[:, d_, h, 2:3]),
                     ["kf", "DEC"], ["kst"])
                for src, srck, dst, dstk, pb in ((qs, "qs", qsT, "qsT", 0), (ks, "ks", ksT, "ksT", 1)):
                    for t0 in range(0, NT, 4):
                        n4 = min(4, NT - t0)
                        for j in range(n4):
                            S.op("pe", lambda e, src=src, t0=t0, j=j, pb=pb: e.transpose(ps[pb][:, j * 128:(j + 1) * 128], src[:, t0 + j, :], ident),
                                 [srck, "ident"], [f"ps{pb}"])
                        if pb == 0:
                            S.op("act", lambda e, dst=dst, t0=t0, n4=n4, pb=pb: e.activation(out=dst[:, t0 * 128:(t0 + n4) * 128], in_=ps[pb][:, 0:n4 * 128], func=AF.Copy),
                                 [f"ps{pb}"], [dstk])
                        else:
                            S.op("dve", lambda e, dst=dst, t0=t0, n4=n4, pb=pb: e.tensor_copy(out=dst[:, t0 * 128:(t0 + n4) * 128], in_=ps[pb][:, 0:n4 * 128]),
                                 [f"ps{pb}"], [dstk])
                S.op("dve", lambda e: e.memset(Sf, 0.0), [], ["Sf"])
                S.op("pool", lambda e: e.memset(Sb, 0.0), [], ["Sb"])
                for ci, t in enumerate(order_f if d_ == 0 else order_b):
                    sl = slice(t * 128, (t + 1) * 128)
                    pS = ps[2 + ci % 2]; pSk = f"ps{2 + ci % 2}"
                    pO = ps[4 + ci % 2]; pOk = f"ps{4 + ci % 2}"
                    sc_ = scb[ci % 2]; sck = f"scb{ci % 2}"
                    S.op("pe", lambda e, pS=pS, sl=sl: e.matmul(pS[:, 0:128], lhsT=ksT[:, sl], rhs=qsT[:, sl], start=True, stop=True),
                         ["ksT", "qsT"], [pSk])
                    S.op("dve", lambda e, pS=pS, sc_=sc_, mk=mk: e.tensor_tensor(out=sc_, in0=pS[:, 0:128], in1=mk, op=ALU.mult),
                         [pSk, mkk], [sck])
                    S.op("pe", lambda e, pO=pO, sc_=sc_, t=t: e.matmul(pO[:, 0:128], lhsT=sc_, rhs=vb[:, t, :], start=True, stop=False),
                         [sck, "vb"], [pOk])
                    S.op("pe", lambda e, pO=pO, sl=sl: e.matmul(pO[:, 0:128], lhsT=qsT[:, sl], rhs=Sb, start=False, stop=True),
                         ["qsT", "Sb"], [pOk])
                    if d_ == 0:
                        S.op("act", lambda e, pO=pO, t=t: e.activation(out=RET[:, t, :], in_=pO[:, 0:128], func=AF.Copy),
                             [pOk], [("RET", t)])
                    else:
                        S.op("dve", lambda e, pO=pO, t=t: e.tensor_tensor(out=RET[:, t, :], in0=pO[:, 0:128], in1=RET[:, t, :], op=ALU.add),
                             [pOk, ("RET", t)], [("RET", t)])
                    S.op("pe", lambda e, t=t: e.matmul(ps[6][:, 0:128], lhsT=kst[:, t, :], rhs=vb[:, t, :], start=True, stop=True),
                         ["kst", "vb"], ["ps6"])
                    S.op("dve", lambda e, d_=d_, h=h: e.scalar_tensor_tensor(out=Sf, in0=Sf, scalar=DEC[:, d_, h, 3:4], in1=ps[6][:, 0:128],
                                                                             op0=ALU.mult, op1=ALU.add),
                         ["Sf", "ps6", "DEC"], ["Sf"])
                    S.op("act", lambda e: e.activation(out=Sb, in_=Sf, func=AF.Copy), ["Sf"], ["Sb"])
            RALL = [("RET", t) for t in range(NT)]
            S.op("dve", lambda e: e.tensor_reduce(out=stat[:, 0, :], in_=RET, axis=AX.X, op=ALU.add), RALL, ["st0"])
            S.op("pool", lambda e: e.tensor_tensor(out=sq, in0=RET, in1=RET, op=ALU.mult), RALL, ["sq"])
            S.op("dve", lambda e: e.tensor_reduce(out=stat[:, 1, :], in_=sq, axis=AX.X, op=ALU.add), ["sq"], ["st1"])
            S.op("dve", lambda e: e.tensor_scalar(out=stat[:, 0, :], in0=stat[:, 0, :], scalar1=1.0 / 128, scalar2=None, op0=ALU.mult), ["st0"], ["st0"])
            S.op("dve", lambda e: e.tensor_tensor(out=stat[:, 2, :], in0=stat[:, 0, :], in1=stat[:, 0, :], op=ALU.mult), ["st0"], ["st2"])
            S.op("dve", lambda e: e.scalar_tensor_tensor(out=stat[:, 1, :], in0=stat[:, 1, :], scalar=1.0 / 128, in1=stat[:, 2, :],
                                                         op0=ALU.mult, op1=ALU.subtract), ["st1", "st2"], ["st1"])
            S.op("act", lambda e: e.activation(out=stat[:, 1, :], in_=stat[:, 1, :], func=AF.Sqrt, bias=eps_t[:, 1:2]), ["st1", "eps"], ["st1"])
            S.op("dve", lambda e: e.reciprocal(out=stat[:, 3, :], in_=stat[:, 1, :]), ["st1"], ["st3"])
            S.op("act", lambda e: e.activation(out=gf, in_=gf, func=AF.Silu), ["gf"], ["gf"])
            for t in range(NT):
                S.op("dve", lambda e, t=t: e.tensor_scalar(out=RET[:, t, :], in0=RET[:, t, :], scalar1=stat[:, 0, t:t + 1],
                                                           scalar2=stat[:, 3, t:t + 1], op0=ALU.subtract, op1=ALU.mult),
                     [("RET", t), "st0", "st3"], [("RET", t)])
            S.op("pool", lambda e: e.tensor_tensor(out=RET, in0=RET, in1=gf, op=ALU.mult), RALL + ["gf"], RALL)
            S.dma("qsp", MIX[:, h * 128:(h + 1) * 128].rearrange("(t p) c -> p t c", p=128), RET, reads=RALL, writes=["MIX"])
        if l == 0 and "MIX" in dbg_out:
            S.dma("qsp", dbg_out["MIX"], MIX, reads=["MIX"], writes=["dbg"])
        if stop == "P3":
            S.finish(["dbg"])
            return nc

        S.barrier()
        a4 = Arena(nc, SB0 + 12 * 1024, SB_LIMIT)
        tg = f"s5L{l}"
        def A4(n, shp, dt=F32):
            return a4(n + tg, shp, dt)
        ublk = A4("ublk", [128, 1024]); ysb = A4("ysb", [128, 1024]); dsk = A4("dsk", [128, 1024])
        uT32 = A4("uT32", [32, 32, 128], BF16)
        BUr = A4("BUr", [128, 32, 128]); BUi = A4("BUi", [128, 32, 128])
        Hbr = A4("Hbr", [128, 32, 128], BF16); Hbi = A4("Hbi", [128, 32, 128], BF16)
        BZr = A4("BZr", [128, 32, 32]); BZi = A4("BZi", [128, 32, 32])
        Bbr = A4("Bbr", [128, 32, 32]); Bbi = A4("Bbi", [128, 32, 32])
        CZr = A4("CZr", [128, 32, 32]); CZi = A4("CZi", [128, 32, 32])
        Cbr = A4("Cbr", [128, 32, 32], BF16); Cbi = A4("Cbi", [128, 32, 32], BF16)
        BbT = A4("BbT", [32, 32, 2, 128], BF16)
        pm = {n: A4(n, [128, 32]) for n in ("ar", "ai", "dt", "xr", "ang", "mag", "c", "s", "lr", "li", "nr", "den",
                                            "fr", "fi", "nfi", "t1", "t2", "t3", "t4", "Hr", "Hi", "nli")}
        hpi = A4("hpi", [128, 1])
        S.op("dve", lambda e: e.memset(hpi, float(np.pi / 2)), [], ["hpi"])
        S.dma("qsp", dsk, I["s5_d"][l].partition_broadcast(128), writes=["dsk"])

        def tt_(eng, out, a, b, op, rk, wk):
            S.op(eng, lambda e: e.tensor_tensor(out=out, in0=a, in1=b, op=op), rk, wk)

        for d_ in range(2):
            S.dma("qsp", pm["ar"], I["s5_ar"][l, d_], writes=["p.ar"])
            S.dma("qpool", pm["ai"], I["s5_ai"][l, d_], writes=["p.ai"])
            S.dma("qsp", pm["dt"], I["s5_ls"][l, d_], writes=["p.dt"])
            S.dma("qsp", BZr, I["s5_bzr"][l, d_], writes=["BZr"])
            S.dma("qpool", BZi, I["s5_bzi"][l, d_], writes=["BZi"])
            S.dma("qsp", CZr, I["s5_czr"][l, d_], writes=["CZr"])
            S.dma("qpool", CZi, I["s5_czi"][l, d_], writes=["CZi"])
            P = pm
            S.op("act", lambda e: e.activation(out=P["dt"], in_=P["dt"], func=AF.Exp), ["p.dt"], ["p.dt"])
            tt_("dve", P["xr"], P["ar"], P["dt"], ALU.mult, ["p.ar", "p.dt"], ["p.xr"])
            tt_("dve", P["ang"], P["ai"], P["dt"], ALU.mult, ["p.ai", "p.dt"], ["p.ang"])
            S.op("act", lambda e: e.activation(out=P["mag"], in_=P["xr"], func=AF.Exp), ["p.xr"], ["p.mag"])
            S.op("act", lambda e: e.activation(out=P["s"], in_=P["ang"], func=AF.Sin, scale=1.0 / 16), ["p.ang"], ["p.s"])
            S.op("act", lambda e: e.activation(out=P["c"], in_=P["ang"], func=AF.Sin, scale=-1.0 / 16, bias=hpi[:, 0:1]), ["p.ang", "hpi"], ["p.c"])
            for _ in range(4):
                tt_("dve", P["t1"], P["c"], P["c"], ALU.mult, ["p.c"], ["p.t1"])
                tt_("dve", P["t2"], P["s"], P["s"], ALU.mult, ["p.s"], ["p.t2"])
                tt_("dve", P["t3"], P["c"], P["s"], ALU.mult, ["p.c", "p.s"], ["p.t3"])
                tt_("dve", P["c"], P["t1"], P["t2"], ALU.subtract, ["p.t1", "p.t2"], ["p.c"])
                tt_("dve", P["s"], P["t3"], P["t3"], ALU.add, ["p.t3"], ["p.s"])
            tt_("dve", P["lr"], P["mag"], P["c"], ALU.mult, ["p.mag", "p.c"], ["p.lr"])
            tt_("dve", P["li"], P["mag"], P["s"], ALU.mult, ["p.mag", "p.s"], ["p.li"])
            S.op("dve", lambda e: e.tensor_scalar(out=P["nli"], in0=P["li"], scalar1=-1.0, scalar2=None, op0=ALU.mult), ["p.li"], ["p.nli"])
            S.op("dve", lambda e: e.tensor_scalar(out=P["nr"], in0=P["lr"], scalar1=-1.0, scalar2=None, op0=ALU.add), ["p.lr"], ["p.nr"])
            tt_("dve", P["t1"], P["ar"], P["ar"], ALU.mult, ["p.ar"], ["p.t1"])
            tt_("dve", P["t2"], P["ai"], P["ai"], ALU.mult, ["p.ai"], ["p.t2"])
            tt_("dve", P["den"], P["t1"], P["t2"], ALU.add, ["p.t1", "p.t2"], ["p.den"])
            S.op("dve", lambda e: e.reciprocal(out=P["den"], in_=P["den"]), ["p.den"], ["p.den"])
            tt_("dve", P["t1"], P["nr"], P["ar"], ALU.mult, ["p.nr", "p.ar"], ["p.t1"])
            tt_("dve", P["t2"], P["li"], P["ai"], ALU.mult, ["p.li", "p.ai"], ["p.t2"])
            tt_("dve", P["t1"], P["t1"], P["t2"], ALU.add, ["p.t1", "p.t2"], ["p.t1"])
            tt_("dve", P["fr"], P["t1"], P["den"], ALU.mult, ["p.t1", "p.den"], ["p.fr"])
            tt_("dve", P["t1"], P["li"], P["ar"], ALU.mult, ["p.li", "p.ar"], ["p.t1"])
            tt_("dve", P["t2"], P["nr"], P["ai"], ALU.mult, ["p.nr", "p.ai"], ["p.t2"])
            tt_("dve", P["t1"], P["t1"], P["t2"], ALU.subtract, ["p.t1", "p.t2"], ["p.t1"])
            tt_("dve", P["fi"], P["t1"], P["den"], ALU.mult, ["p.t1", "p.den"], ["p.fi"])
            S.op("dve", lambda e: e.tensor_scalar(out=P["nfi"], in0=P["fi"], scalar1=-1.0, scalar2=None, op0=ALU.mult), ["p.fi"], ["p.nfi"])
            for gh in range(32):
                S.op("dve", lambda e, gh=gh: e.tensor_scalar(out=Bbr[:, gh, :], in0=BZr[:, gh, :], scalar1=P["fr"][:, gh:gh + 1], scalar2=None, op0=ALU.mult),
                     ["BZr", "p.fr"], [("Bbr", gh)])
                S.op("dve", lambda e, gh=gh: e.scalar_tensor_tensor(out=Bbr[:, gh, :], in0=BZi[:, gh, :], scalar=P["nfi"][:, gh:gh + 1], in1=Bbr[:, gh, :],
                                                                    op0=ALU.mult, op1=ALU.add), ["BZi", "p.nfi", ("Bbr", gh)], [("Bbr", gh)])
                S.op("dve", lambda e, gh=gh: e.tensor_scalar(out=Bbi[:, gh, :], in0=BZi[:, gh, :], scalar1=P["fr"][:, gh:gh + 1], scalar2=None, op0=ALU.mult),
                     ["BZi", "p.fr"], [("Bbi", gh)])
                S.op("dve", lambda e, gh=gh: e.scalar_tensor_tensor(out=Bbi[:, gh, :], in0=BZr[:, gh, :], scalar=P["fi"][:, gh:gh + 1], in1=Bbi[:, gh, :],
                                                                    op0=ALU.mult, op1=ALU.add), ["BZr", "p.fi", ("Bbi", gh)], [("Bbi", gh)])
            for ri, src in enumerate((Bbr, Bbi)):
                srck = "Bbr" if ri == 0 else "Bbi"
                for g0 in range(0, 32, 4):
                    for j in range(4):
                        S.op("pe", lambda e, src=src, g0=g0, j=j: e.transpose(ps[0][0:32, j * 128:(j + 1) * 128], src[:, g0 + j, :], ident),
                             [(srck, g0 + j), "ident"], ["ps0"])
                    S.op("act", lambda e, g0=g0, ri=ri: e.activation(out=BbT[:, g0:g0 + 4, ri, :], in_=ps[0][0:32, :].rearrange("p (j c) -> p j c", j=4), func=AF.Copy),
                         ["ps0"], ["BbT"])
            S.op("act", lambda e: e.activation(out=Cbr, in_=CZr, func=AF.Copy), ["CZr"], ["Cbr"])
            S.op("act", lambda e: e.activation(out=Cbi, in_=CZi, func=AF.Copy, scale=-1.0), ["CZi"], ["Cbi"])
            S.op("dve", lambda e: e.memset(P["Hr"], 0.0), [], ["Hr"])
            S.op("dve", lambda e: e.memset(P["Hi"], 0.0), [], ["Hi"])
            order = list(range(NT)) if d_ == 0 else [1, 0] + list(range(NT - 1, 1, -1))
            for t in order:
                S.dma("qsp", ublk, PROJ[t * 128:(t + 1) * 128, 4096:5120], reads=["PROJ"], writes=["ublk"])
                for g0 in range(0, 32, 4):
                    for j in range(4):
                        S.op("pe", lambda e, g0=g0, j=j: e.transpose(ps[1][0:32, j * 128:(j + 1) * 128], ublk[:, (g0 + j) * 32:(g0 + j + 1) * 32], ident),
                             ["ublk", "ident"], ["ps1"])
                    S.op("act", lambda e, g0=g0: e.activation(out=uT32[:, g0:g0 + 4, :], in_=ps[1][0:32, :].rearrange("p (j c) -> p j c", j=4), func=AF.Copy),
                         ["ps1"], ["uT32"])
                for ri, dst, dk in ((0, BUr, "BUr"), (1, BUi, "BUi")):
                    for g0 in range(0, 32, 4):
                        pb = 2 + (g0 // 4) % 2
                        for j in range(4):
                            S.op("pe", lambda e, pb=pb, g0=g0, j=j, ri=ri: e.matmul(ps[pb][:, j * 128:(j + 1) * 128], lhsT=BbT[:, g0 + j, ri, :],
                                                                                   rhs=uT32[:, g0 + j, :], start=True, stop=True),
                                 ["BbT", "uT32"], [f"ps{pb}"])
                        S.op("act" if ri == 0 else "pool" if False else "act",
                             lambda e, pb=pb, dst=dst, g0=g0: e.activation(out=dst[:, g0:g0 + 4, :], in_=ps[pb].rearrange("p (j c) -> p j c", j=4), func=AF.Copy),
                             [f"ps{pb}"], [dk])
                taus = range(128) if d_ == 0 else range(127, -1, -1)
                prev_r, prev_i = P["Hr"], P["Hi"]
                for tau in taus:
                    cr, ci_ = BUr[:, :, tau], BUi[:, :, tau]
                    S.op("dve", lambda e, a=prev_r: e.tensor_tensor(out=P["t1"], in0=P["lr"], in1=a, op=ALU.mult), ["BUr", "Hr", "p.lr"], ["p.t1"])
                    S.op("dve", lambda e, a=prev_i: e.tensor_tensor(out=P["t2"], in0=P["nli"], in1=a, op=ALU.mult), ["BUi", "Hi", "p.nli"], ["p.t2"])
                    S.op("dve", lambda e, a=prev_i: e.tensor_tensor(out=P["t3"], in0=P["lr"], in1=a, op=ALU.mult), ["BUi", "Hi", "p.lr"], ["p.t3"])
                    S.op("dve", lambda e, a=prev_r: e.tensor_tensor(out=P["t4"], in0=P["li"], in1=a, op=ALU.mult), ["BUr", "Hr", "p.li"], ["p.t4"])
                    S.op("dve", lambda e: e.tensor_tensor(out=P["t1"], in0=P["t1"], in1=P["t2"], op=ALU.add), ["p.t1", "p.t2"], ["p.t1"])
                    S.op("dve", lambda e: e.tensor_tensor(out=P["t3"], in0=P["t3"], in1=P["t4"], op=ALU.add), ["p.t3", "p.t4"], ["p.t3"])
                    S.op("dve", lambda e, cr=cr: e.tensor_tensor(out=cr, in0=cr, in1=P["t1"], op=ALU.add), ["BUr", "p.t1"], ["BUr"])
                    S.op("dve", lambda e, ci_=ci_: e.tensor_tensor(out=ci_, in0=ci_, in1=P["t3"], op=ALU.add), ["BUi", "p.t3"], ["BUi"])
                    prev_r, prev_i = cr, ci_
                S.op("dve", lambda e, a=prev_r: e.tensor_copy(out=P["Hr"], in_=a), ["BUr"], ["Hr"])
                S.op("dve", lambda e, a=prev_i: e.tensor_copy(out=P["Hi"], in_=a), ["BUi"], ["Hi"])
                S.op("act", lambda e: e.activation(out=Hbr, in_=BUr, func=AF.Copy), ["BUr"], ["Hbr"])
                S.op("pool", lambda e: e.tensor_copy(out=Hbi, in_=BUi), ["BUi"], ["Hbi"])
                for gh in range(32):
                    pb = 4 + gh // 16
                    c0 = (gh % 16) * 32
                    S.op("pe", lambda e, pb=pb, c0=c0, gh=gh: e.matmul(ps[pb][:, c0:c0 + 32], lhsT=Hbr[:, gh, :], rhs=Cbr[:, gh, :], start=True, stop=False),
                         ["Hbr", "Cbr"], [f"ps{pb}"])
                    S.op("pe", lambda e, pb=pb, c0=c0, gh=gh: e.matmul(ps[pb][:, c0:c0 + 32], lhsT=Hbi[:, gh, :], rhs=Cbi[:, gh, :], start=False, stop=True),
                         ["Hbi", "Cbi"], [f"ps{pb}"])
                if d_ == 0:
                    S.op("pool", lambda e: e.tensor_tensor(out=ysb, in0=ublk, in1=dsk, op=ALU.mult), ["ublk", "dsk"], ["ysb"])
                else:
                    S.dma("qpool", ysb, Y5[t * 128:(t + 1) * 128, :], reads=["Y5"], writes=["ysb"])
                for hb in range(2):
                    S.op("dve", lambda e, hb=hb: e.tensor_tensor(out=ysb[:, hb * 512:(hb + 1) * 512], in0=ps[4 + hb], in1=ysb[:, hb * 512:(hb + 1) * 512], op=ALU.add),
                         [f"ps{4 + hb}", "ysb"], ["ysb"])
                S.dma("qsp", Y5[t * 128:(t + 1) * 128, :], ysb, reads=["ysb"], writes=["Y5"])
        if l == 0 and "Y5" in dbg_out:
            S.dma("qsp", dbg_out["Y5"], Y5, reads=["Y5"], writes=["dbg"])
        if stop == "P4":
            S.finish(["dbg"])
            return nc

        S.barrier()
        last = (l == DEPTH - 1)
        T0 = 2 if last else 0
        for t in range(T0, NT):
            x_ = xt[t % 2]; xk = f"xt{t % 2}"
            y_ = x_[:, 0:1024]; w_ = x_[:, 1024:2048]
            S.dma("qsp", y_, Y5[t * 128:(t + 1) * 128, :], reads=["Y5"], writes=[xk])
            S.op("dve", lambda e, y_=y_, w_=w_: e.tensor_tensor(out=w_, in0=y_, in1=y_, op=ALU.mult), [xk], [xk])
            S.op("dve", lambda e, w_=w_: e.tensor_scalar(out=w_, in0=w_, scalar1=0.044715, scalar2=1.0, op0=ALU.mult, op1=ALU.add), [xk], [xk])
            S.op("dve", lambda e, y_=y_, w_=w_: e.tensor_tensor(out=w_, in0=w_, in1=y_, op=ALU.mult), [xk], [xk])
            S.op("act", lambda e, w_=w_: e.activation(out=w_, in_=w_, func=AF.Tanh, scale=float(np.sqrt(2.0 / np.pi))), [xk], [xk])
            S.op("dve", lambda e, w_=w_: e.tensor_scalar(out=w_, in0=w_, scalar1=0.5, scalar2=0.5, op0=ALU.mult, op1=ALU.add), [xk], [xk])
            S.op("dve", lambda e, y_=y_, w_=w_: e.tensor_tensor(out=y_, in0=w_, in1=y_, op=ALU.mult), [xk], [xk])
            S.dma("qpool", Z5[t * 128:(t + 1) * 128, :], y_, reads=[xk], writes=["Z5"])
            for kq in range(2):
                p_ = ps[1 + kq]; pk = f"ps{1 + kq}"
                for j in range(4):
                    kk = kq * 4 + j
                    S.op("pe", lambda e, p_=p_, j=j, kk=kk, y_=y_: e.transpose(p_[:, j * 128:(j + 1) * 128], y_[:, kk * 128:(kk + 1) * 128], ident),
                         [xk, "ident"], [pk])
                S.op("act", lambda e, p_=p_, kq=kq, t=t: e.activation(out=hT[:, kq * 4:(kq + 1) * 4, t * 128:(t + 1) * 128],
                                                                       in_=p_.rearrange("p (j c) -> p j c", j=4), func=AF.Copy), [pk], [("hT", t)])
        S.dma("qsp", junk[:, 0:1024], I["s5_glu_b"][l].partition_broadcast(128), writes=["junk"])
        for cb in range(2):
            W = big[cb % 2]; wk = f"big{cb % 2}"; Wb = wbf[0]; wbk = "wbf0"
            S.dma("qsp", W[:, 0:8, :], I["s5_glu_w"][l, :, cb * 512:(cb + 1) * 512].rearrange("(k p) c -> p k c", p=128), writes=[wk])
            S.op("act", lambda e, W=W, Wb=Wb: e.activation(out=Wb[:, 0:8, :], in_=W[:, 0:8, :], func=AF.Copy), [wk], [wbk])
            for t in range(T0, NT):
                p_ = ps[3 + t % 2]; pk = f"ps{3 + t % 2}"
                for kk in range(8):
                    S.op("pe", lambda e, p_=p_, kk=kk, t=t, Wb=Wb: e.matmul(p_, lhsT=hT[:, kk, t * 128:(t + 1) * 128], rhs=Wb[:, kk, :],
                                                                            start=(kk == 0), stop=(kk == 7)), [("hT", t), wbk], [pk])
                o_ = po[t % 2]; ok = f"po{t % 2}"; z_ = rt[t % 2]; zk = f"rt{t % 2}"
                S.dma("qpool", z_, Z5[t * 128:(t + 1) * 128, cb * 512:(cb + 1) * 512], reads=["Z5"], writes=[zk])
                S.op("dve", lambda e, o_=o_, p_=p_, cb=cb: e.tensor_tensor(out=o_, in0=p_, in1=junk[:, cb * 512:(cb + 1) * 512], op=ALU.add), [pk, "junk"], [ok])
                S.op("act", lambda e, o_=o_: e.activation(out=o_, in_=o_, func=AF.Sigmoid), [ok], [ok])
                S.op("pool", lambda e, o_=o_, z_=z_: e.tensor_tensor(out=o_, in0=o_, in1=z_, op=ALU.mult), [ok, zk], [ok])
                S.dma("qsp", MIX[t * 128:(t + 1) * 128, 1024 + cb * 512:1024 + (cb + 1) * 512], o_, reads=[ok], writes=["MIX"])

        S.barrier()
        for t in range(T0, NT):
            x_ = xt[t % 2]; xk = f"xt{t % 2}"
            S.dma("qsp" if t % 2 == 0 else "qpool", x_, MIX[t * 128:(t + 1) * 128, :], reads=["MIX"], writes=[xk])
            for kq in range(4):
                p_ = ps[1 + kq % 2]; pk = f"ps{1 + kq % 2}"
                for j in range(4):
                    kk = kq * 4 + j
                    S.op("pe", lambda e, p_=p_, j=j, kk=kk, x_=x_: e.transpose(p_[:, j * 128:(j + 1) * 128], x_[:, kk * 128:(kk + 1) * 128], ident),
                         [xk, "ident"], [pk])
                S.op("act" if kq % 2 == 0 else "dve",
                     (lambda e, p_=p_, kq=kq, t=t: e.activation(out=hT[:, kq * 4:(kq + 1) * 4, t * 128:(t + 1) * 128], in_=p_.rearrange("p (j c) -> p j c", j=4), func=AF.Copy))
                     if kq % 2 == 0 else
                     (lambda e, p_=p_, kq=kq, t=t: e.tensor_copy(out=hT[:, kq * 4:(kq + 1) * 4, t * 128:(t + 1) * 128], in_=p_.rearrange("p (j c) -> p j c", j=4))),
                     [pk], [("hT", t)])
        S.barrier()
        for tt in range(2):
            S.dma("qsp", xt[tt], MOD[l, tt, 2 * D:3 * D].partition_broadcast(128), reads=["MOD"], writes=[f"xt{tt}"])
        for cb in range(4):
            W = big[cb % 2]; wk = f"big{cb % 2}"; Wb = wbf[0]; wbk = "wbf0"
            S.dma("qsp" if cb % 2 == 0 else "qpool", W, I["w_out"][l, :, cb * 512:(cb + 1) * 512].rearrange("(k p) c -> p k c", p=128), writes=[wk])
            S.op("act", lambda e, W=W, Wb=Wb: e.activation(out=Wb[:, 0:8, :], in_=W[:, 0:8, :], func=AF.Copy), [wk], [wbk])
            S.op("pool", lambda e, W=W, Wb=Wb: e.tensor_copy(out=Wb[:, 8:16, :], in_=W[:, 8:16, :]), [wk], [wbk])
            for t in range(T0, NT):
                tt = 0 if t < 2 else 1
                p_ = ps[3 + t % 2]; pk = f"ps{3 + t % 2}"
                for kk in range(KT):
                    S.op("pe", lambda e, p_=p_, kk=kk, t=t, Wb=Wb: e.matmul(p_, lhsT=hT[:, kk, t * 128:(t + 1) * 128], rhs=Wb[:, kk, :],
                                                                            start=(kk == 0), stop=(kk == KT - 1)), [("hT", t), wbk], [pk])
                o_ = po[t % 2]; ok = f"po{t % 2}"; z_ = rt[t % 2]; zk = f"rt{t % 2}"
                S.dma("qpool", z_, XR[t * 128:(t + 1) * 128, cb * 512:(cb + 1) * 512], reads=["XR"], writes=[zk])
                S.op("dve", lambda e, o_=o_, p_=p_, cb=cb, tt=tt: e.tensor_tensor(out=o_, in0=p_, in1=xt[tt][:, cb * 512:(cb + 1) * 512], op=ALU.mult), [pk, f"xt{tt}"], [ok])
                S.op("pool", lambda e, o_=o_, z_=z_: e.tensor_tensor(out=o_, in0=o_, in1=z_, op=ALU.add), [ok, zk], [ok])
                S.dma("qsp", XR[t * 128:(t + 1) * 128, cb * 512:(cb + 1) * 512], o_, reads=[ok], writes=["XR"])
        if l == 0 and "X1" in dbg_out:
            S.dma("qsp", dbg_out["X1"], XR, reads=["XR"], writes=["dbg"])
        if stop == "P5":
            S.finish(["dbg"])
            return nc

        S.barrier()
        norm_mod_T(l, "norm2_g", 3, 4, router=(RLEVEL >= 0), t_start=T0)
        if l == 0 and "GATE" in dbg_out:
            S.dma("qsp", dbg_out["GATE"], GATE, reads=["GATE"], writes=["dbg"])
        if "HT2" in dbg_out:
            S.dma("qpool", dbg_out["HT2"].rearrange("(k p) t -> p k t", p=128), hT, reads=HT_ALL, writes=["dbg"])
        if stop == "P6":
            S.finish(["dbg"])
            print("counts", S.count, "waits", S.nwaits)
            return nc

        S.barrier()
        ARB = SB0 + 12 * 1024
        wbf2 = nc.alloc_sbuf_tensor_at(f"wbf2L{l}", [128, KT, 512], BF16, offset=ARB + KT * NTOK * 2).ap()
        GT = pers(f"GT{l}", [128, NT, 32])
        S.dma("qsp", GT, GATE.rearrange("(t p) e -> p t e", p=128), reads=["GATE"], writes=["GT"])
        aTt = [rt[0].bitcast(BF16), rt[1].bitcast(BF16)]
        for ex in range(32):
            S.dma("qsp", big[0], I["exp_w_gate"][l, ex].rearrange("(k p) c -> p k c", p=128), writes=["big0"])
            S.dma("qpool", big[1], I["exp_w_up"][l, ex].rearrange("(k p) c -> p k c", p=128), writes=["big1"])
            S.op("act", lambda e: e.activation(out=wbf[0], in_=big[0], func=AF.Copy), ["big0"], ["wbf0"])
            S.op("pool", lambda e: e.tensor_copy(out=wbf2, in_=big[1]), ["big1"], ["wbf2"])
            for t in range(T0, NT):
                pG = ps[2 + t % 2]; pGk = f"ps{2 + t % 2}"; pU = ps[4 + t % 2]; pUk = f"ps{4 + t % 2}"
                for kk in range(KT):
                    S.op("pe", lambda e, pG=pG, kk=kk, t=t: e.matmul(pG, lhsT=hT[:, kk, t * 128:(t + 1) * 128], rhs=wbf[0][:, kk, :],
                                                                     start=(kk == 0), stop=(kk == KT - 1)), [("hT", t), "wbf0"], [pGk])
                for kk in range(KT):
                    S.op("pe", lambda e, pU=pU, kk=kk, t=t: e.matmul(pU, lhsT=hT[:, kk, t * 128:(t + 1) * 128], rhs=wbf2[:, kk, :],
                                                                     start=(kk == 0), stop=(kk == KT - 1)), [("hT", t), "wbf2"], [pUk])
                o_ = po[t % 2]; ok = f"po{t % 2}"
                S.op("act", lambda e, o_=o_, pG=pG: e.activation(out=o_, in_=pG, func=AF.Silu), [pGk], [ok])
                S.op("dve", lambda e, o_=o_, pU=pU, t=t, ex=ex: e.scalar_tensor_tensor(out=o_, in0=o_, scalar=GT[:, t, ex:ex + 1], in1=pU,
                                                                                       op0=ALU.mult, op1=ALU.mult), [ok, pUk, "GT"], [ok])
                for j in range(4):
                    S.op("pe", lambda e, o_=o_, j=j: e.transpose(ps[6][:, j * 128:(j + 1) * 128], o_[:, j * 128:(j + 1) * 128], ident), [ok, "ident"], ["ps6"])
                a_ = aTt[t % 2][:, 0:512]; ak = f"rt{t % 2}"
                S.op("act", lambda e, a_=a_: e.activation(out=a_, in_=ps[6], func=AF.Copy), ["ps6"], [ak])
                S.dma("qsp" if t % 2 == 0 else "qpool", ATd[ex, :, :, t * 128:(t + 1) * 128], a_.rearrange("p (k c) -> p k c", k=4), reads=[ak], writes=["ATd"])
        S.barrier()
        acc = nc.alloc_sbuf_tensor_at(f"accL{l}", [128, NT, 512], F32, offset=ARB).ap()
        ATe = [nc.alloc_sbuf_tensor_at(f"ATe{i}L{l}", [128, 4, NTOK], BF16, offset=ARB + NT * 512 * 4 + i * 4 * NTOK * 2).ap() for i in range(1)]
        for tt in range(2):
            S.dma("qsp", xt[tt], MOD[l, tt, 5 * D:6 * D].partition_broadcast(128), reads=["MOD"], writes=[f"xt{tt}"])
        for dc in range(4):
            S.op("dve", lambda e: e.memset(acc, 0.0), [], [("acc", t) for t in range(NT)])
            for ex in range(32):
                S.dma("qsp", big[0][:, 0:4, :], I["exp_w_down"][l, ex, :, dc * 512:(dc + 1) * 512].rearrange("(k p) c -> p k c", p=128), writes=["big0"])
                S.op("act", lambda e: e.activation(out=wbf[0][:, 0:4, :], in_=big[0][:, 0:4, :], func=AF.Copy), ["big0"], ["wbf0"])
                S.dma("qpool", ATe[0], ATd[ex], reads=["ATd"], writes=["ATe"])
                for t in range(T0, NT):
                    p_ = ps[2 + t % 2]; pk = f"ps{2 + t % 2}"
                    for kk in range(4):
                        S.op("pe", lambda e, p_=p_, kk=kk, t=t: e.matmul(p_, lhsT=ATe[0][:, kk, t * 128:(t + 1) * 128], rhs=wbf[0][:, kk, :],
                                                                         start=(kk == 0), stop=(kk == 3)), ["ATe", "wbf0"], [pk])
                    S.op("dve", lambda e, p_=p_, t=t: e.tensor_tensor(out=acc[:, t, :], in0=p_, in1=acc[:, t, :], op=ALU.add), [pk, ("acc", t)], [("acc", t)])
            for t in range(T0, NT):
                tt = 0 if t < 2 else 1
                o_ = po[t % 2]; ok = f"po{t % 2}"
                S.dma("qpool", o_, XR[t * 128:(t + 1) * 128, dc * 512:(dc + 1) * 512], reads=["XR"], writes=[ok])
                S.op("dve", lambda e, t=t, tt=tt, dc=dc: e.tensor_tensor(out=acc[:, t, :], in0=acc[:, t, :], in1=xt[tt][:, dc * 512:(dc + 1) * 512], op=ALU.mult),
                     [("acc", t), f"xt{tt}"], [("acc", t)])
                S.op("pool", lambda e, o_=o_, t=t: e.tensor_tensor(out=o_, in0=o_, in1=acc[:, t, :], op=ALU.add), [ok, ("acc", t)], [ok])
                S.dma("qsp", XR[t * 128:(t + 1) * 128, dc * 512:(dc + 1) * 512], o_, reads=[ok], writes=["XR"])
        if l == 0 and "X2" in dbg_out:
            S.dma("qsp", dbg_out["X2"], XR, reads=["XR"], writes=["dbg"])
        if stop == "P7":
            S.finish(["dbg"])
            return nc

    S.barrier()
    S.dma("qsp", junk, I["final_g"].partition_broadcast(128), writes=["junk"])
    for t in range(2, NT):
        x_ = xt[t % 2]; xk = f"xt{t % 2}"
        S.dma("qsp" if t % 2 == 0 else "qpool", x_, XR[t * 128:(t + 1) * 128, :], reads=["XR"], writes=[xk])
        ss = small[:, 0:1]
        S.op("act", lambda e, x_=x_: e.activation(out=big[1][:, 0:4, :].rearrange("p a b -> p (a b)"), in_=x_, func=AF.Square, accum_out=ss), [xk], ["sqj", "ss"])
        S.op("act", lambda e: e.activation(out=small[:, 1:2], in_=ss, func=AF.Sqrt, scale=1.0 / D, bias=eps_t[:, 0:1]), ["ss"], ["rs"])
        S.op("dve", lambda e: e.reciprocal(out=small[:, 2:3], in_=small[:, 1:2]), ["rs"], ["rstd"])
        S.op("dve", lambda e, x_=x_: e.scalar_tensor_tensor(out=x_, in0=x_, scalar=small[:, 2:3], in1=junk, op0=ALU.mult, op1=ALU.mult), [xk, "rstd", "junk"], [xk])
        S.dma("qsp", out_final[(t - 2) * 128:(t - 1) * 128, :], x_, reads=[xk], writes=["OUT"])
    S.finish(["OUT"])
    return nc


def rope_tables():
    rows = NLAT // 64
    row = np.repeat(np.arange(rows, dtype=np.float32), 64)
    col = np.tile(np.arange(64, dtype=np.float32), rows)
    n_freq = 32
    inv = (np.float32(10000.0) ** (-np.arange(n_freq, dtype=np.float32) / n_freq)).astype(np.float32)
    ang = np.concatenate([row[:, None] * inv, col[:, None] * inv], axis=-1).astype(np.float32)
    cos = np.cos(ang).astype(np.float32)
    sin = np.sin(ang).astype(np.float32)
    return np.tile(cos, (1, 4)), np.tile(sin, (1, 4))


_i = np.arange(128, dtype=np.float32)
POS_TAB = np.ascontiguousarray(np.stack([
    np.stack([_i + 1, -(_i + 1), 127 - _i, np.full(128, 128.0, np.float32)], -1),
    np.stack([128 - _i, -(128 - _i), _i, np.full(128, 128.0, np.float32)], -1)], 1).astype(np.float32))
MASK_F = (np.arange(128)[None, :] >= np.arange(128)[:, None]).astype(np.float32)
MASK_B = (np.arange(128)[:, None] > np.arange(128)[None, :]).astype(np.float32)


def s5_layouts(inputs):
    out = {}
    def rg(a):
        L_ = a.shape[0]
        return np.ascontiguousarray(a.reshape(L_, 2, 32, 2, 64).transpose(0, 1, 3, 4, 2).reshape(L_, 2, 128, 32))
    out["s5_ar"] = rg(inputs["s5_a_re"]); out["s5_ai"] = rg(inputs["s5_a_im"])
    ls = inputs["s5_log_step"]
    out["s5_ls"] = rg(np.broadcast_to(ls[..., None], ls.shape + (64,)))
    def bz(b):
        L_ = b.shape[0]
        bb = b.reshape(L_, 2, 32, 2, 64, 16)
        z = np.zeros((L_, 2, 2, 64, 32, 2, 16), np.float32)
        for gl in range(2):
            z[:, :, gl, :, :, gl, :] = bb[:, :, :, gl].transpose(0, 1, 3, 2, 4)
        return np.ascontiguousarray(z.reshape(L_, 2, 128, 32, 32))
    out["s5_bzr"] = bz(inputs["s5_b_re"]); out["s5_bzi"] = bz(inputs["s5_b_im"])
    out["s5_czr"] = bz(inputs["s5_c_re"].transpose(0, 1, 2, 4, 3)); out["s5_czi"] = bz(inputs["s5_c_im"].transpose(0, 1, 2, 4, 3))
    out["s5_d"] = np.ascontiguousarray(inputs["s5_d"].reshape(DEPTH, 1024))
    return out


def make_in_maps(inputs, nb=4):
    cos4, sin4 = rope_tables()
    s5l = s5_layouts(inputs)
    rwc = np.concatenate([inputs["router_grp_w"], inputs["router_exp_w"]], -1)
    RW_ = np.ascontiguousarray(rwc.reshape(DEPTH, KT, 128, 36).transpose(0, 2, 1, 3))
    RB_ = np.ascontiguousarray(np.concatenate([inputs["router_grp_b"], inputs["router_exp_b"]], -1))
    maps = []
    for b in range(nb):
        cc = np.stack([inputs["c_ctx"], inputs["c"][b]], 0)
        ccT = np.ascontiguousarray(cc.reshape(2, KT, 128).transpose(2, 1, 0))
        m = {
            "xin": np.ascontiguousarray(inputs["x"][b]),
            "ctxin": np.ascontiguousarray(inputs["ctx"][b]),
            "ccT": ccT,
            "ada_w": inputs["ada_w"], "ada_b": inputs["ada_b"],
            "norm1_g": inputs["norm1_g"], "w_in": inputs["w_in"],
            "ident": np.eye(128, dtype=np.float32),
            "rope_cos": cos4, "rope_sin": sin4,
            "ret_decay": np.ascontiguousarray(inputs["ret_decay"].reshape(DEPTH, 16)),
            "pos": POS_TAB, "maskF": MASK_F, "maskB": MASK_B,
            **s5l,
            "s5_glu_w": inputs["s5_glu_w"], "s5_glu_b": inputs["s5_glu_b"], "w_out": inputs["w_out"],
            "norm2_g": inputs["norm2_g"], "final_g": inputs["final_g"],
            "rw": RW_, "rb": RB_,
            "exp_w_gate": inputs["exp_w_gate"], "exp_w_up": inputs["exp_w_up"], "exp_w_down": inputs["exp_w_down"],
        }
        maps.append(m)
    return maps


def kernel(**inputs):
    inputs = {k_: np.asarray(v) for k_, v in inputs.items()}
    nc = build()
    maps = make_in_maps(inputs)
    res = run_bass_kernel_spmd(nc, maps, core_ids=list(range(4)))
    return np.stack([r["out"] for r in res.results], 0)
```

```python
import numpy as np
import concourse.bass as bass
import concourse.mybir as mybir
from concourse.bass_utils import run_bass_kernel_spmd

F32 = mybir.dt.float32
BF16 = mybir.dt.bfloat16
AF = mybir.ActivationFunctionType
ALU = mybir.AluOpType
AX = mybir.AxisListType

D = 2048
NCTX = 256
NLAT = 2048
NTOK = NCTX + NLAT
NT = NTOK // 128
KT = D // 128
INW = 5120
DEPTH = 2
import os
RLEVEL = int(os.environ.get("RLEVEL", "9"))
RFLAGS = os.environ.get("RFLAGS", "WBE")
EPOCH = 30000
NDMASEM = 8


class Sched:
    def __init__(self, nc, same_engine_sync=("dve", "act", "pool")):
        self.nc = nc
        self.E = {"pe": nc.tensor, "dve": nc.vector, "act": nc.scalar,
                  "pool": nc.gpsimd, "sp": nc.sync}
        self.same = set(same_engine_sync)
        self.dmaq = {"qsp": "sp", "qpool": "pool", "qact": "act"}
        self.sems = {}
        self.count = {}
        self.waited = {}
        self.res = {}
        self.nwaits = 0

    def _sem(self, stream, idx):
        if stream in self.dmaq:
            key = (stream, (idx - 1) % NDMASEM)
            val = 16 * ((idx - 1) // NDMASEM + 1)
        else:
            key = (stream, (idx - 1) // EPOCH)
            val = (idx - 1) % EPOCH + 1
        if key not in self.sems:
            self.sems[key] = self.nc.alloc_semaphore(f"s_{key[0]}_{key[1]}")
        return self.sems[key], val

    def _is_waited(self, eng, stream, idx):
        w = self.waited.setdefault(eng, {})
        if stream in self.dmaq:
            return idx in w.get(stream, ())
        return w.get(stream, 0) >= idx

    def _wait(self, eng, stream, idx):
        if self._is_waited(eng, stream, idx):
            return
        if stream == eng and eng not in self.same:
            return
        h, v = self._sem(stream, idx)
        self.E[eng].wait_ge(h, v)
        self.nwaits += 1
        w = self.waited[eng]
        if stream in self.dmaq:
            w.setdefault(stream, set()).add(idx)
        else:
            w[stream] = idx

    def _deps(self, eng, reads, writes):
        need = set()
        for k in reads:
            st = self.res.get(k)
            if st and st[0]:
                need.add(st[0])
        for k in writes:
            st = self.res.get(k)
            if st:
                if st[0]:
                    need.add(st[0])
                need.update(st[1])
        mx = {}
        for s, i in need:
            if s in self.dmaq:
                self._wait(eng, s, i)
            else:
                mx[s] = max(mx.get(s, 0), i)
        for s, i in mx.items():
            self._wait(eng, s, i)

    def _record(self, stream, idx, reads, writes):
        for k in reads:
            st = self.res.setdefault(k, [None, []])
            if stream not in self.dmaq:
                st[1] = [r for r in st[1] if r[0] != stream]
            st[1].append((stream, idx))
        for k in writes:
            self.res[k] = [(stream, idx), []]

    def op(self, eng, fn, reads=(), writes=()):
        self._deps(eng, reads, writes)
        idx = self.count.get(eng, 0) + 1
        self.count[eng] = idx
        h, v = self._sem(eng, idx)
        fn(self.E[eng]).then_inc(h, 1)
        self._record(eng, idx, reads, writes)
        return idx

    def dma(self, q, out, in_, reads=(), writes=(), **kw):
        eng = self.dmaq[q]
        self._deps(eng, reads, writes)
        idx = self.count.get(q, 0) + 1
        self.count[q] = idx
        if idx > NDMASEM:
            self._wait(eng, q, idx - NDMASEM)
        h, v = self._sem(q, idx)
        self.E[eng].dma_start(out=out, in_=in_, **kw).then_inc(h, 16)
        self._record(q, idx, reads, writes)
        return idx

    def finish(self, keys, eng="sp"):
        self._deps(eng, list(keys), [])

    def barrier(self):
        for eng in self.E:
            for s_, n in list(self.count.items()):
                if n == 0:
                    continue
                if s_ in self.dmaq:
                    for i in range(max(1, n - NDMASEM + 1), n + 1):
                        self._wait(eng, s_, i)
                elif s_ != eng or eng in self.same:
                    self._wait(eng, s_, n)
        self.res = {}


class Arena:
    def __init__(self, nc, base, limit):
        self.nc, self.off, self.limit = nc, base, limit
        self.n = 0

    def __call__(self, name, shape, dt=F32):
        esz = 2 if dt == BF16 else 4
        size = esz
        for d_ in shape[1:]:
            size *= d_
        off = (self.off + 31) // 32 * 32
        self.off = off + size
        assert self.off <= self.limit, (name, self.off, self.limit)
        return self.nc.alloc_sbuf_tensor_at(name, list(shape), dt, offset=off).ap()


class K:
    pass


def build(stop=None, dbg=()):
    nc = bass.Bass("TRN2", target_bir_lowering=False)
    S = Sched(nc, same_engine_sync=tuple(x for x in os.environ.get("SAMESYNC", "dve,act,pool").split(",") if x))
    k = K()
    k.nc, k.S = nc, S

    def din(name, shape, dt=F32):
        return nc.dram_tensor(name, list(shape), dt, kind="ExternalInput").ap()

    def dscr(name, shape, dt=F32):
        return nc.dram_tensor(name, list(shape), dt, kind="Internal").ap()

    def dout(name, shape, dt=F32):
        return nc.dram_tensor(name, list(shape), dt, kind="ExternalOutput").ap()

    def sb(name, shape, dt=F32):
        return nc.alloc_sbuf_tensor(name, list(shape), dt).ap()

    I = {}
    I["xin"] = din("xin", [NLAT, D])
    I["ctxin"] = din("ctxin", [NCTX, D])
    I["ccT"] = din("ccT", [128, KT, 2])
    I["ada_w"] = din("ada_w", [DEPTH, D, 6 * D])
    I["ada_b"] = din("ada_b", [DEPTH, 6 * D])
    I["norm1_g"] = din("norm1_g", [DEPTH, D])
    I["w_in"] = din("w_in", [DEPTH, D, INW])
    I["ident"] = din("ident", [128, 128])
    I["rope_cos"] = din("rope_cos", [NLAT, 256])
    I["rope_sin"] = din("rope_sin", [NLAT, 256])
    I["ret_decay"] = din("ret_decay", [DEPTH, 16])
    I["pos"] = din("pos", [128, 2, 4])
    I["maskF"] = din("maskF", [128, 128])
    I["maskB"] = din("maskB", [128, 128])
    for nm in ("s5_ar", "s5_ai", "s5_ls"):
        I[nm] = din(nm, [DEPTH, 2, 128, 32])
    for nm in ("s5_bzr", "s5_bzi", "s5_czr", "s5_czi"):
        I[nm] = din(nm, [DEPTH, 2, 128, 32, 32])
    I["s5_d"] = din("s5_d", [DEPTH, 1024])
    I["s5_glu_w"] = din("s5_glu_w", [DEPTH, 1024, 1024])
    I["s5_glu_b"] = din("s5_glu_b", [DEPTH, 1024])
    I["w_out"] = din("w_out", [DEPTH, D, D])
    I["norm2_g"] = din("norm2_g", [DEPTH, D])
    I["final_g"] = din("final_g", [D])
    I["rw"] = din("rw", [DEPTH, 128, KT, 36])
    I["rb"] = din("rb", [DEPTH, 36])
    I["exp_w_gate"] = din("exp_w_gate", [DEPTH, 32, D, 512])
    I["exp_w_up"] = din("exp_w_up", [DEPTH, 32, D, 512])
    I["exp_w_down"] = din("exp_w_down", [DEPTH, 32, 512, D])
    out_final = dout("out", [NLAT, D])

    XR = dscr("XR", [NTOK, D])
    MOD = dscr("MOD", [DEPTH, 2, 6 * D])
    PROJ = dscr("PROJ", [NTOK, INW])
    MIX = dscr("MIX", [NTOK, D])
    Y5 = dscr("Y5", [NTOK, 1024])
    Z5 = dscr("Z5", [NTOK, 1024])
    GATE = dscr("GATE", [NTOK, 32])
    ATd = dscr("ATd", [32, 128, 4, NTOK], BF16)
    dbg_out = {}
    for name, shape in dbg:
        dbg_out[name] = dout("dbg_" + name, shape)

    SLAB = 204 * 1024
    slab = nc.alloc_sbuf_tensor("slab", [128, SLAB // 4], F32)
    SB0 = nc.lookup_mloc(slab).addr
    SB_LIMIT = SB0 + SLAB
    pers = Arena(nc, SB0, SB0 + 12 * 1024)
    ident = pers("ident_sb", [128, 128])
    small = pers("small", [128, 64])
    eps_t = pers("eps_t", [128, 2])
    S.op("dve", lambda e: e.memset(eps_t[:, 0:1], 1e-6), [], ["eps"])
    S.op("dve", lambda e: e.memset(eps_t[:, 1:2], 1e-5), [], ["eps"])
    S.dma("qsp", ident, I["ident"], writes=["ident"])

    ps = [nc.alloc_psum_tensor(f"ps{i}", [128, 512], F32).ap() for i in range(8)]

    S.dma("qsp", XR[0:NCTX, :], I["ctxin"], writes=["XR"])
    S.dma("qpool", XR[NCTX:NTOK, :], I["xin"], writes=["XR"])

    ar = Arena(nc, SB0 + 12 * 1024, SB_LIMIT)
    hT = ar("hT", [128, KT, NTOK], BF16)
    xt = [ar(f"xt{i}", [128, D]) for i in range(2)]
    big = [ar(f"big{i}", [128, KT, 512]) for i in range(2)]
    wbf = [ar(f"wbf{i}", [128, KT, 512], BF16) for i in range(1)]
    wbf.append(wbf[0])
    junk = ar("junk", [128, D])
    bc = [big[0][:, 4 * j:4 * j + 4, :].rearrange("p a b -> p (a b)") for j in range(4)]
    sb = ar

    ccT = sb("ccT_sb", [128, KT, 2])
    sT = sb("sT", [128, KT, 2])
    S.dma("qsp", ccT, I["ccT"], writes=["ccT"])
    S.op("act", lambda e: e.activation(out=sT, in_=ccT, func=AF.Silu), ["ccT"], ["sT"])
    ab = sb("ab", [2, 512])
    mo = sb("mo", [2, 512])
    for l in range(DEPTH):
        for cb in range(24):
            W = big[cb % 2]
            wk = f"big{cb % 2}"
            S.dma("qsp" if cb % 2 == 0 else "qpool", W,
                  I["ada_w"][l, :, cb * 512:(cb + 1) * 512].rearrange("(k p) c -> p k c", p=128),
                  writes=[wk])
            S.dma("qsp", ab, I["ada_b"][l, cb * 512:(cb + 1) * 512].partition_broadcast(2), writes=["ab"])
            for kk in range(KT):
                S.op("pe", lambda e, kk=kk, W=W: e.matmul(ps[0][0:2, :], lhsT=sT[:, kk, :], rhs=W[:, kk, :],
                                                           start=(kk == 0), stop=(kk == KT - 1)),
                     ["sT", wk], ["ps0"])
            S.op("dve", lambda e: e.tensor_tensor(out=mo, in0=ps[0][0:2, :], in1=ab, op=ALU.add),
                 ["ps0", "ab"], ["mo"])
            S.dma("qsp", MOD[l, :, cb * 512:(cb + 1) * 512], mo, reads=["mo"], writes=["MOD"])
    if "MOD" in dbg_out:
        S.dma("qsp", dbg_out["MOD"], MOD, reads=["MOD"], writes=["dbg"])
    if stop == "A":
        S.finish(["dbg"])
        return nc

    def load_bc(l, j, tt, which, qn="qsp"):
        S.dma(qn, bc[j], MOD[l, tt, which * D:(which + 1) * D].partition_broadcast(128),
              reads=["MOD"], writes=[f"bc{j}"])

    small2 = pers("small2", [128, 160])
    rbt = pers("rbt", [128, 36])

    def router_tile(l, t, pr):
        L = small2[:, 0:36]; M = small2[:, 40:72]; m8 = small2[:, 72:80]; G1 = small2[:, 80:112]; G2 = small2[:, 112:144]
        sc = small2[:, 144:160]
        k_ = "rt_"
        S.op("dve", lambda e: e.tensor_tensor(out=L, in0=pr, in1=rbt, op=ALU.add), ["ps7", "rbt"], [k_ + "L"])
        S.op("dve", lambda e: e.tensor_reduce(out=sc[:, 0:1], in_=L[:, 0:4], axis=AX.X, op=ALU.max), [k_ + "L"], [k_ + "gmax"])
        S.op("dve", lambda e: e.tensor_scalar(out=sc[:, 1:2], in0=sc[:, 0:1], scalar1=-1.0, scalar2=None, op0=ALU.mult), [k_ + "gmax"], [k_ + "ngmax"])
        S.op("act", lambda e: e.activation(out=sc[:, 4:8], in_=L[:, 0:4], func=AF.Exp, bias=sc[:, 1:2], accum_out=sc[:, 2:3]),
             [k_ + "L", k_ + "ngmax"], [k_ + "gsum", k_ + "e4"])
        S.op("dve", lambda e: e.reciprocal(out=sc[:, 3:4], in_=sc[:, 2:3]), [k_ + "gsum"], [k_ + "ggate"])
        S.op("dve", lambda e: e.tensor_scalar(out=sc[:, 8:12], in0=L[:, 0:4], scalar1=sc[:, 0:1], scalar2=None, op0=ALU.is_equal), [k_ + "L", k_ + "gmax"], [k_ + "oh"])
        S.op("dve", lambda e: e.tensor_scalar(out=sc[:, 8:12], in0=sc[:, 8:12], scalar1=-1.0, scalar2=1e30, op0=ALU.add, op1=ALU.mult), [k_ + "oh"], [k_ + "oh"])
        for g in range(4):
            S.op("dve", lambda e, g=g: e.tensor_scalar(out=M[:, g * 8:(g + 1) * 8], in0=L[:, 4 + g * 8:12 + g * 8], scalar1=sc[:, 8 + g:9 + g], scalar2=None, op0=ALU.add),
                 [k_ + "L", k_ + "oh"], [k_ + f"M{g}"])
        MALL = [k_ + f"M{g}" for g in range(4)]
        if RLEVEL < 3:
            return
        S.op("dve", lambda e: e.max(out=m8, in_=M), MALL, [k_ + "m8"])
        if RLEVEL < 4:
            return
        S.op("dve", lambda e: e.tensor_tensor(out=sc[:, 12:13], in0=m8[:, 1:2], in1=m8[:, 0:1], op=ALU.subtract), [k_ + "m8"], [k_ + "diff"])
        S.op("act", lambda e: e.activation(out=sc[:, 13:14], in_=sc[:, 12:13], func=AF.Exp), [k_ + "diff"], [k_ + "ed"])
        S.op("dve", lambda e: e.tensor_scalar(out=sc[:, 14:15], in0=sc[:, 13:14], scalar1=1.0, scalar2=None, op0=ALU.add), [k_ + "ed"], [k_ + "w1"])
        S.op("dve", lambda e: e.reciprocal(out=sc[:, 14:15], in_=sc[:, 14:15]), [k_ + "w1"], [k_ + "w1"])
        S.op("dve", lambda e: e.tensor_tensor(out=sc[:, 15:16], in0=sc[:, 13:14], in1=sc[:, 14:15], op=ALU.mult), [k_ + "ed", k_ + "w1"], [k_ + "w2"])
        S.op("dve", lambda e: e.tensor_tensor(out=sc[:, 14:15], in0=sc[:, 14:15], in1=sc[:, 3:4], op=ALU.mult), [k_ + "w1", k_ + "ggate"], [k_ + "w1"])
        S.op("dve", lambda e: e.tensor_tensor(out=sc[:, 15:16], in0=sc[:, 15:16], in1=sc[:, 3:4], op=ALU.mult), [k_ + "w2", k_ + "ggate"], [k_ + "w2"])
        S.op("dve", lambda e: e.tensor_scalar(out=G1, in0=M, scalar1=m8[:, 0:1], scalar2=sc[:, 14:15], op0=ALU.is_equal, op1=ALU.mult), MALL + [k_ + "m8", k_ + "w1"], [k_ + "G1"])
        S.op("dve", lambda e: e.tensor_scalar(out=G2, in0=M, scalar1=m8[:, 1:2], scalar2=sc[:, 15:16], op0=ALU.is_equal, op1=ALU.mult), MALL + [k_ + "m8", k_ + "w2"], [k_ + "G2"])
        S.op("dve", lambda e: e.tensor_tensor(out=G1, in0=G1, in1=G2, op=ALU.add), [k_ + "G1", k_ + "G2"], [k_ + "G1"])
        S.dma("qsp", GATE[t * 128:(t + 1) * 128, :], G1, reads=[k_ + "G1"], writes=["GATE"])

    def norm_mod_T(l, gname, sh_idx, sc_idx, router=False, t_start=0):
        S.dma("qsp", junk, I[gname][l].partition_broadcast(128), writes=["junk"])
        for tt in range(2):
            load_bc(l, 2 * tt, tt, sc_idx)
            load_bc(l, 2 * tt + 1, tt, sh_idx, "qpool")
            S.op("dve", lambda e, tt=tt: e.scalar_tensor_tensor(out=bc[2 * tt], in0=bc[2 * tt], scalar=1.0,
                                                                in1=junk, op0=ALU.add, op1=ALU.mult),
                 [f"bc{2 * tt}", "junk"], [f"bc{2 * tt}"])
        if router:
            h32 = big[1][:, :, 0:128]
            RW = big[1][:, :, 128:164]
            if "W" in RFLAGS:
                S.dma("qsp", RW, I["rw"][l], writes=["RW"])
            if "B" in RFLAGS:
                S.dma("qsp", rbt, I["rb"][l].partition_broadcast(128), writes=["rbt"])
        for t in range(t_start, NT):
            tt = 0 if t < 2 else 1
            x_ = xt[t % 2]
            xk = f"xt{t % 2}"
            S.dma("qsp" if t % 2 == 0 else "qpool", x_, XR[t * 128:(t + 1) * 128, :], reads=["XR"], writes=[xk])
            ss = small[:, 0:1]
            S.op("act", lambda e, x_=x_: e.activation(out=junk, in_=x_, func=AF.Square, accum_out=ss),
                 [xk], ["junk", "ss"])
            S.op("act", lambda e: e.activation(out=small[:, 1:2], in_=ss, func=AF.Sqrt, scale=1.0 / D, bias=eps_t[:, 0:1]),
                 ["ss"], ["rs"])
            S.op("dve", lambda e: e.reciprocal(out=small[:, 2:3], in_=small[:, 1:2]), ["rs"], ["rstd"])
            S.op("dve", lambda e, x_=x_, tt=tt: e.scalar_tensor_tensor(out=x_, in0=x_, scalar=small[:, 2:3],
                                                                       in1=bc[2 * tt], op0=ALU.mult, op1=ALU.mult),
                 [xk, "rstd", f"bc{2 * tt}"], [xk])
            S.op("pool", lambda e, x_=x_, tt=tt: e.tensor_tensor(out=x_, in0=x_, in1=bc[2 * tt + 1], op=ALU.add),
                 [xk, f"bc{2 * tt + 1}"], [xk])
            for kq in range(4):
                p_ = ps[1 + kq % 2]
                pk = f"ps{1 + kq % 2}"
                for j in range(4):
                    kk = kq * 4 + j
                    S.op("pe", lambda e, p_=p_, j=j, kk=kk, x_=x_: e.transpose(p_[:, j * 128:(j + 1) * 128],
                                                                               x_[:, kk * 128:(kk + 1) * 128], ident),
                         [xk, "ident"], [pk])
                S.op("act" if kq % 2 == 0 else "dve",
                     lambda e, p_=p_, kq=kq, t=t: e.activation(out=hT[:, kq * 4:(kq + 1) * 4, t * 128:(t + 1) * 128],
                                                               in_=p_.rearrange("p (j c) -> p j c", j=4), func=AF.Copy)
                     if kq % 2 == 0 else
                     e.tensor_copy(out=hT[:, kq * 4:(kq + 1) * 4, t * 128:(t + 1) * 128],
                                   in_=p_.rearrange("p (j c) -> p j c", j=4)),
                     [pk], [("hT", t)])
                if router and "E" in RFLAGS:
                    S.op("act" if kq % 2 == 0 else "dve",
                         (lambda e, p_=p_, kq=kq: e.tensor_copy(out=h32[:, kq * 4:(kq + 1) * 4, :], in_=p_.rearrange("p (j c) -> p j c", j=4)))
                         if kq % 2 == 1 else
                         (lambda e, p_=p_, kq=kq: e.activation(out=h32[:, kq * 4:(kq + 1) * 4, :], in_=p_.rearrange("p (j c) -> p j c", j=4), func=AF.Copy)),
                         [pk], [("h32", kq)])
            if router and RLEVEL >= 1:
                for kk in range(KT):
                    S.op("pe", lambda e, kk=kk: e.matmul(ps[7][:, 0:36], lhsT=h32[:, kk, :], rhs=RW[:, kk, :], start=(kk == 0), stop=(kk == KT - 1)),
                         [("h32", kk // 4), "RW"], ["ps7"])
                if RLEVEL >= 2:
                    router_tile(l, t, ps[7][:, 0:36])

    HT_ALL = [("hT", t) for t in range(NT)]

    for l in range(DEPTH):
        S.barrier()
        norm_mod_T(l, "norm1_g", 0, 1)
        S.barrier()
        if l == 0 and "hT" in dbg_out:
            S.dma("qsp", dbg_out["hT"].rearrange("(k p) t -> p k t", p=128), hT, reads=HT_ALL, writes=["dbg"])
        if stop == "P1":
            S.finish(["dbg"])
            return nc

        cs = sb("cs", [128, 2, 256]) if l == 0 else cs
        po = [sb(f"po{i}", [128, 512]) for i in range(2)] if l == 0 else po
        rt = [sb(f"rt{i}", [128, 512]) for i in range(2)] if l == 0 else rt
        n_evac = 0
        for cb in range(INW // 512):
            W = big[cb % 2]
            wk = f"big{cb % 2}"
            Wb = wbf[cb % 2]
            wbk = f"wbf{cb % 2}"
            S.dma("qsp" if cb % 2 == 0 else "qpool", W,
                  I["w_in"][l, :, cb * 512:(cb + 1) * 512].rearrange("(k p) c -> p k c", p=128), writes=[wk])
            S.op("act", lambda e, W=W, Wb=Wb: e.activation(out=Wb[:, 0:8, :], in_=W[:, 0:8, :], func=AF.Copy), [wk], [wbk])
            S.op("pool", lambda e, W=W, Wb=Wb: e.tensor_copy(out=Wb[:, 8:16, :], in_=W[:, 8:16, :]), [wk], [wbk])
            for t in range(NT):
                p_ = ps[3 + t % 2]
                pk = f"ps{3 + t % 2}"
                for kk in range(KT):
                    S.op("pe", lambda e, p_=p_, kk=kk, t=t, Wb=Wb: e.matmul(p_, lhsT=hT[:, kk, t * 128:(t + 1) * 128],
                                                                            rhs=Wb[:, kk, :], start=(kk == 0), stop=(kk == KT - 1)),
                         [("hT", t), wbk], [pk])
                o_ = po[n_evac % 2]
                ok = f"po{n_evac % 2}"
                n_evac += 1
                is_qk = cb < 4
                is_k = cb in (2, 3)
                if is_qk and t >= 2:
                    lt = t - 2
                    S.dma("qsp", cs[:, 0, :], I["rope_cos"][lt * 128:(lt + 1) * 128, :], writes=["cs0"])
                    S.dma("qpool", cs[:, 1, :], I["rope_sin"][lt * 128:(lt + 1) * 128, :], writes=["cs1"])
                    pv = p_.rearrange("p (m two) -> p m two", two=2)
                    ov = o_.rearrange("p (m two) -> p m two", two=2)
                    r0 = rt[0].rearrange("p (m two) -> p m two", two=2)
                    r1 = rt[1].rearrange("p (m two) -> p m two", two=2)
                    sc = (128 ** -0.5) if is_k else 1.0
                    S.op("dve", lambda e, pv=pv, r0=r0: e.tensor_tensor(out=r0[:, :, 0], in0=pv[:, :, 0], in1=cs[:, 0, :], op=ALU.mult),
                         [pk, "cs0"], ["rt0a"])
                    S.op("dve", lambda e, pv=pv, r0=r0: e.tensor_tensor(out=r0[:, :, 1], in0=pv[:, :, 1], in1=cs[:, 1, :], op=ALU.mult),
                         [pk, "cs1"], ["rt0b"])
                    S.op("dve", lambda e, pv=pv, r1=r1: e.tensor_tensor(out=r1[:, :, 0], in0=pv[:, :, 0], in1=cs[:, 1, :], op=ALU.mult),
                         [pk, "cs1"], ["rt1a"])
                    S.op("dve", lambda e, pv=pv, r1=r1: e.tensor_tensor(out=r1[:, :, 1], in0=pv[:, :, 1], in1=cs[:, 0, :], op=ALU.mult),
                         [pk, "cs0"], ["rt1b"])
                    S.op("pool", lambda e, ov=ov, r0=r0: e.tensor_tensor(out=ov[:, :, 0], in0=r0[:, :, 0], in1=r0[:, :, 1], op=ALU.subtract),
                         ["rt0a", "rt0b"], [ok])
                    S.op("pool", lambda e, ov=ov, r1=r1: e.tensor_tensor(out=ov[:, :, 1], in0=r1[:, :, 0], in1=r1[:, :, 1], op=ALU.add),
                         ["rt1a", "rt1b"], [ok])
                    if is_k:
                        S.op("act", lambda e, o_=o_, sc=sc: e.activation(out=o_, in_=o_, func=AF.Copy, scale=sc), [ok], [ok])
                elif is_k:
                    S.op("act", lambda e, o_=o_, p_=p_: e.activation(out=o_, in_=p_, func=AF.Copy, scale=128 ** -0.5), [pk], [ok])
                else:
                    S.op("act", lambda e, o_=o_, p_=p_: e.activation(out=o_, in_=p_, func=AF.Copy), [pk], [ok])
                S.dma("qsp" if n_evac % 2 else "qpool", PROJ[t * 128:(t + 1) * 128, cb * 512:(cb + 1) * 512], o_,
                      reads=[ok], writes=["PROJ"])
        if l == 0 and "PROJ" in dbg_out:
            S.dma("qsp", dbg_out["PROJ"], PROJ, reads=["PROJ"], writes=["dbg"])
        if stop == "P2":
            S.finish(["dbg"])
            return nc

        S.barrier()
        a3 = Arena(nc, SB0 + 12 * 1024, SB_LIMIT)
        tg = f"L{l}"
        qf = a3("qf" + tg, [128, NT, 128]); kf = a3("kf" + tg, [128, NT, 128])
        vf = a3("vf" + tg, [128, NT, 128]); gf = a3("gf" + tg, [128, NT, 128])
        qs = a3("qs" + tg, [128, NT, 128]); ks = a3("ks" + tg, [128, NT, 128])
        qsT = a3("qsT" + tg, [128, NT * 128], BF16); ksT = a3("ksT" + tg, [128, NT * 128], BF16)
        kst = a3("kst" + tg, [128, NT, 128], BF16); vb = a3("vb" + tg, [128, NT, 128], BF16)
        RET = a3("RET" + tg, [128, NT, 128]); sq = a3("sq" + tg, [128, NT, 128])
        mF = a3("mF" + tg, [128, 128]); mB = a3("mB" + tg, [128, 128])
        scb = [a3(f"scb{i}" + tg, [128, 128], BF16) for i in range(2)]
        Sf = a3("Sf" + tg, [128, 128]); Sb = a3("Sb" + tg, [128, 128], BF16)
        DEC = a3("DEC" + tg, [128, 2, 8, 4]); lg = a3("lg" + tg, [128, 16]); POS = a3("POS" + tg, [128, 2, 4])
        stat = a3("stat" + tg, [128, 4, NT])
        S.dma("qsp", lg, I["ret_decay"][l].partition_broadcast(128), writes=["lg"])
        S.dma("qsp", POS, I["pos"], writes=["POS"])
        S.dma("qpool", mF, I["maskF"], writes=["mF"])
        S.dma("qpool", mB, I["maskB"], writes=["mB"])
        S.op("act", lambda e: e.activation(out=lg, in_=lg, func=AF.Exp), ["lg"], ["lg"])
        S.op("dve", lambda e: e.tensor_scalar(out=lg, in0=lg, scalar1=-1.0, scalar2=None, op0=ALU.mult), ["lg"], ["lg"])
        for d_ in range(2):
            for h in range(8):
                S.op("act", lambda e, d_=d_, h=h: e.activation(out=DEC[:, d_, h, :], in_=POS[:, d_, :], func=AF.Exp,
                                                               scale=lg[:, d_ * 8 + h:d_ * 8 + h + 1]),
                     ["lg", "POS"], ["DEC"])
        order_f = list(range(NT))
        order_b = [1, 0] + list(range(NT - 1, 1, -1))
        for h in range(8):
            def hv(off):
                return PROJ[:, off + h * 128: off + (h + 1) * 128].rearrange("(t p) c -> p t c", p=128)
            S.dma("qsp", qf, hv(0), reads=["PROJ"], writes=["qf"])
            S.dma("qpool", kf, hv(1024), reads=["PROJ"], writes=["kf"])
            S.dma("qsp", vf, hv(2048), reads=["PROJ"], writes=["vf"])
            S.dma("qpool", gf, hv(3072), reads=["PROJ"], writes=["gf"])
            S.op("pool", lambda e: e.tensor_copy(out=vb, in_=vf), ["vf"], ["vb"])
            for d_ in range(2):
                mk, mkk = (mF, "mF") if d_ == 0 else (mB, "mB")
                S.op("dve", lambda e, d_=d_, h=h: e.tensor_scalar(out=qs, in0=qf, scalar1=DEC[:, d_, h, 0:1], scalar2=None, op0=ALU.mult),
                     ["qf", "DEC"], ["qs"])
                S.op("pool", lambda e, d_=d_, h=h: e.tensor_scalar(out=ks, in0=kf, scalar1=DEC[:, d_, h, 1:2], scalar2=None, op0=ALU.mult),
                     ["kf", "DEC"], ["ks"])
                S.op("act", lambda e, d_=d_, h=h: e.activation(out=kst, in_=kf, func=AF.Copy, scale=DEC[:, d_, h, 2:3]),
                     ["kf", "DEC"], ["kst"])
                for src, srck, dst, dstk, pb in ((qs, "qs", qsT, "qsT", 0), (ks, "ks", ksT, "ksT", 1)):
                    for t0 in range(0, NT, 4):
                        n4 = min(4, NT - t0)
                        for j in range(n4):
                            S.op("pe", lambda e, src=src, t0=t0, j=j, pb=pb: e.transpose(ps[pb][:, j * 128:(j + 1) * 128], src[:, t0 + j, :], ident),
                                 [srck, "ident"], [f"ps{pb}"])
                        if pb == 0:
                            S.op("act", lambda e, dst=dst, t0=t0, n4=n4, pb=pb: e.activation(out=dst[:, t0 * 128:(t0 + n4) * 128], in_=ps[pb][:, 0:n4 * 128], func=AF.Copy),
                                 [f"ps{pb}"], [dstk])
                        else:
                            S.op("dve", lambda e, dst=dst, t0=t0, n4=n4, pb=pb: e.tensor_copy(out=dst[:, t0 * 128:(t0 + n4) * 128], in_=ps[pb][:, 0:n4 * 128]),
                                 [f"ps{pb}"], [dstk])
                S.op("dve", lambda e: e.memset(Sf, 0.0), [], ["Sf"])
                S.op("pool", lambda e: e.memset(Sb, 0.0), [], ["Sb"])
                for ci, t in enumerate(order_f if d_ == 0 else order_b):
                    sl = slice(t * 128, (t + 1) * 128)
                    pS = ps[2 + ci % 2]; pSk = f"ps{2 + ci % 2}"
                    pO = ps[4 + ci % 2]; pOk = f"ps{4 + ci % 2}"
                    sc_ = scb[ci % 2]; sck = f"scb{ci % 2}"
                    S.op("pe", lambda e, pS=pS, sl=sl: e.matmul(pS[:, 0:128], lhsT=ksT[:, sl], rhs=qsT[:, sl], start=True, stop=True),
                         ["ksT", "qsT"], [pSk])
                    S.op("dve", lambda e, pS=pS, sc_=sc_, mk=mk: e.tensor_tensor(out=sc_, in0=pS[:, 0:128], in1=mk, op=ALU.mult),
                         [pSk, mkk], [sck])
                    S.op("pe", lambda e, pO=pO, sc_=sc_, t=t: e.matmul(pO[:, 0:128], lhsT=sc_, rhs=vb[:, t, :], start=True, stop=False),
                         [sck, "vb"], [pOk])
                    S.op("pe", lambda e, pO=pO, sl=sl: e.matmul(pO[:, 0:128], lhsT=qsT[:, sl], rhs=Sb, start=False, stop=True),
                         ["qsT", "Sb"], [pOk])
                    if d_ == 0:
                        S.op("act", lambda e, pO=pO, t=t: e.activation(out=RET[:, t, :], in_=pO[:, 0:128], func=AF.Copy),
                             [pOk], [("RET", t)])
                    else:
                        S.op("dve", lambda e, pO=pO, t=t: e.tensor_tensor(out=RET[:, t, :], in0=pO[:, 0:128], in1=RET[:, t, :], op=ALU.add),
                             [pOk, ("RET", t)], [("RET", t)])
                    S.op("pe", lambda e, t=t: e.matmul(ps[6][:, 0:128], lhsT=kst[:, t, :], rhs=vb[:, t, :], start=True, stop=True),
                         ["kst", "vb"], ["ps6"])
                    S.op("dve", lambda e, d_=d_, h=h: e.scalar_tensor_tensor(out=Sf, in0=Sf, scalar=DEC[:, d_, h, 3:4], in1=ps[6][:, 0:128],
                                                                             op0=ALU.mult, op1=ALU.add),
                         ["Sf", "ps6", "DEC"], ["Sf"])
                    S.op("act", lambda e: e.activation(out=Sb, in_=Sf, func=AF.Copy), ["Sf"], ["Sb"])
            RALL = [("RET", t) for t in range(NT)]
            S.op("dve", lambda e: e.tensor_reduce(out=stat[:, 0, :], in_=RET, axis=AX.X, op=ALU.add), RALL, ["st0"])
            S.op("pool", lambda e: e.tensor_tensor(out=sq, in0=RET, in1=RET, op=ALU.mult), RALL, ["sq"])
            S.op("dve", lambda e: e.tensor_reduce(out=stat[:, 1, :], in_=sq, axis=AX.X, op=ALU.add), ["sq"], ["st1"])
            S.op("dve", lambda e: e.tensor_scalar(out=stat[:, 0, :], in0=stat[:, 0, :], scalar1=1.0 / 128, scalar2=None, op0=ALU.mult), ["st0"], ["st0"])
            S.op("dve", lambda e: e.tensor_tensor(out=stat[:, 2, :], in0=stat[:, 0, :], in1=stat[:, 0, :], op=ALU.mult), ["st0"], ["st2"])
            S.op("dve", lambda e: e.scalar_tensor_tensor(out=stat[:, 1, :], in0=stat[:, 1, :], scalar=1.0 / 128, in1=stat[:, 2, :],
                                                         op0=ALU.mult, op1=ALU.subtract), ["st1", "st2"], ["st1"])
            S.op("act", lambda e: e.activation(out=stat[:, 1, :], in_=stat[:, 1, :], func=AF.Sqrt, bias=eps_t[:, 1:2]), ["st1", "eps"], ["st1"])
            S.op("dve", lambda e: e.reciprocal(out=stat[:, 3, :], in_=stat[:, 1, :]), ["st1"], ["st3"])
            S.op("act", lambda e: e.activation(out=gf, in_=gf, func=AF.Silu), ["gf"], ["gf"])
            for t in range(NT):
                S.op("dve", lambda e, t=t: e.tensor_scalar(out=RET[:, t, :], in0=RET[:, t, :], scalar1=stat[:, 0, t:t + 1],
                                                           scalar2=stat[:, 3, t:t + 1], op0=ALU.subtract, op1=ALU.mult),
                     [("RET", t), "st0", "st3"], [("RET", t)])
            S.op("pool", lambda e: e.tensor_tensor(out=RET, in0=RET, in1=gf, op=ALU.mult), RALL + ["gf"], RALL)
            S.dma("qsp", MIX[:, h * 128:(h + 1) * 128].rearrange("(t p) c -> p t c", p=128), RET, reads=RALL, writes=["MIX"])
        if l == 0 and "MIX" in dbg_out:
            S.dma("qsp", dbg_out["MIX"], MIX, reads=["MIX"], writes=["dbg"])
        if stop == "P3":
            S.finish(["dbg"])
            return nc

        S.barrier()
        a4 = Arena(nc, SB0 + 12 * 1024, SB_LIMIT)
        tg = f"s5L{l}"
        def A4(n, shp, dt=F32):
            return a4(n + tg, shp, dt)
        NJ = 4
        BbT = A4("BbT", [32, 32, NJ, 2, 128], BF16)
        BUX = A4("BUX", [128, 3, 32, 128])
        BUr = BUX[:, 0]; BUi = BUX[:, 1]
        ublk = A4("ublk", [128, 1024]); ysb = A4("ysb", [128, 1024]); dsk = A4("dsk", [128, 1024])
        uTe = A4("uTe", [32, 32, 128 + 3], BF16)
        Hbr = A4("Hbr", [128, 32, 128], BF16); Hbi = A4("Hbi", [128, 32, 128], BF16)
        Cbr = A4("Cbr", [128, 32, 32], BF16); Cbi = A4("Cbi", [128, 32, 32], BF16)
        LR4 = A4("LR4", [128, 2, 32, 4]); LI4 = A4("LI4", [128, 2, 32, 4])
        Hc = A4("Hc", [128, 3, 32, 4]); T1 = A4("T1", [128, 2, 32, 4]); T2 = A4("T2", [128, 2, 32, 4])
        pm = {n: A4(n, [128, 32]) for n in ("ar", "ai", "dt", "xr", "ang", "mag", "c", "s", "lr", "li", "nr", "den",
                                            "fr", "fi", "nfi", "t1", "t2", "t3", "nli", "l4r", "l4i", "nl4i")}
        hpi = A4("hpi", [128, 1])
        BZr = A4("BZr", [128, 32, 32]); BZi = A4("BZi", [128, 32, 32])
        Bbr = A4("Bbr", [128, 32, 32]); Bbi = A4("Bbi", [128, 32, 32]); Bt = A4("Bt", [128, 32, 32])
        CZr = A4("CZr", [128, 32, 32]); CZi = A4("CZi", [128, 32, 32])
        S.op("dve", lambda e: e.memset(hpi, float(np.pi / 2)), [], ["hpi"])
        S.dma("qsp", dsk, I["s5_d"][l].partition_broadcast(128), writes=["dsk"])

        def tt_(eng, out, a, b, op, rk, wk):
            S.op(eng, lambda e: e.tensor_tensor(out=out, in0=a, in1=b, op=op), rk, wk)

        for d_ in range(2):
            S.dma("qsp", pm["ar"], I["s5_ar"][l, d_], writes=["p.ar"])
            S.dma("qpool", pm["ai"], I["s5_ai"][l, d_], writes=["p.ai"])
            S.dma("qsp", pm["dt"], I["s5_ls"][l, d_], writes=["p.dt"])
            S.dma("qsp", BZr, I["s5_bzr"][l, d_], writes=["BZr"])
            S.dma("qpool", BZi, I["s5_bzi"][l, d_], writes=["BZi"])
            S.dma("qsp", CZr, I["s5_czr"][l, d_], writes=["CZr"])
            S.dma("qpool", CZi, I["s5_czi"][l, d_], writes=["CZi"])
            P = pm
            S.op("act", lambda e: e.activation(out=P["dt"], in_=P["dt"], func=AF.Exp), ["p.dt"], ["p.dt"])
            tt_("dve", P["xr"], P["ar"], P["dt"], ALU.mult, ["p.ar", "p.dt"], ["p.xr"])
            tt_("dve", P["ang"], P["ai"], P["dt"], ALU.mult, ["p.ai", "p.dt"], ["p.ang"])
            S.op("act", lambda e: e.activation(out=P["mag"], in_=P["xr"], func=AF.Exp), ["p.xr"], ["p.mag"])
            S.op("act", lambda e: e.activation(out=P["s"], in_=P["ang"], func=AF.Sin, scale=1.0 / 16), ["p.ang"], ["p.s"])
            S.op("act", lambda e: e.activation(out=P["c"], in_=P["ang"], func=AF.Sin, scale=-1.0 / 16, bias=hpi[:, 0:1]), ["p.ang", "hpi"], ["p.c"])
            for _ in range(4):
                tt_("dve", P["t1"], P["c"], P["c"], ALU.mult, ["p.c"], ["p.t1"])
                tt_("dve", P["t2"], P["s"], P["s"], ALU.mult, ["p.s"], ["p.t2"])
                tt_("dve", P["t3"], P["c"], P["s"], ALU.mult, ["p.c", "p.s"], ["p.t3"])
                tt_("dve", P["c"], P["t1"], P["t2"], ALU.subtract, ["p.t1", "p.t2"], ["p.c"])
                tt_("dve", P["s"], P["t3"], P["t3"], ALU.add, ["p.t3"], ["p.s"])
            tt_("dve", P["lr"], P["mag"], P["c"], ALU.mult, ["p.mag", "p.c"], ["p.lr"])
            tt_("dve", P["li"], P["mag"], P["s"], ALU.mult, ["p.mag", "p.s"], ["p.li"])
            S.op("dve", lambda e: e.tensor_scalar(out=P["nli"], in0=P["li"], scalar1=-1.0, scalar2=None, op0=ALU.mult), ["p.li"], ["p.nli"])
            S.op("dve", lambda e: e.tensor_scalar(out=P["nr"], in0=P["lr"], scalar1=-1.0, scalar2=None, op0=ALU.add), ["p.lr"], ["p.nr"])
            tt_("dve", P["t1"], P["lr"], P["lr"], ALU.mult, ["p.lr"], ["p.t1"])
            tt_("dve", P["t2"], P["li"], P["li"], ALU.mult, ["p.li"], ["p.t2"])
            tt_("dve", P["t3"], P["lr"], P["li"], ALU.mult, ["p.lr", "p.li"], ["p.t3"])
            tt_("dve", P["l4r"], P["t1"], P["t2"], ALU.subtract, ["p.t1", "p.t2"], ["p.l4r"])
            tt_("dve", P["l4i"], P["t3"], P["t3"], ALU.add, ["p.t3"], ["p.l4i"])
            tt_("dve", P["t1"], P["l4r"], P["l4r"], ALU.mult, ["p.l4r"], ["p.t1"])
            tt_("dve", P["t2"], P["l4i"], P["l4i"], ALU.mult, ["p.l4i"], ["p.t2"])
            tt_("dve", P["t3"], P["l4r"], P["l4i"], ALU.mult, ["p.l4r", "p.l4i"], ["p.t3"])
            tt_("dve", P["l4r"], P["t1"], P["t2"], ALU.subtract, ["p.t1", "p.t2"], ["p.l4r"])
            tt_("dve", P["l4i"], P["t3"], P["t3"], ALU.add, ["p.t3"], ["p.l4i"])
            S.op("dve", lambda e: e.tensor_scalar(out=P["nl4i"], in0=P["l4i"], scalar1=-1.0, scalar2=None, op0=ALU.mult), ["p.l4i"], ["p.nl4i"])
            for r_ in range(4):
                for hh in range(2):
                    S.op("dve", lambda e, hh=hh, r_=r_: e.tensor_copy(out=LR4[:, hh, :, r_], in_=P["l4r"]), ["p.l4r"], ["LR4"])
                S.op("dve", lambda e, r_=r_: e.tensor_copy(out=LI4[:, 0, :, r_], in_=P["nl4i"]), ["p.nl4i"], ["LI4"])
                S.op("dve", lambda e, r_=r_: e.tensor_copy(out=LI4[:, 1, :, r_], in_=P["l4i"]), ["p.l4i"], ["LI4"])
            tt_("dve", P["t1"], P["ar"], P["ar"], ALU.mult, ["p.ar"], ["p.t1"])
            tt_("dve", P["t2"], P["ai"], P["ai"], ALU.mult, ["p.ai"], ["p.t2"])
            tt_("dve", P["den"], P["t1"], P["t2"], ALU.add, ["p.t1", "p.t2"], ["p.den"])
            S.op("dve", lambda e: e.reciprocal(out=P["den"], in_=P["den"]), ["p.den"], ["p.den"])
            tt_("dve", P["t1"], P["nr"], P["ar"], ALU.mult, ["p.nr", "p.ar"], ["p.t1"])
            tt_("dve", P["t2"], P["li"], P["ai"], ALU.mult, ["p.li", "p.ai"], ["p.t2"])
            tt_("dve", P["t1"], P["t1"], P["t2"], ALU.add, ["p.t1", "p.t2"], ["p.t1"])
            tt_("dve", P["fr"], P["t1"], P["den"], ALU.mult, ["p.t1", "p.den"], ["p.fr"])
            tt_("dve", P["t1"], P["li"], P["ar"], ALU.mult, ["p.li", "p.ar"], ["p.t1"])
            tt_("dve", P["t2"], P["nr"], P["ai"], ALU.mult, ["p.nr", "p.ai"], ["p.t2"])
            tt_("dve", P["t1"], P["t1"], P["t2"], ALU.subtract, ["p.t1", "p.t2"], ["p.t1"])
            tt_("dve", P["fi"], P["t1"], P["den"], ALU.mult, ["p.t1", "p.den"], ["p.fi"])
            S.op("dve", lambda e: e.tensor_scalar(out=P["nfi"], in0=P["fi"], scalar1=-1.0, scalar2=None, op0=ALU.mult), ["p.fi"], ["p.nfi"])
            for gh in range(32):
                S.op("dve", lambda e, gh=gh: e.tensor_scalar(out=Bbr[:, gh, :], in0=BZr[:, gh, :], scalar1=P["fr"][:, gh:gh + 1], scalar2=None, op0=ALU.mult),
                     ["BZr", "p.fr"], [("Bbr", gh)])
                S.op("dve", lambda e, gh=gh: e.scalar_tensor_tensor(out=Bbr[:, gh, :], in0=BZi[:, gh, :], scalar=P["nfi"][:, gh:gh + 1], in1=Bbr[:, gh, :],
                                                                    op0=ALU.mult, op1=ALU.add), ["BZi", "p.nfi", ("Bbr", gh)], [("Bbr", gh)])
                S.op("dve", lambda e, gh=gh: e.tensor_scalar(out=Bbi[:, gh, :], in0=BZi[:, gh, :], scalar1=P["fr"][:, gh:gh + 1], scalar2=None, op0=ALU.mult),
                     ["BZi", "p.fr"], [("Bbi", gh)])
                S.op("dve", lambda e, gh=gh: e.scalar_tensor_tensor(out=Bbi[:, gh, :], in0=BZr[:, gh, :], scalar=P["fi"][:, gh:gh + 1], in1=Bbi[:, gh, :],
                                                                     op0=ALU.mult, op1=ALU.add), ["BZr", "p.fi", ("Bbi", gh)], [("Bbi", gh)])
            for j_ in range(NJ):
                if j_ > 0:
                    for gh in range(32):
                        S.op("dve", lambda e, gh=gh: e.tensor_scalar(out=Bt[:, gh, :], in0=Bbr[:, gh, :], scalar1=P["li"][:, gh:gh + 1], scalar2=None, op0=ALU.mult),
                             [("Bbr", gh), "p.li"], [("Bt", gh)])
                        S.op("dve", lambda e, gh=gh: e.tensor_scalar(out=Bbr[:, gh, :], in0=Bbr[:, gh, :], scalar1=P["lr"][:, gh:gh + 1], scalar2=None, op0=ALU.mult),
                             [("Bbr", gh), "p.lr"], [("Bbr", gh)])
                        S.op("dve", lambda e, gh=gh: e.scalar_tensor_tensor(out=Bbr[:, gh, :], in0=Bbi[:, gh, :], scalar=P["nli"][:, gh:gh + 1], in1=Bbr[:, gh, :],
                                                                            op0=ALU.mult, op1=ALU.add), [("Bbi", gh), "p.nli", ("Bbr", gh)], [("Bbr", gh)])
                        S.op("dve", lambda e, gh=gh: e.scalar_tensor_tensor(out=Bbi[:, gh, :], in0=Bbi[:, gh, :], scalar=P["lr"][:, gh:gh + 1], in1=Bt[:, gh, :],
                                                                            op0=ALU.mult, op1=ALU.add), [("Bbi", gh), "p.lr", ("Bt", gh)], [("Bbi", gh)])
                for ri, src in enumerate((Bbr, Bbi)):
                    srck = "Bbr" if ri == 0 else "Bbi"
                    for g0 in range(0, 32, 4):
                        for j in range(4):
                            S.op("pe", lambda e, src=src, g0=g0, j=j: e.transpose(ps[0][0:32, j * 128:(j + 1) * 128], src[:, g0 + j, :], ident),
                                 [(srck, g0 + j), "ident"], ["ps0"])
                        S.op("act", lambda e, g0=g0, ri=ri, j_=j_: e.activation(out=BbT[:, g0:g0 + 4, j_, ri, :], in_=ps[0][0:32, :].rearrange("p (j c) -> p j c", j=4), func=AF.Copy),
                             ["ps0"], ["BbT"])
            S.op("act", lambda e: e.activation(out=Cbr, in_=CZr, func=AF.Copy), ["CZr"], ["Cbr"])
            S.op("act", lambda e: e.activation(out=Cbi, in_=CZi, func=AF.Copy, scale=-1.0), ["CZi"], ["Cbi"])
            S.op("dve", lambda e: e.memset(Hc[:, :, 0:16], 0.0), [], ["Hca"])
            S.op("pool", lambda e: e.memset(Hc[:, :, 16:32], 0.0), [], ["Hcb"])
            S.op("pool", lambda e: e.memset(uTe, 0.0), [], ["uTe"])
            order = list(range(NT)) if d_ == 0 else [1, 0] + list(range(NT - 1, 1, -1))
            c0u = 3 if d_ == 0 else 0
            for bi, t in enumerate(order):
                S.dma("qsp", ublk, PROJ[t * 128:(t + 1) * 128, 4096:5120], reads=["PROJ"], writes=["ublk"])
                if bi > 0:
                    if d_ == 0:
                        S.op("pool", lambda e: e.tensor_copy(out=uTe[:, :, 0:3], in_=uTe[:, :, 128:131]), ["uTe"], ["uTe"])
                    else:
                        S.op("pool", lambda e: e.tensor_copy(out=uTe[:, :, 128:131], in_=uTe[:, :, 0:3]), ["uTe"], ["uTe"])
                for g0 in range(0, 32, 4):
                    for j in range(4):
                        S.op("pe", lambda e, g0=g0, j=j: e.transpose(ps[1][0:32, j * 128:(j + 1) * 128], ublk[:, (g0 + j) * 32:(g0 + j + 1) * 32], ident),
                             ["ublk", "ident"], ["ps1"])
                    S.op("act", lambda e, g0=g0: e.activation(out=uTe[:, g0:g0 + 4, c0u:c0u + 128], in_=ps[1][0:32, :].rearrange("p (j c) -> p j c", j=4), func=AF.Copy),
                         ["ps1"], ["uTe"])
                for ri, dst in ((0, BUr), (1, BUi)):
                    for g0 in range(0, 32, 4):
                        pb = 2 + (g0 // 4) % 2
                        for j in range(4):
                            for j_ in range(NJ):
                                cj = (c0u - j_) if d_ == 0 else (c0u + j_)
                                S.op("pe", lambda e, pb=pb, g0=g0, j=j, ri=ri, j_=j_, cj=cj: e.matmul(ps[pb][:, j * 128:(j + 1) * 128], lhsT=BbT[:, g0 + j, j_, ri, :],
                                                                                                     rhs=uTe[:, g0 + j, cj:cj + 128], start=(j_ == 0), stop=(j_ == NJ - 1)),
                                     ["BbT", "uTe"], [f"ps{pb}"])
                        S.op("act", lambda e, pb=pb, dst=dst, g0=g0: e.activation(out=dst[:, g0:g0 + 4, :], in_=ps[pb].rearrange("p (j c) -> p j c", j=4), func=AF.Copy),
                             [f"ps{pb}"], ["BUXa", "BUXb"])
                ks = range(32) if d_ == 0 else range(31, -1, -1)
                for eng, gs, sfx in (("dve", slice(0, 16), "a"), ("pool", slice(16, 32), "b")):
                    prev = Hc[:, :, gs, :]
                    pk_ = "Hc" + sfx
                    bk_ = "BUX" + sfx
                    for k_ in ks:
                        cur = BUX[:, :, gs, 4 * k_:4 * k_ + 4]
                        S.op(eng, lambda e, prev=prev, gs=gs: e.tensor_tensor(out=T1[:, :, gs, :], in0=LR4[:, :, gs, :], in1=prev[:, 0:2], op=ALU.mult),
                             [pk_, bk_, "LR4"], ["T1" + sfx])
                        S.op(eng, lambda e, prev=prev, gs=gs: e.tensor_tensor(out=T2[:, :, gs, :], in0=LI4[:, :, gs, :], in1=prev[:, 1:3], op=ALU.mult),
                             [pk_, bk_, "LI4"], ["T2" + sfx])
                        S.op(eng, lambda e, cur=cur, gs=gs: e.tensor_tensor(out=cur[:, 0:2], in0=cur[:, 0:2], in1=T1[:, :, gs, :], op=ALU.add),
                             [bk_, "T1" + sfx], [bk_])
                        S.op(eng, lambda e, cur=cur, gs=gs: e.tensor_tensor(out=cur[:, 0:2], in0=cur[:, 0:2], in1=T2[:, :, gs, :], op=ALU.add),
                             [bk_, "T2" + sfx], [bk_])
                        S.op(eng, lambda e, cur=cur: e.tensor_copy(out=cur[:, 2], in_=cur[:, 0]), [bk_], [bk_])
                        prev = cur
                    S.op(eng, lambda e, prev=prev, gs=gs: e.tensor_copy(out=Hc[:, :, gs, :], in_=prev), [bk_], [pk_])
                S.op("act", lambda e: e.activation(out=Hbr, in_=BUr, func=AF.Copy), ["BUXa", "BUXb"], ["Hbr"])
                S.op("act", lambda e: e.activation(out=Hbi, in_=BUi, func=AF.Copy), ["BUXa", "BUXb"], ["Hbi"])
                for gh in range(32):
                    pb = 4 + gh // 16
                    c0 = (gh % 16) * 32
                    S.op("pe", lambda e, pb=pb, c0=c0, gh=gh: e.matmul(ps[pb][:, c0:c0 + 32], lhsT=Hbr[:, gh, :], rhs=Cbr[:, gh, :], start=True, stop=False),
                         ["Hbr", "Cbr"], [f"ps{pb}"])
                    S.op("pe", lambda e, pb=pb, c0=c0, gh=gh: e.matmul(ps[pb][:, c0:c0 + 32], lhsT=Hbi[:, gh, :], rhs=Cbi[:, gh, :], start=False, stop=True),
                         ["Hbi", "Cbi"], [f"ps{pb}"])
                if d_ == 0:
                    S.op("pool", lambda e: e.tensor_tensor(out=ysb, in0=ublk, in1=dsk, op=ALU.mult), ["ublk", "dsk"], ["ysb"])
                else:
                    S.dma("qpool", ysb, Y5[t * 128:(t + 1) * 128, :], reads=["Y5"], writes=["ysb"])
                for hb in range(2):
                    S.op("dve", lambda e, hb=hb: e.tensor_tensor(out=ysb[:, hb * 512:(hb + 1) * 512], in0=ps[4 + hb], in1=ysb[:, hb * 512:(hb + 1) * 512], op=ALU.add),
                         [f"ps{4 + hb}", "ysb"], ["ysb"])
                S.dma("qsp", Y5[t * 128:(t + 1) * 128, :], ysb, reads=["ysb"], writes=["Y5"])
        if l == 0 and "Y5" in dbg_out:
            S.dma("qsp", dbg_out["Y5"], Y5, reads=["Y5"], writes=["dbg"])
        if stop == "P4":
            S.finish(["dbg"])
            return nc

        S.barrier()
        last = (l == DEPTH - 1)
        T0 = 2 if last else 0
        for t in range(T0, NT):
            x_ = xt[t % 2]; xk = f"xt{t % 2}"
            y_ = x_[:, 0:1024]; w_ = x_[:, 1024:2048]
            S.dma("qsp", y_, Y5[t * 128:(t + 1) * 128, :], reads=["Y5"], writes=[xk])
            S.op("dve", lambda e, y_=y_, w_=w_: e.tensor_tensor(out=w_, in0=y_, in1=y_, op=ALU.mult), [xk], [xk])
            S.op("dve", lambda e, w_=w_: e.tensor_scalar(out=w_, in0=w_, scalar1=0.044715, scalar2=1.0, op0=ALU.mult, op1=ALU.add), [xk], [xk])
            S.op("dve", lambda e, y_=y_, w_=w_: e.tensor_tensor(out=w_, in0=w_, in1=y_, op=ALU.mult), [xk], [xk])
            S.op("act", lambda e, w_=w_: e.activation(out=w_, in_=w_, func=AF.Tanh, scale=float(np.sqrt(2.0 / np.pi))), [xk], [xk])
            S.op("dve", lambda e, w_=w_: e.tensor_scalar(out=w_, in0=w_, scalar1=0.5, scalar2=0.5, op0=ALU.mult, op1=ALU.add), [xk], [xk])
            S.op("dve", lambda e, y_=y_, w_=w_: e.tensor_tensor(out=y_, in0=w_, in1=y_, op=ALU.mult), [xk], [xk])
            S.dma("qpool", Z5[t * 128:(t + 1) * 128, :], y_, reads=[xk], writes=["Z5"])
            for kq in range(2):
                p_ = ps[1 + kq]; pk = f"ps{1 + kq}"
                for j in range(4):
                    kk = kq * 4 + j
                    S.op("pe", lambda e, p_=p_, j=j, kk=kk, y_=y_: e.transpose(p_[:, j * 128:(j + 1) * 128], y_[:, kk * 128:(kk + 1) * 128], ident),
                         [xk, "ident"], [pk])
                S.op("act", lambda e, p_=p_, kq=kq, t=t: e.activation(out=hT[:, kq * 4:(kq + 1) * 4, t * 128:(t + 1) * 128],
                                                                       in_=p_.rearrange("p (j c) -> p j c", j=4), func=AF.Copy), [pk], [("hT", t)])
        S.dma("qsp", junk[:, 0:1024], I["s5_glu_b"][l].partition_broadcast(128), writes=["junk"])
        for cb in range(2):
            W = big[cb % 2]; wk = f"big{cb % 2}"; Wb = wbf[0]; wbk = "wbf0"
            S.dma("qsp", W[:, 0:8, :], I["s5_glu_w"][l, :, cb * 512:(cb + 1) * 512].rearrange("(k p) c -> p k c", p=128), writes=[wk])
            S.op("act", lambda e, W=W, Wb=Wb: e.activation(out=Wb[:, 0:8, :], in_=W[:, 0:8, :], func=AF.Copy), [wk], [wbk])
            for t in range(T0, NT):
                p_ = ps[3 + t % 2]; pk = f"ps{3 + t % 2}"
                for kk in range(8):
                    S.op("pe", lambda e, p_=p_, kk=kk, t=t, Wb=Wb: e.matmul(p_, lhsT=hT[:, kk, t * 128:(t + 1) * 128], rhs=Wb[:, kk, :],
                                                                            start=(kk == 0), stop=(kk == 7)), [("hT", t), wbk], [pk])
                o_ = po[t % 2]; ok = f"po{t % 2}"; z_ = rt[t % 2]; zk = f"rt{t % 2}"
                S.dma("qpool", z_, Z5[t * 128:(t + 1) * 128, cb * 512:(cb + 1) * 512], reads=["Z5"], writes=[zk])
                S.op("dve", lambda e, o_=o_, p_=p_, cb=cb: e.tensor_tensor(out=o_, in0=p_, in1=junk[:, cb * 512:(cb + 1) * 512], op=ALU.add), [pk, "junk"], [ok])
                S.op("act", lambda e, o_=o_: e.activation(out=o_, in_=o_, func=AF.Sigmoid), [ok], [ok])
                S.op("pool", lambda e, o_=o_, z_=z_: e.tensor_tensor(out=o_, in0=o_, in1=z_, op=ALU.mult), [ok, zk], [ok])
                S.dma("qsp", MIX[t * 128:(t + 1) * 128, 1024 + cb * 512:1024 + (cb + 1) * 512], o_, reads=[ok], writes=["MIX"])

        S.barrier()
        for t in range(T0, NT):
            x_ = xt[t % 2]; xk = f"xt{t % 2}"
            S.dma("qsp" if t % 2 == 0 else "qpool", x_, MIX[t * 128:(t + 1) * 128, :], reads=["MIX"], writes=[xk])
            for kq in range(4):
                p_ = ps[1 + kq % 2]; pk = f"ps{1 + kq % 2}"
                for j in range(4):
                    kk = kq * 4 + j
                    S.op("pe", lambda e, p_=p_, j=j, kk=kk, x_=x_: e.transpose(p_[:, j * 128:(j + 1) * 128], x_[:, kk * 128:(kk + 1) * 128], ident),
                         [xk, "ident"], [pk])
                S.op("act" if kq % 2 == 0 else "dve",
                     (lambda e, p_=p_, kq=kq, t=t: e.activation(out=hT[:, kq * 4:(kq + 1) * 4, t * 128:(t + 1) * 128], in_=p_.rearrange("p (j c) -> p j c", j=4), func=AF.Copy))
                     if kq % 2 == 0 else
                     (lambda e, p_=p_, kq=kq, t=t: e.tensor_copy(out=hT[:, kq * 4:(kq + 1) * 4, t * 128:(t + 1) * 128], in_=p_.rearrange("p (j c) -> p j c", j=4))),
                     [pk], [("hT", t)])
        S.barrier()
        for tt in range(2):
            S.dma("qsp", xt[tt], MOD[l, tt, 2 * D:3 * D].partition_broadcast(128), reads=["MOD"], writes=[f"xt{tt}"])
        for cb in range(4):
            W = big[cb % 2]; wk = f"big{cb % 2}"; Wb = wbf[0]; wbk = "wbf0"
            S.dma("qsp" if cb % 2 == 0 else "qpool", W, I["w_out"][l, :, cb * 512:(cb + 1) * 512].rearrange("(k p) c -> p k c", p=128), writes=[wk])
            S.op("act", lambda e, W=W, Wb=Wb: e.activation(out=Wb[:, 0:8, :], in_=W[:, 0:8, :], func=AF.Copy), [wk], [wbk])
            S.op("pool", lambda e, W=W, Wb=Wb: e.tensor_copy(out=Wb[:, 8:16, :], in_=W[:, 8:16, :]), [wk], [wbk])
            for t in range(T0, NT):
                tt = 0 if t < 2 else 1
                p_ = ps[3 + t % 2]; pk = f"ps{3 + t % 2}"
                for kk in range(KT):
                    S.op("pe", lambda e, p_=p_, kk=kk, t=t, Wb=Wb: e.matmul(p_, lhsT=hT[:, kk, t * 128:(t + 1) * 128], rhs=Wb[:, kk, :],
                                                                            start=(kk == 0), stop=(kk == KT - 1)), [("hT", t), wbk], [pk])
                o_ = po[t % 2]; ok = f"po{t % 2}"; z_ = rt[t % 2]; zk = f"rt{t % 2}"
                S.dma("qpool", z_, XR[t * 128:(t + 1) * 128, cb * 512:(cb + 1) * 512], reads=["XR"], writes=[zk])
                S.op("dve", lambda e, o_=o_, p_=p_, cb=cb, tt=tt: e.tensor_tensor(out=o_, in0=p_, in1=xt[tt][:, cb * 512:(cb + 1) * 512], op=ALU.mult), [pk, f"xt{tt}"], [ok])
                S.op("pool", lambda e, o_=o_, z_=z_: e.tensor_tensor(out=o_, in0=o_, in1=z_, op=ALU.add), [ok, zk], [ok])
                S.dma("qsp", XR[t * 128:(t + 1) * 128, cb * 512:(cb + 1) * 512], o_, reads=[ok], writes=["XR"])
        if l == 0 and "X1" in dbg_out:
            S.dma("qsp", dbg_out["X1"], XR, reads=["XR"], writes=["dbg"])
        if stop == "P5":
            S.finish(["dbg"])
            return nc

        S.barrier()
        norm_mod_T(l, "norm2_g", 3, 4, router=(RLEVEL >= 0), t_start=T0)
        if l == 0 and "GATE" in dbg_out:
            S.dma("qsp", dbg_out["GATE"], GATE, reads=["GATE"], writes=["dbg"])
        if "HT2" in dbg_out:
            S.dma("qpool", dbg_out["HT2"].rearrange("(k p) t -> p k t", p=128), hT, reads=HT_ALL, writes=["dbg"])
        if stop == "P6":
            S.finish(["dbg"])
            print("counts", S.count, "waits", S.nwaits)
            return nc

        S.barrier()
        ARB = SB0 + 12 * 1024
        wbf2 = nc.alloc_sbuf_tensor_at(f"wbf2L{l}", [128, KT, 512], BF16, offset=ARB + KT * NTOK * 2).ap()
        GT = pers(f"GT{l}", [128, NT, 32])
        S.dma("qsp", GT, GATE.rearrange("(t p) e -> p t e", p=128), reads=["GATE"], writes=["GT"])
        aTt = [rt[0].bitcast(BF16), rt[1].bitcast(BF16)]
        for ex in range(32):
            S.dma("qsp", big[0], I["exp_w_gate"][l, ex].rearrange("(k p) c -> p k c", p=128), writes=["big0"])
            S.dma("qpool", big[1], I["exp_w_up"][l, ex].rearrange("(k p) c -> p k c", p=128), writes=["big1"])
            S.op("act", lambda e: e.activation(out=wbf[0], in_=big[0], func=AF.Copy), ["big0"], ["wbf0"])
            S.op("pool", lambda e: e.tensor_copy(out=wbf2, in_=big[1]), ["big1"], ["wbf2"])
            for t in range(T0, NT):
                pG = ps[2 + t % 2]; pGk = f"ps{2 + t % 2}"; pU = ps[4 + t % 2]; pUk = f"ps{4 + t % 2}"
                for kk in range(KT):
                    S.op("pe", lambda e, pG=pG, kk=kk, t=t: e.matmul(pG, lhsT=hT[:, kk, t * 128:(t + 1) * 128], rhs=wbf[0][:, kk, :],
                                                                     start=(kk == 0), stop=(kk == KT - 1)), [("hT", t), "wbf0"], [pGk])
                for kk in range(KT):
                    S.op("pe", lambda e, pU=pU, kk=kk, t=t: e.matmul(pU, lhsT=hT[:, kk, t * 128:(t + 1) * 128], rhs=wbf2[:, kk, :],
                                                                     start=(kk == 0), stop=(kk == KT - 1)), [("hT", t), "wbf2"], [pUk])
                o_ = po[t % 2]; ok = f"po{t % 2}"
                S.op("act", lambda e, o_=o_, pG=pG: e.activation(out=o_, in_=pG, func=AF.Silu), [pGk], [ok])
                S.op("dve", lambda e, o_=o_, pU=pU, t=t, ex=ex: e.scalar_tensor_tensor(out=o_, in0=o_, scalar=GT[:, t, ex:ex + 1], in1=pU,
                                                                                       op0=ALU.mult, op1=ALU.mult), [ok, pUk, "GT"], [ok])
                for j in range(4):
                    S.op("pe", lambda e, o_=o_, j=j: e.transpose(ps[6][:, j * 128:(j + 1) * 128], o_[:, j * 128:(j + 1) * 128], ident), [ok, "ident"], ["ps6"])
                a_ = aTt[t % 2][:, 0:512]; ak = f"rt{t % 2}"
                S.op("act", lambda e, a_=a_: e.activation(out=a_, in_=ps[6], func=AF.Copy), ["ps6"], [ak])
                S.dma("qsp" if t % 2 == 0 else "qpool", ATd[ex, :, :, t * 128:(t + 1) * 128], a_.rearrange("p (k c) -> p k c", k=4), reads=[ak], writes=["ATd"])
        S.barrier()
        acc = nc.alloc_sbuf_tensor_at(f"accL{l}", [128, NT, 2, 512], F32, offset=ARB).ap()
        RB0 = ARB + KT * NTOK * 2 + 2 * D * 4
        ATe = [nc.alloc_sbuf_tensor_at(f"ATe{i}L{l}", [128, 4, NTOK], BF16, offset=RB0 + i * 18432).ap() for i in range(2)]
        Wd32 = [nc.alloc_sbuf_tensor_at(f"Wd32_{i}L{l}", [128, 4, 1024], F32, offset=RB0 + 36864 + i * 16384).ap() for i in range(2)]
        Wd16 = [nc.alloc_sbuf_tensor_at(f"Wd16_{i}L{l}", [128, 4, 1024], BF16, offset=RB0 + 36864 + 32768 + i * 8192).ap() for i in range(2)]
        for tt in range(2):
            S.dma("qsp", xt[tt], MOD[l, tt, 5 * D:6 * D].partition_broadcast(128), reads=["MOD"], writes=[f"xt{tt}"])
        for dg in range(2):
            S.op("dve", lambda e: e.memset(acc, 0.0), [], [("acc", t) for t in range(NT)])
            for ex in range(32):
                bi_ = ex % 2
                S.dma("qsp", Wd32[bi_], I["exp_w_down"][l, ex, :, dg * 1024:(dg + 1) * 1024].rearrange("(k p) c -> p k c", p=128), writes=[f"Wd32_{bi_}"])
                S.op("act", lambda e, bi_=bi_: e.activation(out=Wd16[bi_], in_=Wd32[bi_], func=AF.Copy), [f"Wd32_{bi_}"], [f"Wd16_{bi_}"])
                S.dma("qpool", ATe[bi_], ATd[ex], reads=["ATd"], writes=[f"ATe{bi_}"])
                for t in range(T0, NT):
                    for hb in range(2):
                        p_ = ps[2 + (2 * t + hb) % 4]; pk = f"ps{2 + (2 * t + hb) % 4}"
                        for kk in range(4):
                            S.op("pe", lambda e, p_=p_, kk=kk, t=t, hb=hb, bi_=bi_: e.matmul(p_, lhsT=ATe[bi_][:, kk, t * 128:(t + 1) * 128], rhs=Wd16[bi_][:, kk, hb * 512:(hb + 1) * 512],
                                                                                         start=(kk == 0), stop=(kk == 3)), [f"ATe{bi_}", f"Wd16_{bi_}"], [pk])
                        S.op("dve", lambda e, p_=p_, t=t, hb=hb: e.tensor_tensor(out=acc[:, t, hb, :], in0=p_, in1=acc[:, t, hb, :], op=ALU.add), [pk, ("acc", t)], [("acc", t)])
            for t in range(T0, NT):
                tt = 0 if t < 2 else 1
                for hb in range(2):
                    dc = dg * 2 + hb
                    o_ = po[hb]; ok = f"po{hb}"
                    S.dma("qpool", o_, XR[t * 128:(t + 1) * 128, dc * 512:(dc + 1) * 512], reads=["XR"], writes=[ok])
                    S.op("dve", lambda e, t=t, tt=tt, dc=dc, hb=hb: e.tensor_tensor(out=acc[:, t, hb, :], in0=acc[:, t, hb, :], in1=xt[tt][:, dc * 512:(dc + 1) * 512], op=ALU.mult),
                         [("acc", t), f"xt{tt}"], [("acc", t)])
                    S.op("pool", lambda e, o_=o_, t=t, hb=hb: e.tensor_tensor(out=o_, in0=o_, in1=acc[:, t, hb, :], op=ALU.add), [ok, ("acc", t)], [ok])
                    S.dma("qsp", XR[t * 128:(t + 1) * 128, dc * 512:(dc + 1) * 512], o_, reads=[ok], writes=["XR"])
        if l == 0 and "X2" in dbg_out:
            S.dma("qsp", dbg_out["X2"], XR, reads=["XR"], writes=["dbg"])
        if stop == "P7":
            S.finish(["dbg"])
            return nc

    S.barrier()
    S.dma("qsp", junk, I["final_g"].partition_broadcast(128), writes=["junk"])
    for t in range(2, NT):
        x_ = xt[t % 2]; xk = f"xt{t % 2}"
        S.dma("qsp" if t % 2 == 0 else "qpool", x_, XR[t * 128:(t + 1) * 128, :], reads=["XR"], writes=[xk])
        ss = small[:, 0:1]
        S.op("act", lambda e, x_=x_: e.activation(out=big[1][:, 0:4, :].rearrange("p a b -> p (a b)"), in_=x_, func=AF.Square, accum_out=ss), [xk], ["sqj", "ss"])
        S.op("act", lambda e: e.activation(out=small[:, 1:2], in_=ss, func=AF.Sqrt, scale=1.0 / D, bias=eps_t[:, 0:1]), ["ss"], ["rs"])
        S.op("dve", lambda e: e.reciprocal(out=small[:, 2:3], in_=small[:, 1:2]), ["rs"], ["rstd"])
        S.op("dve", lambda e, x_=x_: e.scalar_tensor_tensor(out=x_, in0=x_, scalar=small[:, 2:3], in1=junk, op0=ALU.mult, op1=ALU.mult), [xk, "rstd", "junk"], [xk])
        S.dma("qsp", out_final[(t - 2) * 128:(t - 1) * 128, :], x_, reads=[xk], writes=["OUT"])
    S.finish(["OUT"])
    return nc


def rope_tables():
    rows = NLAT // 64
    row = np.repeat(np.arange(rows, dtype=np.float32), 64)
    col = np.tile(np.arange(64, dtype=np.float32), rows)
    n_freq = 32
    inv = (np.float32(10000.0) ** (-np.arange(n_freq, dtype=np.float32) / n_freq)).astype(np.float32)
    ang = np.concatenate([row[:, None] * inv, col[:, None] * inv], axis=-1).astype(np.float32)
    cos = np.cos(ang).astype(np.float32)
    sin = np.sin(ang).astype(np.float32)
    return np.tile(cos, (1, 4)), np.tile(sin, (1, 4))


_i = np.arange(128, dtype=np.float32)
POS_TAB = np.ascontiguousarray(np.stack([
    np.stack([_i + 1, -(_i + 1), 127 - _i, np.full(128, 128.0, np.float32)], -1),
    np.stack([128 - _i, -(128 - _i), _i, np.full(128, 128.0, np.float32)], -1)], 1).astype(np.float32))
MASK_F = (np.arange(128)[None, :] >= np.arange(128)[:, None]).astype(np.float32)
MASK_B = (np.arange(128)[:, None] > np.arange(128)[None, :]).astype(np.float32)


def s5_layouts(inputs):
    out = {}
    def rg(a):
        L_ = a.shape[0]
        return np.ascontiguousarray(a.reshape(L_, 2, 32, 2, 64).transpose(0, 1, 3, 4, 2).reshape(L_, 2, 128, 32))
    out["s5_ar"] = rg(inputs["s5_a_re"]); out["s5_ai"] = rg(inputs["s5_a_im"])
    ls = inputs["s5_log_step"]
    out["s5_ls"] = rg(np.broadcast_to(ls[..., None], ls.shape + (64,)))
    def bz(b):
        L_ = b.shape[0]
        bb = b.reshape(L_, 2, 32, 2, 64, 16)
        z = np.zeros((L_, 2, 2, 64, 32, 2, 16), np.float32)
        for gl in range(2):
            z[:, :, gl, :, :, gl, :] = bb[:, :, :, gl].transpose(0, 1, 3, 2, 4)
        return np.ascontiguousarray(z.reshape(L_, 2, 128, 32, 32))
    out["s5_bzr"] = bz(inputs["s5_b_re"]); out["s5_bzi"] = bz(inputs["s5_b_im"])
    out["s5_czr"] = bz(inputs["s5_c_re"].transpose(0, 1, 2, 4, 3)); out["s5_czi"] = bz(inputs["s5_c_im"].transpose(0, 1, 2, 4, 3))
    out["s5_d"] = np.ascontiguousarray(inputs["s5_d"].reshape(DEPTH, 1024))
    return out


def make_in_maps(inputs, nb=4):
    cos4, sin4 = rope_tables()
    s5l = s5_layouts(inputs)
    rwc = np.concatenate([inputs["router_grp_w"], inputs["router_exp_w"]], -1)
    RW_ = np.ascontiguousarray(rwc.reshape(DEPTH, KT, 128, 36).transpose(0, 2, 1, 3))
    RB_ = np.ascontiguousarray(np.concatenate([inputs["router_grp_b"], inputs["router_exp_b"]], -1))
    maps = []
    for b in range(nb):
        cc = np.stack([inputs["c_ctx"], inputs["c"][b]], 0)
        ccT = np.ascontiguousarray(cc.reshape(2, KT, 128).transpose(2, 1, 0))
        m = {
            "xin": np.ascontiguousarray(inputs["x"][b]),
            "ctxin": np.ascontiguousarray(inputs["ctx"][b]),
            "ccT": ccT,
            "ada_w": inputs["ada_w"], "ada_b": inputs["ada_b"],
            "norm1_g": inputs["norm1_g"], "w_in": inputs["w_in"],
            "ident": np.eye(128, dtype=np.float32),
            "rope_cos": cos4, "rope_sin": sin4,
            "ret_decay": np.ascontiguousarray(inputs["ret_decay"].reshape(DEPTH, 16)),
            "pos": POS_TAB, "maskF": MASK_F, "maskB": MASK_B,
            **s5l,
            "s5_glu_w": inputs["s5_glu_w"], "s5_glu_b": inputs["s5_glu_b"], "w_out": inputs["w_out"],
            "norm2_g": inputs["norm2_g"], "final_g": inputs["final_g"],
            "rw": RW_, "rb": RB_,
            "exp_w_gate": inputs["exp_w_gate"], "exp_w_up": inputs["exp_w_up"], "exp_w_down": inputs["exp_w_down"],
        }
        maps.append(m)
    return maps


def kernel(**inputs):
    inputs = {k_: np.asarray(v) for k_, v in inputs.items()}
    nc = build()
    maps = make_in_maps(inputs)
    res = run_bass_kernel_spmd(nc, maps, core_ids=list(range(4)))
    return np.stack([r["out"] for r in res.results], 0)
```

```python
import numpy as np
import concourse.bass as bass
import concourse.mybir as mybir
from concourse.bass_utils import run_bass_kernel_spmd

F32 = mybir.dt.float32
BF16 = mybir.dt.bfloat16
AF = mybir.ActivationFunctionType
ALU = mybir.AluOpType
AX = mybir.AxisListType

D = 2048
NCTX = 256
NLAT = 2048
NTOK = NCTX + NLAT
NT = NTOK // 128
KT = D // 128
INW = 5120
DEPTH = 2
import os
RLEVEL = int(os.environ.get("RLEVEL", "9"))
RFLAGS = os.environ.get("RFLAGS", "WBE")
_sp = int(os.environ.get("S5SPLIT", "32"))
S5_SPLIT = (("dve", slice(0, _sp), "a"),) + ((("pool", slice(_sp, 32), "b"),) if _sp < 32 else ())
EPOCH = 30000
NDMASEM = 8


class Sched:
    def __init__(self, nc, same_engine_sync=("dve", "act", "pool")):
        self.nc = nc
        self.E = {"pe": nc.tensor, "dve": nc.vector, "act": nc.scalar,
                  "pool": nc.gpsimd, "sp": nc.sync}
        self.same = set(same_engine_sync)
        self.dmaq = {"qsp": "sp", "qpool": "pool", "qact": "act"}
        self.sems = {}
        self.count = {}
        self.waited = {}
        self.res = {}
        self.nwaits = 0

    def _sem(self, stream, idx):
        if stream in self.dmaq:
            key = (stream, (idx - 1) % NDMASEM)
            val = 16 * ((idx - 1) // NDMASEM + 1)
        else:
            key = (stream, (idx - 1) // EPOCH)
            val = (idx - 1) % EPOCH + 1
        if key not in self.sems:
            self.sems[key] = self.nc.alloc_semaphore(f"s_{key[0]}_{key[1]}")
        return self.sems[key], val

    def _is_waited(self, eng, stream, idx):
        w = self.waited.setdefault(eng, {})
        if stream in self.dmaq:
            return idx in w.get(stream, ())
        return w.get(stream, 0) >= idx

    def _wait(self, eng, stream, idx):
        if self._is_waited(eng, stream, idx):
            return
        if stream == eng and eng not in self.same:
            return
        h, v = self._sem(stream, idx)
        self.E[eng].wait_ge(h, v)
        self.nwaits += 1
        w = self.waited[eng]
        if stream in self.dmaq:
            w.setdefault(stream, set()).add(idx)
        else:
            w[stream] = idx

    def _deps(self, eng, reads, writes):
        need = set()
        for k in reads:
            st = self.res.get(k)
            if st and st[0]:
                need.add(st[0])
        for k in writes:
            st = self.res.get(k)
            if st:
                if st[0]:
                    need.add(st[0])
                need.update(st[1])
        mx = {}
        for s, i in need:
            if s in self.dmaq:
                self._wait(eng, s, i)
            else:
                mx[s] = max(mx.get(s, 0), i)
        for s, i in mx.items():
            self._wait(eng, s, i)

    def _record(self, stream, idx, reads, writes):
        for k in reads:
            st = self.res.setdefault(k, [None, []])
            if stream not in self.dmaq:
                st[1] = [r for r in st[1] if r[0] != stream]
            st[1].append((stream, idx))
        for k in writes:
            self.res[k] = [(stream, idx), []]

    def op(self, eng, fn, reads=(), writes=()):
        self._deps(eng, reads, writes)
        idx = self.count.get(eng, 0) + 1
        self.count[eng] = idx
        h, v = self._sem(eng, idx)
        fn(self.E[eng]).then_inc(h, 1)
        self._record(eng, idx, reads, writes)
        return idx

    def dma(self, q, out, in_, reads=(), writes=(), **kw):
        eng = self.dmaq[q]
        self._deps(eng, reads, writes)
        idx = self.count.get(q, 0) + 1
        self.count[q] = idx
        if idx > NDMASEM:
            self._wait(eng, q, idx - NDMASEM)
        h, v = self._sem(q, idx)
        self.E[eng].dma_start(out=out, in_=in_, **kw).then_inc(h, 16)
        self._record(q, idx, reads, writes)
        return idx

    def finish(self, keys, eng="sp"):
        self._deps(eng, list(keys), [])

    def barrier(self):
        for eng in self.E:
            for s_, n in list(self.count.items()):
                if n == 0:
                    continue
                if s_ in self.dmaq:
                    for i in range(max(1, n - NDMASEM + 1), n + 1):
                        self._wait(eng, s_, i)
                elif s_ != eng or eng in self.same:
                    self._wait(eng, s_, n)
        self.res = {}


class Arena:
    def __init__(self, nc, base, limit):
        self.nc, self.off, self.limit = nc, base, limit
        self.n = 0

    def __call__(self, name, shape, dt=F32):
        esz = 2 if dt == BF16 else 4
        size = esz
        for d_ in shape[1:]:
            size *= d_
        off = (self.off + 31) // 32 * 32
        self.off = off + size
        assert self.off <= self.limit, (name, self.off, self.limit)
        return self.nc.alloc_sbuf_tensor_at(name, list(shape), dt, offset=off).ap()


class K:
    pass


def build(stop=None, dbg=()):
    nc = bass.Bass("TRN2", target_bir_lowering=False)
    S = Sched(nc, same_engine_sync=tuple(x for x in os.environ.get("SAMESYNC", "dve,act,pool").split(",") if x))
    k = K()
    k.nc, k.S = nc, S

    def din(name, shape, dt=F32):
        return nc.dram_tensor(name, list(shape), dt, kind="ExternalInput").ap()

    def dscr(name, shape, dt=F32):
        return nc.dram_tensor(name, list(shape), dt, kind="Internal").ap()

    def dout(name, shape, dt=F32):
        return nc.dram_tensor(name, list(shape), dt, kind="ExternalOutput").ap()

    def sb(name, shape, dt=F32):
        return nc.alloc_sbuf_tensor(name, list(shape), dt).ap()

    I = {}
    I["xin"] = din("xin", [NLAT, D])
    I["ctxin"] = din("ctxin", [NCTX, D])
    I["ccT"] = din("ccT", [128, KT, 2])
    I["ada_w"] = din("ada_w", [DEPTH, D, 6 * D])
    I["ada_b"] = din("ada_b", [DEPTH, 6 * D])
    I["norm1_g"] = din("norm1_g", [DEPTH, D])
    I["w_in"] = din("w_in", [DEPTH, D, INW])
    I["ident"] = din("ident", [128, 128])
    I["rope_cos"] = din("rope_cos", [NLAT, 256])
    I["rope_sin"] = din("rope_sin", [NLAT, 256])
    I["ret_decay"] = din("ret_decay", [DEPTH, 16])
    I["pos"] = din("pos", [128, 2, 4])
    I["maskF"] = din("maskF", [128, 128])
    I["maskB"] = din("maskB", [128, 128])
    for nm in ("s5_ar", "s5_ai", "s5_ls"):
        I[nm] = din(nm, [DEPTH, 2, 128, 32])
    for nm in ("s5_bzr", "s5_bzi", "s5_czr", "s5_czi"):
        I[nm] = din(nm, [DEPTH, 2, 128, 32, 32])
    I["s5_d"] = din("s5_d", [DEPTH, 1024])
    I["s5_glu_w"] = din("s5_glu_w", [DEPTH, 1024, 1024])
    I["s5_glu_b"] = din("s5_glu_b", [DEPTH, 1024])
    I["w_out"] = din("w_out", [DEPTH, D, D])
    I["norm2_g"] = din("norm2_g", [DEPTH, D])
    I["final_g"] = din("final_g", [D])
    I["rw"] = din("rw", [DEPTH, 128, KT, 36])
    I["rb"] = din("rb", [DEPTH, 36])
    I["exp_w_gate"] = din("exp_w_gate", [DEPTH, 32, D, 512])
    I["exp_w_up"] = din("exp_w_up", [DEPTH, 32, D, 512])
    I["exp_w_down"] = din("exp_w_down", [DEPTH, 32, 512, D])
    out_final = dout("out", [NLAT, D])

    XR = dscr("XR", [NTOK, D])
    MOD = dscr("MOD", [DEPTH, 2, 6 * D])
    PROJ = dscr("PROJ", [NTOK, INW])
    MIX = dscr("MIX", [NTOK, D])
    Y5 = dscr("Y5", [NTOK, 1024])
    Z5 = dscr("Z5", [NTOK, 1024])
    GATE = dscr("GATE", [NTOK, 32])
    ATd = dscr("ATd", [32, 128, 4, NTOK], BF16)
    dbg_out = {}
    for name, shape in dbg:
        dbg_out[name] = dout("dbg_" + name, shape)

    SLAB = 204 * 1024
    slab = nc.alloc_sbuf_tensor("slab", [128, SLAB // 4], F32)
    SB0 = nc.lookup_mloc(slab).addr
    SB_LIMIT = SB0 + SLAB
    pers = Arena(nc, SB0, SB0 + 12 * 1024)
    ident = pers("ident_sb", [128, 128])
    small = pers("small", [128, 64])
    eps_t = pers("eps_t", [128, 2])
    S.op("dve", lambda e: e.memset(eps_t[:, 0:1], 1e-6), [], ["eps"])
    S.op("dve", lambda e: e.memset(eps_t[:, 1:2], 1e-5), [], ["eps"])
    S.dma("qsp", ident, I["ident"], writes=["ident"])

    ps = [nc.alloc_psum_tensor(f"ps{i}", [128, 512], F32).ap() for i in range(8)]

    S.dma("qsp", XR[0:NCTX, :], I["ctxin"], writes=["XR"])
    S.dma("qpool", XR[NCTX:NTOK, :], I["xin"], writes=["XR"])

    ar = Arena(nc, SB0 + 12 * 1024, SB_LIMIT)
    hT = ar("hT", [128, KT, NTOK], BF16)
    xt = [ar(f"xt{i}", [128, D]) for i in range(2)]
    big = [ar(f"big{i}", [128, KT, 512]) for i in range(2)]
    wbf = [ar(f"wbf{i}", [128, KT, 512], BF16) for i in range(1)]
    wbf.append(wbf[0])
    junk = ar("junk", [128, D])
    bc = [big[0][:, 4 * j:4 * j + 4, :].rearrange("p a b -> p (a b)") for j in range(4)]
    sb = ar

    ccT = sb("ccT_sb", [128, KT, 2])
    sT = sb("sT", [128, KT, 2])
    S.dma("qsp", ccT, I["ccT"], writes=["ccT"])
    S.op("act", lambda e: e.activation(out=sT, in_=ccT, func=AF.Silu), ["ccT"], ["sT"])
    ab = sb("ab", [2, 512])
    mo = sb("mo", [2, 512])
    for l in range(DEPTH):
        for cb in range(24):
            W = big[cb % 2]
            wk = f"big{cb % 2}"
            S.dma("qsp" if cb % 2 == 0 else "qpool", W,
                  I["ada_w"][l, :, cb * 512:(cb + 1) * 512].rearrange("(k p) c -> p k c", p=128),
                  writes=[wk])
            S.dma("qsp", ab, I["ada_b"][l, cb * 512:(cb + 1) * 512].partition_broadcast(2), writes=["ab"])
            for kk in range(KT):
                S.op("pe", lambda e, kk=kk, W=W: e.matmul(ps[0][0:2, :], lhsT=sT[:, kk, :], rhs=W[:, kk, :],
                                                           start=(kk == 0), stop=(kk == KT - 1)),
                     ["sT", wk], ["ps0"])
            S.op("dve", lambda e: e.tensor_tensor(out=mo, in0=ps[0][0:2, :], in1=ab, op=ALU.add),
                 ["ps0", "ab"], ["mo"])
            S.dma("qsp", MOD[l, :, cb * 512:(cb + 1) * 512], mo, reads=["mo"], writes=["MOD"])
    if "MOD" in dbg_out:
        S.dma("qsp", dbg_out["MOD"], MOD, reads=["MOD"], writes=["dbg"])
    if stop == "A":
        S.finish(["dbg"])
        return nc

    def load_bc(l, j, tt, which, qn="qsp"):
        S.dma(qn, bc[j], MOD[l, tt, which * D:(which + 1) * D].partition_broadcast(128),
              reads=["MOD"], writes=[f"bc{j}"])

    small2 = pers("small2", [128, 160])
    rbt = pers("rbt", [128, 36])

    def router_tile(l, t, pr):
        L = small2[:, 0:36]; M = small2[:, 40:72]; m8 = small2[:, 72:80]; G1 = small2[:, 80:112]; G2 = small2[:, 112:144]
        sc = small2[:, 144:160]
        k_ = "rt_"
        S.op("dve", lambda e: e.tensor_tensor(out=L, in0=pr, in1=rbt, op=ALU.add), ["ps7", "rbt"], [k_ + "L"])
        S.op("dve", lambda e: e.tensor_reduce(out=sc[:, 0:1], in_=L[:, 0:4], axis=AX.X, op=ALU.max), [k_ + "L"], [k_ + "gmax"])
        S.op("dve", lambda e: e.tensor_scalar(out=sc[:, 1:2], in0=sc[:, 0:1], scalar1=-1.0, scalar2=None, op0=ALU.mult), [k_ + "gmax"], [k_ + "ngmax"])
        S.op("act", lambda e: e.activation(out=sc[:, 4:8], in_=L[:, 0:4], func=AF.Exp, bias=sc[:, 1:2], accum_out=sc[:, 2:3]),
             [k_ + "L", k_ + "ngmax"], [k_ + "gsum", k_ + "e4"])
        S.op("dve", lambda e: e.reciprocal(out=sc[:, 3:4], in_=sc[:, 2:3]), [k_ + "gsum"], [k_ + "ggate"])
        S.op("dve", lambda e: e.tensor_scalar(out=sc[:, 8:12], in0=L[:, 0:4], scalar1=sc[:, 0:1], scalar2=None, op0=ALU.is_equal), [k_ + "L", k_ + "gmax"], [k_ + "oh"])
        S.op("dve", lambda e: e.tensor_scalar(out=sc[:, 8:12], in0=sc[:, 8:12], scalar1=-1.0, scalar2=1e30, op0=ALU.add, op1=ALU.mult), [k_ + "oh"], [k_ + "oh"])
        for g in range(4):
            S.op("dve", lambda e, g=g: e.tensor_scalar(out=M[:, g * 8:(g + 1) * 8], in0=L[:, 4 + g * 8:12 + g * 8], scalar1=sc[:, 8 + g:9 + g], scalar2=None, op0=ALU.add),
                 [k_ + "L", k_ + "oh"], [k_ + f"M{g}"])
        MALL = [k_ + f"M{g}" for g in range(4)]
        if RLEVEL < 3:
            return
        S.op("dve", lambda e: e.max(out=m8, in_=M), MALL, [k_ + "m8"])
        if RLEVEL < 4:
            return
        S.op("dve", lambda e: e.tensor_tensor(out=sc[:, 12:13], in0=m8[:, 1:2], in1=m8[:, 0:1], op=ALU.subtract), [k_ + "m8"], [k_ + "diff"])
        S.op("act", lambda e: e.activation(out=sc[:, 13:14], in_=sc[:, 12:13], func=AF.Exp), [k_ + "diff"], [k_ + "ed"])
        S.op("dve", lambda e: e.tensor_scalar(out=sc[:, 14:15], in0=sc[:, 13:14], scalar1=1.0, scalar2=None, op0=ALU.add), [k_ + "ed"], [k_ + "w1"])
        S.op("dve", lambda e: e.reciprocal(out=sc[:, 14:15], in_=sc[:, 14:15]), [k_ + "w1"], [k_ + "w1"])
        S.op("dve", lambda e: e.tensor_tensor(out=sc[:, 15:16], in0=sc[:, 13:14], in1=sc[:, 14:15], op=ALU.mult), [k_ + "ed", k_ + "w1"], [k_ + "w2"])
        S.op("dve", lambda e: e.tensor_tensor(out=sc[:, 14:15], in0=sc[:, 14:15], in1=sc[:, 3:4], op=ALU.mult), [k_ + "w1", k_ + "ggate"], [k_ + "w1"])
        S.op("dve", lambda e: e.tensor_tensor(out=sc[:, 15:16], in0=sc[:, 15:16], in1=sc[:, 3:4], op=ALU.mult), [k_ + "w2", k_ + "ggate"], [k_ + "w2"])
        S.op("dve", lambda e: e.tensor_scalar(out=G1, in0=M, scalar1=m8[:, 0:1], scalar2=sc[:, 14:15], op0=ALU.is_equal, op1=ALU.mult), MALL + [k_ + "m8", k_ + "w1"], [k_ + "G1"])
        S.op("dve", lambda e: e.tensor_scalar(out=G2, in0=M, scalar1=m8[:, 1:2], scalar2=sc[:, 15:16], op0=ALU.is_equal, op1=ALU.mult), MALL + [k_ + "m8", k_ + "w2"], [k_ + "G2"])
        S.op("dve", lambda e: e.tensor_tensor(out=G1, in0=G1, in1=G2, op=ALU.add), [k_ + "G1", k_ + "G2"], [k_ + "G1"])
        S.dma("qsp", GATE[t * 128:(t + 1) * 128, :], G1, reads=[k_ + "G1"], writes=["GATE"])

    def norm_mod_T(l, gname, sh_idx, sc_idx, router=False, t_start=0):
        S.dma("qsp", junk, I[gname][l].partition_broadcast(128), writes=["junk"])
        for tt in range(2):
            load_bc(l, 2 * tt, tt, sc_idx)
            load_bc(l, 2 * tt + 1, tt, sh_idx, "qpool")
            S.op("dve", lambda e, tt=tt: e.scalar_tensor_tensor(out=bc[2 * tt], in0=bc[2 * tt], scalar=1.0,
                                                                in1=junk, op0=ALU.add, op1=ALU.mult),
                 [f"bc{2 * tt}", "junk"], [f"bc{2 * tt}"])
        if router:
            h32 = big[1][:, :, 0:128]
            RW = big[1][:, :, 128:164]
            if "W" in RFLAGS:
                S.dma("qsp", RW, I["rw"][l], writes=["RW"])
            if "B" in RFLAGS:
                S.dma("qsp", rbt, I["rb"][l].partition_broadcast(128), writes=["rbt"])
        for t in range(t_start, NT):
            tt = 0 if t < 2 else 1
            x_ = xt[t % 2]
            xk = f"xt{t % 2}"
            S.dma("qsp" if t % 2 == 0 else "qpool", x_, XR[t * 128:(t + 1) * 128, :], reads=["XR"], writes=[xk])
            ss = small[:, 0:1]
            S.op("act", lambda e, x_=x_: e.activation(out=junk, in_=x_, func=AF.Square, accum_out=ss),
                 [xk], ["junk", "ss"])
            S.op("act", lambda e: e.activation(out=small[:, 1:2], in_=ss, func=AF.Sqrt, scale=1.0 / D, bias=eps_t[:, 0:1]),
                 ["ss"], ["rs"])
            S.op("dve", lambda e: e.reciprocal(out=small[:, 2:3], in_=small[:, 1:2]), ["rs"], ["rstd"])
            S.op("dve", lambda e, x_=x_, tt=tt: e.scalar_tensor_tensor(out=x_, in0=x_, scalar=small[:, 2:3],
                                                                       in1=bc[2 * tt], op0=ALU.mult, op1=ALU.mult),
                 [xk, "rstd", f"bc{2 * tt}"], [xk])
            S.op("pool", lambda e, x_=x_, tt=tt: e.tensor_tensor(out=x_, in0=x_, in1=bc[2 * tt + 1], op=ALU.add),
                 [xk, f"bc{2 * tt + 1}"], [xk])
            for kq in range(4):
                p_ = ps[1 + kq % 2]
                pk = f"ps{1 + kq % 2}"
                for j in range(4):
                    kk = kq * 4 + j
                    S.op("pe", lambda e, p_=p_, j=j, kk=kk, x_=x_: e.transpose(p_[:, j * 128:(j + 1) * 128],
                                                                               x_[:, kk * 128:(kk + 1) * 128], ident),
                         [xk, "ident"], [pk])
                S.op("act" if kq % 2 == 0 else "dve",
                     lambda e, p_=p_, kq=kq, t=t: e.activation(out=hT[:, kq * 4:(kq + 1) * 4, t * 128:(t + 1) * 128],
                                                               in_=p_.rearrange("p (j c) -> p j c", j=4), func=AF.Copy)
                     if kq % 2 == 0 else
                     e.tensor_copy(out=hT[:, kq * 4:(kq + 1) * 4, t * 128:(t + 1) * 128],
                                   in_=p_.rearrange("p (j c) -> p j c", j=4)),
                     [pk], [("hT", t)])
                if router and "E" in RFLAGS:
                    S.op("act" if kq % 2 == 0 else "dve",
                         (lambda e, p_=p_, kq=kq: e.tensor_copy(out=h32[:, kq * 4:(kq + 1) * 4, :], in_=p_.rearrange("p (j c) -> p j c", j=4)))
                         if kq % 2 == 1 else
                         (lambda e, p_=p_, kq=kq: e.activation(out=h32[:, kq * 4:(kq + 1) * 4, :], in_=p_.rearrange("p (j c) -> p j c", j=4), func=AF.Copy)),
                         [pk], [("h32", kq)])
            if router and RLEVEL >= 1:
                for kk in range(KT):
                    S.op("pe", lambda e, kk=kk: e.matmul(ps[7][:, 0:36], lhsT=h32[:, kk, :], rhs=RW[:, kk, :], start=(kk == 0), stop=(kk == KT - 1)),
                         [("h32", kk // 4), "RW"], ["ps7"])
                if RLEVEL >= 2:
                    router_tile(l, t, ps[7][:, 0:36])

    HT_ALL = [("hT", t) for t in range(NT)]

    for l in range(DEPTH):
        S.barrier()
        norm_mod_T(l, "norm1_g", 0, 1)
        S.barrier()
        if l == 0 and "hT" in dbg_out:
            S.dma("qsp", dbg_out["hT"].rearrange("(k p) t -> p k t", p=128), hT, reads=HT_ALL, writes=["dbg"])
        if stop == "P1":
            S.finish(["dbg"])
            return nc

        cs = sb("cs", [128, 2, 256]) if l == 0 else cs
        po = [sb(f"po{i}", [128, 512]) for i in range(2)] if l == 0 else po
        rt = [sb(f"rt{i}", [128, 512]) for i in range(2)] if l == 0 else rt
        n_evac = 0
        for cb in range(INW // 512):
            W = big[cb % 2]
            wk = f"big{cb % 2}"
            Wb = wbf[cb % 2]
            wbk = f"wbf{cb % 2}"
            S.dma("qsp" if cb % 2 == 0 else "qpool", W,
                  I["w_in"][l, :, cb * 512:(cb + 1) * 512].rearrange("(k p) c -> p k c", p=128), writes=[wk])
            S.op("act", lambda e, W=W, Wb=Wb: e.activation(out=Wb[:, 0:8, :], in_=W[:, 0:8, :], func=AF.Copy), [wk], [wbk])
            S.op("pool", lambda e, W=W, Wb=Wb: e.tensor_copy(out=Wb[:, 8:16, :], in_=W[:, 8:16, :]), [wk], [wbk])
            for t in range(NT):
                p_ = ps[3 + t % 2]
                pk = f"ps{3 + t % 2}"
                for kk in range(KT):
                    S.op("pe", lambda e, p_=p_, kk=kk, t=t, Wb=Wb: e.matmul(p_, lhsT=hT[:, kk, t * 128:(t + 1) * 128],
                                                                            rhs=Wb[:, kk, :], start=(kk == 0), stop=(kk == KT - 1)),
                         [("hT", t), wbk], [pk])
                o_ = po[n_evac % 2]
                ok = f"po{n_evac % 2}"
                n_evac += 1
                is_qk = cb < 4
                is_k = cb in (2, 3)
                if is_qk and t >= 2:
                    lt = t - 2
                    S.dma("qsp", cs[:, 0, :], I["rope_cos"][lt * 128:(lt + 1) * 128, :], writes=["cs0"])
                    S.dma("qpool", cs[:, 1, :], I["rope_sin"][lt * 128:(lt + 1) * 128, :], writes=["cs1"])
                    pv = p_.rearrange("p (m two) -> p m two", two=2)
                    ov = o_.rearrange("p (m two) -> p m two", two=2)
                    r0 = rt[0].rearrange("p (m two) -> p m two", two=2)
                    r1 = rt[1].rearrange("p (m two) -> p m two", two=2)
                    sc = (128 ** -0.5) if is_k else 1.0
                    S.op("dve", lambda e, pv=pv, r0=r0: e.tensor_tensor(out=r0[:, :, 0], in0=pv[:, :, 0], in1=cs[:, 0, :], op=ALU.mult),
                         [pk, "cs0"], ["rt0a"])
                    S.op("dve", lambda e, pv=pv, r0=r0: e.tensor_tensor(out=r0[:, :, 1], in0=pv[:, :, 1], in1=cs[:, 1, :], op=ALU.mult),
                         [pk, "cs1"], ["rt0b"])
                    S.op("dve", lambda e, pv=pv, r1=r1: e.tensor_tensor(out=r1[:, :, 0], in0=pv[:, :, 0], in1=cs[:, 1, :], op=ALU.mult),
                         [pk, "cs1"], ["rt1a"])
                    S.op("dve", lambda e, pv=pv, r1=r1: e.tensor_tensor(out=r1[:, :, 1], in0=pv[:, :, 1], in1=cs[:, 0, :], op=ALU.mult),
                         [pk, "cs0"], ["rt1b"])
                    S.op("pool", lambda e, ov=ov, r0=r0: e.tensor_tensor(out=ov[:, :, 0], in0=r0[:, :, 0], in1=r0[:, :, 1], op=ALU.subtract),
                         ["rt0a", "rt0b"], [ok])
                    S.op("pool", lambda e, ov=ov, r1=r1: e.tensor_tensor(out=ov[:, :, 1], in0=r1[:, :, 0], in1=r1[:, :, 1], op=ALU.add),
                         ["rt1a", "rt1b"], [ok])
                    if is_k:
                        S.op("act", lambda e, o_=o_, sc=sc: e.activation(out=o_, in_=o_, func=AF.Copy, scale=sc), [ok], [ok])
                elif is_k:
                    S.op("act", lambda e, o_=o_, p_=p_: e.activation(out=o_, in_=p_, func=AF.Copy, scale=128 ** -0.5), [pk], [ok])
                else:
                    S.op("act", lambda e, o_=o_, p_=p_: e.activation(out=o_, in_=p_, func=AF.Copy), [pk], [ok])
                S.dma("qsp" if n_evac % 2 else "qpool", PROJ[t * 128:(t + 1) * 128, cb * 512:(cb + 1) * 512], o_,
                      reads=[ok], writes=["PROJ"])
        if l == 0 and "PROJ" in dbg_out:
            S.dma("qsp", dbg_out["PROJ"], PROJ, reads=["PROJ"], writes=["dbg"])
        if stop == "P2":
            S.finish(["dbg"])
            return nc

        S.barrier()
        a3 = Arena(nc, SB0 + 12 * 1024, SB_LIMIT)
        tg = f"L{l}"
        qf = a3("qf" + tg, [128, NT, 128]); kf = a3("kf" + tg, [128, NT, 128])
        vf = a3("vf" + tg, [128, NT, 128]); gf = a3("gf" + tg, [128, NT, 128])
        qs = a3("qs" + tg, [128, NT, 128]); ks = a3("ks" + tg, [128, NT, 128])
        qsT = a3("qsT" + tg, [128, NT * 128], BF16); ksT = a3("ksT" + tg, [128, NT * 128], BF16)
        kst = a3("kst" + tg, [128, NT, 128], BF16); vb = a3("vb" + tg, [128, NT, 128], BF16)
        RET = a3("RET" + tg, [128, NT, 128]); sq = a3("sq" + tg, [128, NT, 128])
        mF = a3("mF" + tg, [128, 128]); mB = a3("mB" + tg, [128, 128])
        scb = [a3(f"scb{i}" + tg, [128, 128], BF16) for i in range(2)]
        Sf = a3("Sf" + tg, [128, 128]); Sb = a3("Sb" + tg, [128, 128], BF16)
        DEC = a3("DEC" + tg, [128, 2, 8, 4]); lg = a3("lg" + tg, [128, 16]); POS = a3("POS" + tg, [128, 2, 4])
        stat = a3("stat" + tg, [128, 4, NT])
        S.dma("qsp", lg, I["ret_decay"][l].partition_broadcast(128), writes=["lg"])
        S.dma("qsp", POS, I["pos"], writes=["POS"])
        S.dma("qpool", mF, I["maskF"], writes=["mF"])
        S.dma("qpool", mB, I["maskB"], writes=["mB"])
        S.op("act", lambda e: e.activation(out=lg, in_=lg, func=AF.Exp), ["lg"], ["lg"])
        S.op("dve", lambda e: e.tensor_scalar(out=lg, in0=lg, scalar1=-1.0, scalar2=None, op0=ALU.mult), ["lg"], ["lg"])
        for d_ in range(2):
            for h in range(8):
                S.op("act", lambda e, d_=d_, h=h: e.activation(out=DEC[:, d_, h, :], in_=POS[:, d_, :], func=AF.Exp,
                                                               scale=lg[:, d_ * 8 + h:d_ * 8 + h + 1]),
                     ["lg", "POS"], ["DEC"])
        order_f = list(range(NT))
        order_b = [1, 0] + list(range(NT - 1, 1, -1))
        for h in range(8):
            def hv(off):
                return PROJ[:, off + h * 128: off + (h + 1) * 128].rearrange("(t p) c -> p t c", p=128)
            S.dma("qsp", qf, hv(0), reads=["PROJ"], writes=["qf"])
            S.dma("qpool", kf, hv(1024), reads=["PROJ"], writes=["kf"])
            S.dma("qsp", vf, hv(2048), reads=["PROJ"], writes=["vf"])
            S.dma("qpool", gf, hv(3072), reads=["PROJ"], writes=["gf"])
            S.op("pool", lambda e: e.tensor_copy(out=vb, in_=vf), ["vf"], ["vb"])
            for d_ in range(2):
                mk, mkk = (mF, "mF") if d_ == 0 else (mB, "mB")
                S.op("dve", lambda e, d_=d_, h=h: e.tensor_scalar(out=qs, in0=qf, scalar1=DEC[:, d_, h, 0:1], scalar2=None, op0=ALU.mult),
                     ["qf", "DEC"], ["qs"])
                S.op("pool", lambda e, d_=d_, h=h: e.tensor_scalar(out=ks, in0=kf, scalar1=DEC[:, d_, h, 1:2], scalar2=None, op0=ALU.mult),
                     ["kf", "DEC"], ["ks"])
                S.op("act", lambda e, d_=d_, h=h: e.activation(out=kst, in_=kf, func=AF.Copy, scale=DEC[:, d_, h, 2:3]),
                     ["kf", "DEC"], ["kst"])
                for src, srck, dst, dstk, pb in ((qs, "qs", qsT, "qsT", 0), (ks, "ks", ksT, "ksT", 1)):
                    for t0 in range(0, NT, 4):
                        n4 = min(4, NT - t0)
                        for j in range(n4):
                            S.op("pe", lambda e, src=src, t0=t0, j=j, pb=pb: e.transpose(ps[pb][:, j * 128:(j + 1) * 128], src[:, t0 + j, :], ident),
                                 [srck, "ident"], [f"ps{pb}"])
                        if pb == 0:
                            S.op("act", lambda e, dst=dst, t0=t0, n4=n4, pb=pb: e.activation(out=dst[:, t0 * 128:(t0 + n4) * 128], in_=ps[pb][:, 0:n4 * 128], func=AF.Copy),
                                 [f"ps{pb}"], [dstk])
                        else:
                            S.op("dve", lambda e, dst=dst, t0=t0, n4=n4, pb=pb: e.tensor_copy(out=dst[:, t0 * 128:(t0 + n4) * 128], in_=ps[pb][:, 0:n4 * 128]),
                                 [f"ps{pb}"], [dstk])
                S.op("dve", lambda e: e.memset(Sf, 0.0), [], ["Sf"])
                S.op("pool", lambda e: e.memset(Sb, 0.0), [], ["Sb"])
                for ci, t in enumerate(order_f if d_ == 0 else order_b):
                    sl = slice(t * 128, (t + 1) * 128)
                    pS = ps[2 + ci % 2]; pSk = f"ps{2 + ci % 2}"
                    pO = ps[4 + ci % 2]; pOk = f"ps{4 + ci % 2}"
                    sc_ = scb[ci % 2]; sck = f"scb{ci % 2}"
                    S.op("pe", lambda e, pS=pS, sl=sl: e.matmul(pS[:, 0:128], lhsT=ksT[:, sl], rhs=qsT[:, sl], start=True, stop=True),
                         ["ksT", "qsT"], [pSk])
                    S.op("dve", lambda e, pS=pS, sc_=sc_, mk=mk: e.tensor_tensor(out=sc_, in0=pS[:, 0:128], in1=mk, op=ALU.mult),
                         [pSk, mkk], [sck])
                    S.op("pe", lambda e, pO=pO, sc_=sc_, t=t: e.matmul(pO[:, 0:128], lhsT=sc_, rhs=vb[:, t, :], start=True, stop=False),
                         [sck, "vb"], [pOk])
                    S.op("pe", lambda e, pO=pO, sl=sl: e.matmul(pO[:, 0:128], lhsT=qsT[:, sl], rhs=Sb, start=False, stop=True),
                         ["qsT", "Sb"], [pOk])
                    if d_ == 0:
                        S.op("act", lambda e, pO=pO, t=t: e.activation(out=RET[:, t, :], in_=pO[:, 0:128], func=AF.Copy),
                             [pOk], [("RET", t)])
                    else:
                        S.op("dve", lambda e, pO=pO, t=t: e.tensor_tensor(out=RET[:, t, :], in0=pO[:, 0:128], in1=RET[:, t, :], op=ALU.add),
                             [pOk, ("RET", t)], [("RET", t)])
                    S.op("pe", lambda e, t=t: e.matmul(ps[6][:, 0:128], lhsT=kst[:, t, :], rhs=vb[:, t, :], start=True, stop=True),
                         ["kst", "vb"], ["ps6"])
                    S.op("dve", lambda e, d_=d_, h=h: e.scalar_tensor_tensor(out=Sf, in0=Sf, scalar=DEC[:, d_, h, 3:4], in1=ps[6][:, 0:128],
                                                                             op0=ALU.mult, op1=ALU.add),
                         ["Sf", "ps6", "DEC"], ["Sf"])
                    S.op("act", lambda e: e.activation(out=Sb, in_=Sf, func=AF.Copy), ["Sf"], ["Sb"])
            RALL = [("RET", t) for t in range(NT)]
            S.op("dve", lambda e: e.tensor_reduce(out=stat[:, 0, :], in_=RET, axis=AX.X, op=ALU.add), RALL, ["st0"])
            S.op("pool", lambda e: e.tensor_tensor(out=sq, in0=RET, in1=RET, op=ALU.mult), RALL, ["sq"])
            S.op("dve", lambda e: e.tensor_reduce(out=stat[:, 1, :], in_=sq, axis=AX.X, op=ALU.add), ["sq"], ["st1"])
            S.op("dve", lambda e: e.tensor_scalar(out=stat[:, 0, :], in0=stat[:, 0, :], scalar1=1.0 / 128, scalar2=None, op0=ALU.mult), ["st0"], ["st0"])
            S.op("dve", lambda e: e.tensor_tensor(out=stat[:, 2, :], in0=stat[:, 0, :], in1=stat[:, 0, :], op=ALU.mult), ["st0"], ["st2"])
            S.op("dve", lambda e: e.scalar_tensor_tensor(out=stat[:, 1, :], in0=stat[:, 1, :], scalar=1.0 / 128, in1=stat[:, 2, :],
                                                         op0=ALU.mult, op1=ALU.subtract), ["st1", "st2"], ["st1"])
            S.op("act", lambda e: e.activation(out=stat[:, 1, :], in_=stat[:, 1, :], func=AF.Sqrt, bias=eps_t[:, 1:2]), ["st1", "eps"], ["st1"])
            S.op("dve", lambda e: e.reciprocal(out=stat[:, 3, :], in_=stat[:, 1, :]), ["st1"], ["st3"])
            S.op("act", lambda e: e.activation(out=gf, in_=gf, func=AF.Silu), ["gf"], ["gf"])
            for t in range(NT):
                S.op("dve", lambda e, t=t: e.tensor_scalar(out=RET[:, t, :], in0=RET[:, t, :], scalar1=stat[:, 0, t:t + 1],
                                                           scalar2=stat[:, 3, t:t + 1], op0=ALU.subtract, op1=ALU.mult),
                     [("RET", t), "st0", "st3"], [("RET", t)])
            S.op("pool", lambda e: e.tensor_tensor(out=RET, in0=RET, in1=gf, op=ALU.mult), RALL + ["gf"], RALL)
            S.dma("qsp", MIX[:, h * 128:(h + 1) * 128].rearrange("(t p) c -> p t c", p=128), RET, reads=RALL, writes=["MIX"])
        if l == 0 and "MIX" in dbg_out:
            S.dma("qsp", dbg_out["MIX"], MIX, reads=["MIX"], writes=["dbg"])
        if stop == "P3":
            S.finish(["dbg"])
            return nc

        S.barrier()
        a4 = Arena(nc, SB0 + 12 * 1024, SB_LIMIT)
        tg = f"s5L{l}"
        def A4(n, shp, dt=F32):
            return a4(n + tg, shp, dt)
        NJ = 4
        BbT = A4("BbT", [32, 32, NJ, 2, 128], BF16)
        BUX = A4("BUX", [128, 3, 32, 128])
        BUr = BUX[:, 0]; BUi = BUX[:, 1]
        ublk = A4("ublk", [128, 1024]); ysb = A4("ysb", [128, 1024]); dsk = A4("dsk", [128, 1024])
        uTe = A4("uTe", [32, 32, 128 + 3], BF16)
        Hbr = A4("Hbr", [128, 32, 128], BF16); Hbi = A4("Hbi", [128, 32, 128], BF16)
        Cbr = A4("Cbr", [128, 32, 32], BF16); Cbi = A4("Cbi", [128, 32, 32], BF16)
        LR4 = A4("LR4", [128, 2, 32, 4]); LI4 = A4("LI4", [128, 2, 32, 4])
        Hc = A4("Hc", [128, 3, 32, 4]); T1 = A4("T1", [128, 2, 32, 4]); T2 = A4("T2", [128, 2, 32, 4])
        pm = {n: A4(n, [128, 32]) for n in ("ar", "ai", "dt", "xr", "ang", "mag", "c", "s", "lr", "li", "nr", "den",
                                            "fr", "fi", "nfi", "t1", "t2", "t3", "nli", "l4r", "l4i", "nl4i")}
        hpi = A4("hpi", [128, 1])
        BZr = A4("BZr", [128, 32, 32]); BZi = A4("BZi", [128, 32, 32])
        Bbr = A4("Bbr", [128, 32, 32]); Bbi = A4("Bbi", [128, 32, 32]); Bt = A4("Bt", [128, 32, 32])
        CZr = A4("CZr", [128, 32, 32]); CZi = A4("CZi", [128, 32, 32])
        S.op("dve", lambda e: e.memset(hpi, float(np.pi / 2)), [], ["hpi"])
        S.dma("qsp", dsk, I["s5_d"][l].partition_broadcast(128), writes=["dsk"])

        def tt_(eng, out, a, b, op, rk, wk):
            S.op(eng, lambda e: e.tensor_tensor(out=out, in0=a, in1=b, op=op), rk, wk)

        for d_ in range(2):
            S.dma("qsp", pm["ar"], I["s5_ar"][l, d_], writes=["p.ar"])
            S.dma("qpool", pm["ai"], I["s5_ai"][l, d_], writes=["p.ai"])
            S.dma("qsp", pm["dt"], I["s5_ls"][l, d_], writes=["p.dt"])
            S.dma("qsp", BZr, I["s5_bzr"][l, d_], writes=["BZr"])
            S.dma("qpool", BZi, I["s5_bzi"][l, d_], writes=["BZi"])
            S.dma("qsp", CZr, I["s5_czr"][l, d_], writes=["CZr"])
            S.dma("qpool", CZi, I["s5_czi"][l, d_], writes=["CZi"])
            P = pm
            S.op("act", lambda e: e.activation(out=P["dt"], in_=P["dt"], func=AF.Exp), ["p.dt"], ["p.dt"])
            tt_("dve", P["xr"], P["ar"], P["dt"], ALU.mult, ["p.ar", "p.dt"], ["p.xr"])
            tt_("dve", P["ang"], P["ai"], P["dt"], ALU.mult, ["p.ai", "p.dt"], ["p.ang"])
            S.op("act", lambda e: e.activation(out=P["mag"], in_=P["xr"], func=AF.Exp), ["p.xr"], ["p.mag"])
            S.op("act", lambda e: e.activation(out=P["s"], in_=P["ang"], func=AF.Sin, scale=1.0 / 16), ["p.ang"], ["p.s"])
            S.op("act", lambda e: e.activation(out=P["c"], in_=P["ang"], func=AF.Sin, scale=-1.0 / 16, bias=hpi[:, 0:1]), ["p.ang", "hpi"], ["p.c"])
            for _ in range(4):
                tt_("dve", P["t1"], P["c"], P["c"], ALU.mult, ["p.c"], ["p.t1"])
                tt_("dve", P["t2"], P["s"], P["s"], ALU.mult, ["p.s"], ["p.t2"])
                tt_("dve", P["t3"], P["c"], P["s"], ALU.mult, ["p.c", "p.s"], ["p.t3"])
                tt_("dve", P["c"], P["t1"], P["t2"], ALU.subtract, ["p.t1", "p.t2"], ["p.c"])
                tt_("dve", P["s"], P["t3"], P["t3"], ALU.add, ["p.t3"], ["p.s"])
            tt_("dve", P["lr"], P["mag"], P["c"], ALU.mult, ["p.mag", "p.c"], ["p.lr"])
            tt_("dve", P["li"], P["mag"], P["s"], ALU.mult, ["p.mag", "p.s"], ["p.li"])
            S.op("dve", lambda e: e.tensor_scalar(out=P["nli"], in0=P["li"], scalar1=-1.0, scalar2=None, op0=ALU.mult), ["p.li"], ["p.nli"])
            S.op("dve", lambda e: e.tensor_scalar(out=P["nr"], in0=P["lr"], scalar1=-1.0, scalar2=None, op0=ALU.add), ["p.lr"], ["p.nr"])
            tt_("dve", P["t1"], P["lr"], P["lr"], ALU.mult, ["p.lr"], ["p.t1"])
            tt_("dve", P["t2"], P["li"], P["li"], ALU.mult, ["p.li"], ["p.t2"])
            tt_("dve", P["t3"], P["lr"], P["li"], ALU.mult, ["p.lr", "p.li"], ["p.t3"])
            tt_("dve", P["l4r"], P["t1"], P["t2"], ALU.subtract, ["p.t1", "p.t2"], ["p.l4r"])
            tt_("dve", P["l4i"], P["t3"], P["t3"], ALU.add, ["p.t3"], ["p.l4i"])
            tt_("dve", P["t1"], P["l4r"], P["l4r"], ALU.mult, ["p.l4r"], ["p.t1"])
            tt_("dve", P["t2"], P["l4i"], P["l4i"], ALU.mult, ["p.l4i"], ["p.t2"])
            tt_("dve", P["t3"], P["l4r"], P["l4i"], ALU.mult, ["p.l4r", "p.l4i"], ["p.t3"])
            tt_("dve", P["l4r"], P["t1"], P["t2"], ALU.subtract, ["p.t1", "p.t2"], ["p.l4r"])
            tt_("dve", P["l4i"], P["t3"], P["t3"], ALU.add, ["p.t3"], ["p.l4i"])
            S.op("dve", lambda e: e.tensor_scalar(out=P["nl4i"], in0=P["l4i"], scalar1=-1.0, scalar2=None, op0=ALU.mult), ["p.l4i"], ["p.nl4i"])
            for r_ in range(4):
                for hh in range(2):
                    S.op("dve", lambda e, hh=hh, r_=r_: e.tensor_copy(out=LR4[:, hh, :, r_], in_=P["l4r"]), ["p.l4r"], ["LR4"])
                S.op("dve", lambda e, r_=r_: e.tensor_copy(out=LI4[:, 0, :, r_], in_=P["nl4i"]), ["p.nl4i"], ["LI4"])
                S.op("dve", lambda e, r_=r_: e.tensor_copy(out=LI4[:, 1, :, r_], in_=P["l4i"]), ["p.l4i"], ["LI4"])
            tt_("dve", P["t1"], P["ar"], P["ar"], ALU.mult, ["p.ar"], ["p.t1"])
            tt_("dve", P["t2"], P["ai"], P["ai"], ALU.mult, ["p.ai"], ["p.t2"])
            tt_("dve", P["den"], P["t1"], P["t2"], ALU.add, ["p.t1", "p.t2"], ["p.den"])
            S.op("dve", lambda e: e.reciprocal(out=P["den"], in_=P["den"]), ["p.den"], ["p.den"])
            tt_("dve", P["t1"], P["nr"], P["ar"], ALU.mult, ["p.nr", "p.ar"], ["p.t1"])
            tt_("dve", P["t2"], P["li"], P["ai"], ALU.mult, ["p.li", "p.ai"], ["p.t2"])
            tt_("dve", P["t1"], P["t1"], P["t2"], ALU.add, ["p.t1", "p.t2"], ["p.t1"])
            tt_("dve", P["fr"], P["t1"], P["den"], ALU.mult, ["p.t1", "p.den"], ["p.fr"])
            tt_("dve", P["t1"], P["li"], P["ar"], ALU.mult, ["p.li", "p.ar"], ["p.t1"])
            tt_("dve", P["t2"], P["nr"], P["ai"], ALU.mult, ["p.nr", "p.ai"], ["p.t2"])
            tt_("dve", P["t1"], P["t1"], P["t2"], ALU.subtract, ["p.t1", "p.t2"], ["p.t1"])
            tt_("dve", P["fi"], P["t1"], P["den"], ALU.mult, ["p.t1", "p.den"], ["p.fi"])
            S.op("dve", lambda e: e.tensor_scalar(out=P["nfi"], in0=P["fi"], scalar1=-1.0, scalar2=None, op0=ALU.mult), ["p.fi"], ["p.nfi"])
            for gh in range(32):
                S.op("dve", lambda e, gh=gh: e.tensor_scalar(out=Bbr[:, gh, :], in0=BZr[:, gh, :], scalar1=P["fr"][:, gh:gh + 1], scalar2=None, op0=ALU.mult),
                     ["BZr", "p.fr"], [("Bbr", gh)])
                S.op("dve", lambda e, gh=gh: e.scalar_tensor_tensor(out=Bbr[:, gh, :], in0=BZi[:, gh, :], scalar=P["nfi"][:, gh:gh + 1], in1=Bbr[:, gh, :],
                                                                    op0=ALU.mult, op1=ALU.add), ["BZi", "p.nfi", ("Bbr", gh)], [("Bbr", gh)])
                S.op("dve", lambda e, gh=gh: e.tensor_scalar(out=Bbi[:, gh, :], in0=BZi[:, gh, :], scalar1=P["fr"][:, gh:gh + 1], scalar2=None, op0=ALU.mult),
                     ["BZi", "p.fr"], [("Bbi", gh)])
                S.op("dve", lambda e, gh=gh: e.scalar_tensor_tensor(out=Bbi[:, gh, :], in0=BZr[:, gh, :], scalar=P["fi"][:, gh:gh + 1], in1=Bbi[:, gh, :],
                                                                     op0=ALU.mult, op1=ALU.add), ["BZr", "p.fi", ("Bbi", gh)], [("Bbi", gh)])
            for j_ in range(NJ):
                if j_ > 0:
                    for gh in range(32):
                        S.op("dve", lambda e, gh=gh: e.tensor_scalar(out=Bt[:, gh, :], in0=Bbr[:, gh, :], scalar1=P["li"][:, gh:gh + 1], scalar2=None, op0=ALU.mult),
                             [("Bbr", gh), "p.li"], [("Bt", gh)])
                        S.op("dve", lambda e, gh=gh: e.tensor_scalar(out=Bbr[:, gh, :], in0=Bbr[:, gh, :], scalar1=P["lr"][:, gh:gh + 1], scalar2=None, op0=ALU.mult),
                             [("Bbr", gh), "p.lr"], [("Bbr", gh)])
                        S.op("dve", lambda e, gh=gh: e.scalar_tensor_tensor(out=Bbr[:, gh, :], in0=Bbi[:, gh, :], scalar=P["nli"][:, gh:gh + 1], in1=Bbr[:, gh, :],
                                                                            op0=ALU.mult, op1=ALU.add), [("Bbi", gh), "p.nli", ("Bbr", gh)], [("Bbr", gh)])
                        S.op("dve", lambda e, gh=gh: e.scalar_tensor_tensor(out=Bbi[:, gh, :], in0=Bbi[:, gh, :], scalar=P["lr"][:, gh:gh + 1], in1=Bt[:, gh, :],
                                                                            op0=ALU.mult, op1=ALU.add), [("Bbi", gh), "p.lr", ("Bt", gh)], [("Bbi", gh)])
                for ri, src in enumerate((Bbr, Bbi)):
                    srck = "Bbr" if ri == 0 else "Bbi"
                    for g0 in range(0, 32, 4):
                        for j in range(4):
                            S.op("pe", lambda e, src=src, g0=g0, j=j: e.transpose(ps[0][0:32, j * 128:(j + 1) * 128], src[:, g0 + j, :], ident),
                                 [(srck, g0 + j), "ident"], ["ps0"])
                        S.op("act", lambda e, g0=g0, ri=ri, j_=j_: e.activation(out=BbT[:, g0:g0 + 4, j_, ri, :], in_=ps[0][0:32, :].rearrange("p (j c) -> p j c", j=4), func=AF.Copy),
                             ["ps0"], ["BbT"])
            S.op("act", lambda e: e.activation(out=Cbr, in_=CZr, func=AF.Copy), ["CZr"], ["Cbr"])
            S.op("act", lambda e: e.activation(out=Cbi, in_=CZi, func=AF.Copy, scale=-1.0), ["CZi"], ["Cbi"])
            S.op("dve", lambda e: e.memset(Hc, 0.0), [], ["Hca", "Hcb"])
            S.op("pool", lambda e: e.memset(uTe, 0.0), [], ["uTe"])
            order = list(range(NT)) if d_ == 0 else [1, 0] + list(range(NT - 1, 1, -1))
            c0u = 3 if d_ == 0 else 0
            for bi, t in enumerate(order):
                S.dma("qsp", ublk, PROJ[t * 128:(t + 1) * 128, 4096:5120], reads=["PROJ"], writes=["ublk"])
                if bi > 0:
                    if d_ == 0:
                        S.op("pool", lambda e: e.tensor_copy(out=uTe[:, :, 0:3], in_=uTe[:, :, 128:131]), ["uTe"], ["uTe"])
                    else:
                        S.op("pool", lambda e: e.tensor_copy(out=uTe[:, :, 128:131], in_=uTe[:, :, 0:3]), ["uTe"], ["uTe"])
                for g0 in range(0, 32, 4):
                    for j in range(4):
                        S.op("pe", lambda e, g0=g0, j=j: e.transpose(ps[1][0:32, j * 128:(j + 1) * 128], ublk[:, (g0 + j) * 32:(g0 + j + 1) * 32], ident),
                             ["ublk", "ident"], ["ps1"])
                    S.op("act", lambda e, g0=g0: e.activation(out=uTe[:, g0:g0 + 4, c0u:c0u + 128], in_=ps[1][0:32, :].rearrange("p (j c) -> p j c", j=4), func=AF.Copy),
                         ["ps1"], ["uTe"])
                for ri, dst in ((0, BUr), (1, BUi)):
                    for g0 in range(0, 32, 4):
                        pb = 2 + (g0 // 4) % 2
                        for j in range(4):
                            for j_ in range(NJ):
                                cj = (c0u - j_) if d_ == 0 else (c0u + j_)
                                S.op("pe", lambda e, pb=pb, g0=g0, j=j, ri=ri, j_=j_, cj=cj: e.matmul(ps[pb][:, j * 128:(j + 1) * 128], lhsT=BbT[:, g0 + j, j_, ri, :],
                                                                                                     rhs=uTe[:, g0 + j, cj:cj + 128], start=(j_ == 0), stop=(j_ == NJ - 1)),
                                     ["BbT", "uTe"], [f"ps{pb}"])
                        S.op("act", lambda e, pb=pb, dst=dst, g0=g0: e.activation(out=dst[:, g0:g0 + 4, :], in_=ps[pb].rearrange("p (j c) -> p j c", j=4), func=AF.Copy),
                             [f"ps{pb}"], ["BUXa", "BUXb"])
                ks = range(32) if d_ == 0 else range(31, -1, -1)
                for eng, gs, sfx in S5_SPLIT:
                    prev = Hc[:, :, gs, :]
                    pk_ = "Hc" + sfx
                    bk_ = "BUX" + sfx
                    for k_ in ks:
                        cur = BUX[:, :, gs, 4 * k_:4 * k_ + 4]
                        S.op(eng, lambda e, prev=prev, gs=gs: e.tensor_tensor(out=T1[:, :, gs, :], in0=LR4[:, :, gs, :], in1=prev[:, 0:2], op=ALU.mult),
                             [pk_, bk_, "LR4"], ["T1" + sfx])
                        S.op(eng, lambda e, prev=prev, gs=gs: e.tensor_tensor(out=T2[:, :, gs, :], in0=LI4[:, :, gs, :], in1=prev[:, 1:3], op=ALU.mult),
                             [pk_, bk_, "LI4"], ["T2" + sfx])
                        S.op(eng, lambda e, cur=cur, gs=gs: e.tensor_tensor(out=cur[:, 0:2], in0=cur[:, 0:2], in1=T1[:, :, gs, :], op=ALU.add),
                             [bk_, "T1" + sfx], [bk_])
                        S.op(eng, lambda e, cur=cur, gs=gs: e.tensor_tensor(out=cur[:, 0:2], in0=cur[:, 0:2], in1=T2[:, :, gs, :], op=ALU.add),
                             [bk_, "T2" + sfx], [bk_])
                        S.op(eng, lambda e, cur=cur: e.tensor_copy(out=cur[:, 2], in_=cur[:, 0]), [bk_], [bk_])
                        prev = cur
                    S.op(eng, lambda e, prev=prev, gs=gs: e.tensor_copy(out=Hc[:, :, gs, :], in_=prev), [bk_], [pk_])
                S.op("act", lambda e: e.activation(out=Hbr, in_=BUr, func=AF.Copy), ["BUXa", "BUXb"], ["Hbr"])
                S.op("act", lambda e: e.activation(out=Hbi, in_=BUi, func=AF.Copy), ["BUXa", "BUXb"], ["Hbi"])
                for gh in range(32):
                    pb = 4 + gh // 16
                    c0 = (gh % 16) * 32
                    S.op("pe", lambda e, pb=pb, c0=c0, gh=gh: e.matmul(ps[pb][:, c0:c0 + 32], lhsT=Hbr[:, gh, :], rhs=Cbr[:, gh, :], start=True, stop=False),
                         ["Hbr", "Cbr"], [f"ps{pb}"])
                    S.op("pe", lambda e, pb=pb, c0=c0, gh=gh: e.matmul(ps[pb][:, c0:c0 + 32], lhsT=Hbi[:, gh, :], rhs=Cbi[:, gh, :], start=False, stop=True),
                         ["Hbi", "Cbi"], [f"ps{pb}"])
                if d_ == 0:
                    S.op("pool", lambda e: e.tensor_tensor(out=ysb, in0=ublk, in1=dsk, op=ALU.mult), ["ublk", "dsk"], ["ysb"])
                else:
                    S.dma("qpool", ysb, Y5[t * 128:(t + 1) * 128, :], reads=["Y5"], writes=["ysb"])
                for hb in range(2):
                    S.op("dve", lambda e, hb=hb: e.tensor_tensor(out=ysb[:, hb * 512:(hb + 1) * 512], in0=ps[4 + hb], in1=ysb[:, hb * 512:(hb + 1) * 512], op=ALU.add),
                         [f"ps{4 + hb}", "ysb"], ["ysb"])
                S.dma("qsp", Y5[t * 128:(t + 1) * 128, :], ysb, reads=["ysb"], writes=["Y5"])
        if l == 0 and "Y5" in dbg_out:
            S.dma("qsp", dbg_out["Y5"], Y5, reads=["Y5"], writes=["dbg"])
        if stop == "P4":
            S.finish(["dbg"])
            return nc

        S.barrier()
        last = (l == DEPTH - 1)
        T0 = 2 if last else 0
        for t in range(T0, NT):
            x_ = xt[t % 2]; xk = f"xt{t % 2}"
            y_ = x_[:, 0:1024]; w_ = x_[:, 1024:2048]
            S.dma("qsp", y_, Y5[t * 128:(t + 1) * 128, :], reads=["Y5"], writes=[xk])
            S.op("dve", lambda e, y_=y_, w_=w_: e.tensor_tensor(out=w_, in0=y_, in1=y_, op=ALU.mult), [xk], [xk])
            S.op("dve", lambda e, w_=w_: e.tensor_scalar(out=w_, in0=w_, scalar1=0.044715, scalar2=1.0, op0=ALU.mult, op1=ALU.add), [xk], [xk])
            S.op("dve", lambda e, y_=y_, w_=w_: e.tensor_tensor(out=w_, in0=w_, in1=y_, op=ALU.mult), [xk], [xk])
            S.op("act", lambda e, w_=w_: e.activation(out=w_, in_=w_, func=AF.Tanh, scale=float(np.sqrt(2.0 / np.pi))), [xk], [xk])
            S.op("dve", lambda e, w_=w_: e.tensor_scalar(out=w_, in0=w_, scalar1=0.5, scalar2=0.5, op0=ALU.mult, op1=ALU.add), [xk], [xk])
            S.op("dve", lambda e, y_=y_, w_=w_: e.tensor_tensor(out=y_, in0=w_, in1=y_, op=ALU.mult), [xk], [xk])
            S.dma("qpool", Z5[t * 128:(t + 1) * 128, :], y_, reads=[xk], writes=["Z5"])
            for kq in range(2):
                p_ = ps[1 + kq]; pk = f"ps{1 + kq}"
                for j in range(4):
                    kk = kq * 4 + j
                    S.op("pe", lambda e, p_=p_, j=j, kk=kk, y_=y_: e.transpose(p_[:, j * 128:(j + 1) * 128], y_[:, kk * 128:(kk + 1) * 128], ident),
                         [xk, "ident"], [pk])
                S.op("act", lambda e, p_=p_, kq=kq, t=t: e.activation(out=hT[:, kq * 4:(kq + 1) * 4, t * 128:(t + 1) * 128],
                                                                       in_=p_.rearrange("p (j c) -> p j c", j=4), func=AF.Copy), [pk], [("hT", t)])
        S.dma("qsp", junk[:, 0:1024], I["s5_glu_b"][l].partition_broadcast(128), writes=["junk"])
        for cb in range(2):
            W = big[cb % 2]; wk = f"big{cb % 2}"; Wb = wbf[0]; wbk = "wbf0"
            S.dma("qsp", W[:, 0:8, :], I["s5_glu_w"][l, :, cb * 512:(cb + 1) * 512].rearrange("(k p) c -> p k c", p=128), writes=[wk])
            S.op("act", lambda e, W=W, Wb=Wb: e.activation(out=Wb[:, 0:8, :], in_=W[:, 0:8, :], func=AF.Copy), [wk], [wbk])
            for t in range(T0, NT):
                p_ = ps[3 + t % 2]; pk = f"ps{3 + t % 2}"
                for kk in range(8):
                    S.op("pe", lambda e, p_=p_, kk=kk, t=t, Wb=Wb: e.matmul(p_, lhsT=hT[:, kk, t * 128:(t + 1) * 128], rhs=Wb[:, kk, :],
                                                                            start=(kk == 0), stop=(kk == 7)), [("hT", t), wbk], [pk])
                o_ = po[t % 2]; ok = f"po{t % 2}"; z_ = rt[t % 2]; zk = f"rt{t % 2}"
                S.dma("qpool", z_, Z5[t * 128:(t + 1) * 128, cb * 512:(cb + 1) * 512], reads=["Z5"], writes=[zk])
                S.op("dve", lambda e, o_=o_, p_=p_, cb=cb: e.tensor_tensor(out=o_, in0=p_, in1=junk[:, cb * 512:(cb + 1) * 512], op=ALU.add), [pk, "junk"], [ok])
                S.op("act", lambda e, o_=o_: e.activation(out=o_, in_=o_, func=AF.Sigmoid), [ok], [ok])
                S.op("pool", lambda e, o_=o_, z_=z_: e.tensor_tensor(out=o_, in0=o_, in1=z_, op=ALU.mult), [ok, zk], [ok])
                S.dma("qsp", MIX[t * 128:(t + 1) * 128, 1024 + cb * 512:1024 + (cb + 1) * 512], o_, reads=[ok], writes=["MIX"])

        S.barrier()
        for t in range(T0, NT):
            x_ = xt[t % 2]; xk = f"xt{t % 2}"
            S.dma("qsp" if t % 2 == 0 else "qpool", x_, MIX[t * 128:(t + 1) * 128, :], reads=["MIX"], writes=[xk])
            for kq in range(4):
                p_ = ps[1 + kq % 2]; pk = f"ps{1 + kq % 2}"
                for j in range(4):
                    kk = kq * 4 + j
                    S.op("pe", lambda e, p_=p_, j=j, kk=kk, x_=x_: e.transpose(p_[:, j * 128:(j + 1) * 128], x_[:, kk * 128:(kk + 1) * 128], ident),
                         [xk, "ident"], [pk])
                S.op("act" if kq % 2 == 0 else "dve",
                     (lambda e, p_=p_, kq=kq, t=t: e.activation(out=hT[:, kq * 4:(kq + 1) * 4, t * 128:(t + 1) * 128], in_=p_.rearrange("p (j c) -> p j c", j=4), func=AF.Copy))
                     if kq % 2 == 0 else
                     (lambda e, p_=p_, kq=kq, t=t: e.tensor_copy(out=hT[:, kq * 4:(kq + 1) * 4, t * 128:(t + 1) * 128], in_=p_.rearrange("p (j c) -> p j c", j=4))),
                     [pk], [("hT", t)])
        S.barrier()
        for tt in range(2):
            S.dma("qsp", xt[tt], MOD[l, tt, 2 * D:3 * D].partition_broadcast(128), reads=["MOD"], writes=[f"xt{tt}"])
        for cb in range(4):
            W = big[cb % 2]; wk = f"big{cb % 2}"; Wb = wbf[0]; wbk = "wbf0"
            S.dma("qsp" if cb % 2 == 0 else "qpool", W, I["w_out"][l, :, cb * 512:(cb + 1) * 512].rearrange("(k p) c -> p k c", p=128), writes=[wk])
            S.op("act", lambda e, W=W, Wb=Wb: e.activation(out=Wb[:, 0:8, :], in_=W[:, 0:8, :], func=AF.Copy), [wk], [wbk])
            S.op("pool", lambda e, W=W, Wb=Wb: e.tensor_copy(out=Wb[:, 8:16, :], in_=W[:, 8:16, :]), [wk], [wbk])
            for t in range(T0, NT):
                tt = 0 if t < 2 else 1
                p_ = ps[3 + t % 2]; pk = f"ps{3 + t % 2}"
                for kk in range(KT):
                    S.op("pe", lambda e, p_=p_, kk=kk, t=t, Wb=Wb: e.matmul(p_, lhsT=hT[:, kk, t * 128:(t + 1) * 128], rhs=Wb[:, kk, :],
                                                                            start=(kk == 0), stop=(kk == KT - 1)), [("hT", t), wbk], [pk])
                o_ = po[t % 2]; ok = f"po{t % 2}"; z_ = rt[t % 2]; zk = f"rt{t % 2}"
                S.dma("qpool", z_, XR[t * 128:(t + 1) * 128, cb * 512:(cb + 1) * 512], reads=["XR"], writes=[zk])
                S.op("dve", lambda e, o_=o_, p_=p_, cb=cb, tt=tt: e.tensor_tensor(out=o_, in0=p_, in1=xt[tt][:, cb * 512:(cb + 1) * 512], op=ALU.mult), [pk, f"xt{tt}"], [ok])
                S.op("pool", lambda e, o_=o_, z_=z_: e.tensor_tensor(out=o_, in0=o_, in1=z_, op=ALU.add), [ok, zk], [ok])
                S.dma("qsp", XR[t * 128:(t + 1) * 128, cb * 512:(cb + 1) * 512], o_, reads=[ok], writes=["XR"])
        if l == 0 and "X1" in dbg_out:
            S.dma("qsp", dbg_out["X1"], XR, reads=["XR"], writes=["dbg"])
        if stop == "P5":
            S.finish(["dbg"])
            return nc

        S.barrier()
        norm_mod_T(l, "norm2_g", 3, 4, router=(RLEVEL >= 0), t_start=T0)
        if l == 0 and "GATE" in dbg_out:
            S.dma("qsp", dbg_out["GATE"], GATE, reads=["GATE"], writes=["dbg"])
        if "HT2" in dbg_out:
            S.dma("qpool", dbg_out["HT2"].rearrange("(k p) t -> p k t", p=128), hT, reads=HT_ALL, writes=["dbg"])
        if stop == "P6":
            S.finish(["dbg"])
            print("counts", S.count, "waits", S.nwaits)
            return nc

        S.barrier()
        ARB = SB0 + 12 * 1024
        wbf2 = nc.alloc_sbuf_tensor_at(f"wbf2L{l}", [128, KT, 512], BF16, offset=ARB + KT * NTOK * 2).ap()
        GT = pers(f"GT{l}", [128, NT, 32])
        S.dma("qsp", GT, GATE.rearrange("(t p) e -> p t e", p=128), reads=["GATE"], writes=["GT"])
        aTt = [rt[0].bitcast(BF16), rt[1].bitcast(BF16)]
        for ex in range(32):
            S.dma("qsp", big[0], I["exp_w_gate"][l, ex].rearrange("(k p) c -> p k c", p=128), writes=["big0"])
            S.dma("qpool", big[1], I["exp_w_up"][l, ex].rearrange("(k p) c -> p k c", p=128), writes=["big1"])
            S.op("act", lambda e: e.activation(out=wbf[0], in_=big[0], func=AF.Copy), ["big0"], ["wbf0"])
            S.op("dve", lambda e: e.tensor_copy(out=wbf2, in_=big[1]), ["big1"], ["wbf2"])
            for t in range(T0, NT):
                pG = ps[2 + t % 2]; pGk = f"ps{2 + t % 2}"; pU = ps[4 + t % 2]; pUk = f"ps{4 + t % 2}"
                for kk in range(KT):
                    S.op("pe", lambda e, pG=pG, kk=kk, t=t: e.matmul(pG, lhsT=hT[:, kk, t * 128:(t + 1) * 128], rhs=wbf[0][:, kk, :],
                                                                     start=(kk == 0), stop=(kk == KT - 1)), [("hT", t), "wbf0"], [pGk])
                for kk in range(KT):
                    S.op("pe", lambda e, pU=pU, kk=kk, t=t: e.matmul(pU, lhsT=hT[:, kk, t * 128:(t + 1) * 128], rhs=wbf2[:, kk, :],
                                                                     start=(kk == 0), stop=(kk == KT - 1)), [("hT", t), "wbf2"], [pUk])
                o_ = po[t % 2]; ok = f"po{t % 2}"
                S.op("act", lambda e, o_=o_, pG=pG: e.activation(out=o_, in_=pG, func=AF.Silu), [pGk], [ok])
                S.op("dve", lambda e, o_=o_, pU=pU, t=t, ex=ex: e.scalar_tensor_tensor(out=o_, in0=o_, scalar=GT[:, t, ex:ex + 1], in1=pU,
                                                                                       op0=ALU.mult, op1=ALU.mult), [ok, pUk, "GT"], [ok])
                for j in range(4):
                    S.op("pe", lambda e, o_=o_, j=j: e.transpose(ps[6][:, j * 128:(j + 1) * 128], o_[:, j * 128:(j + 1) * 128], ident), [ok, "ident"], ["ps6"])
                a_ = aTt[t % 2][:, 0:512]; ak = f"rt{t % 2}"
                S.op("act", lambda e, a_=a_: e.activation(out=a_, in_=ps[6], func=AF.Copy), ["ps6"], [ak])
                S.dma("qsp" if t % 2 == 0 else "qpool", ATd[ex, :, :, t * 128:(t + 1) * 128], a_.rearrange("p (k c) -> p k c", k=4), reads=[ak], writes=["ATd"])
        S.barrier()
        acc = nc.alloc_sbuf_tensor_at(f"accL{l}", [128, NT, 2, 512], F32, offset=ARB).ap()
        RB0 = ARB + KT * NTOK * 2 + 2 * D * 4
        ATe = [nc.alloc_sbuf_tensor_at(f"ATe{i}L{l}", [128, 4, NTOK], BF16, offset=RB0 + i * 18432).ap() for i in range(2)]
        Wd32 = [nc.alloc_sbuf_tensor_at(f"Wd32_{i}L{l}", [128, 4, 1024], F32, offset=RB0 + 36864 + i * 16384).ap() for i in range(2)]
        Wd16 = [nc.alloc_sbuf_tensor_at(f"Wd16_{i}L{l}", [128, 4, 1024], BF16, offset=RB0 + 36864 + 32768 + i * 8192).ap() for i in range(2)]
        for tt in range(2):
            S.dma("qsp", xt[tt], MOD[l, tt, 5 * D:6 * D].partition_broadcast(128), reads=["MOD"], writes=[f"xt{tt}"])
        for dg in range(2):
            S.op("dve", lambda e: e.memset(acc, 0.0), [], [("acc", t) for t in range(NT)])
            for ex in range(32):
                bi_ = ex % 2
                S.dma("qsp", Wd32[bi_], I["exp_w_down"][l, ex, :, dg * 1024:(dg + 1) * 1024].rearrange("(k p) c -> p k c", p=128), writes=[f"Wd32_{bi_}"])
                S.op("act", lambda e, bi_=bi_: e.activation(out=Wd16[bi_], in_=Wd32[bi_], func=AF.Copy), [f"Wd32_{bi_}"], [f"Wd16_{bi_}"])
                S.dma("qpool", ATe[bi_], ATd[ex], reads=["ATd"], writes=[f"ATe{bi_}"])
                for t in range(T0, NT):
                    for hb in range(2):
                        p_ = ps[2 + (2 * t + hb) % 4]; pk = f"ps{2 + (2 * t + hb) % 4}"
                        for kk in range(4):
                            S.op("pe", lambda e, p_=p_, kk=kk, t=t, hb=hb, bi_=bi_: e.matmul(p_, lhsT=ATe[bi_][:, kk, t * 128:(t + 1) * 128], rhs=Wd16[bi_][:, kk, hb * 512:(hb + 1) * 512],
                                                                                         start=(kk == 0), stop=(kk == 3)), [f"ATe{bi_}", f"Wd16_{bi_}"], [pk])
                        S.op("dve", lambda e, p_=p_, t=t, hb=hb: e.tensor_tensor(out=acc[:, t, hb, :], in0=p_, in1=acc[:, t, hb, :], op=ALU.add), [pk, ("acc", t)], [("acc", t)])
            for t in range(T0, NT):
                tt = 0 if t < 2 else 1
                for hb in range(2):
                    dc = dg * 2 + hb
                    o_ = po[hb]; ok = f"po{hb}"
                    S.dma("qpool", o_, XR[t * 128:(t + 1) * 128, dc * 512:(dc + 1) * 512], reads=["XR"], writes=[ok])
                    S.op("dve", lambda e, t=t, tt=tt, dc=dc, hb=hb: e.tensor_tensor(out=acc[:, t, hb, :], in0=acc[:, t, hb, :], in1=xt[tt][:, dc * 512:(dc + 1) * 512], op=ALU.mult),
                         [("acc", t), f"xt{tt}"], [("acc", t)])
                    S.op("pool", lambda e, o_=o_, t=t, hb=hb: e.tensor_tensor(out=o_, in0=o_, in1=acc[:, t, hb, :], op=ALU.add), [ok, ("acc", t)], [ok])
                    S.dma("qsp", XR[t * 128:(t + 1) * 128, dc * 512:(dc + 1) * 512], o_, reads=[ok], writes=["XR"])
        if l == 0 and "X2" in dbg_out:
            S.dma("qsp", dbg_out["X2"], XR, reads=["XR"], writes=["dbg"])
        if stop == "P7":
            S.finish(["dbg"])
            return nc

    S.barrier()
    S.dma("qsp", junk, I["final_g"].partition_broadcast(128), writes=["junk"])
    for t in range(2, NT):
        x_ = xt[t % 2]; xk = f"xt{t % 2}"
        S.dma("qsp" if t % 2 == 0 else "qpool", x_, XR[t * 128:(t + 1) * 128, :], reads=["XR"], writes=[xk])
        ss = small[:, 0:1]
        S.op("act", lambda e, x_=x_: e.activation(out=big[1][:, 0:4, :].rearrange("p a b -> p (a b)"), in_=x_, func=AF.Square, accum_out=ss), [xk], ["sqj", "ss"])
        S.op("act", lambda e: e.activation(out=small[:, 1:2], in_=ss, func=AF.Sqrt, scale=1.0 / D, bias=eps_t[:, 0:1]), ["ss"], ["rs"])
        S.op("dve", lambda e: e.reciprocal(out=small[:, 2:3], in_=small[:, 1:2]), ["rs"], ["rstd"])
        S.op("dve", lambda e, x_=x_: e.scalar_tensor_tensor(out=x_, in0=x_, scalar=small[:, 2:3], in1=junk, op0=ALU.mult, op1=ALU.mult), [xk, "rstd", "junk"], [xk])
        S.dma("qsp", out_final[(t - 2) * 128:(t - 1) * 128, :], x_, reads=[xk], writes=["OUT"])
    S.finish(["OUT"])
    return nc


def rope_tables():
    rows = NLAT // 64
    row = np.repeat(np.arange(rows, dtype=np.float32), 64)
    col = np.tile(np.arange(64, dtype=np.float32), rows)
    n_freq = 32
    inv = (np.float32(10000.0) ** (-np.arange(n_freq, dtype=np.float32) / n_freq)).astype(np.float32)
    ang = np.concatenate([row[:, None] * inv, col[:, None] * inv], axis=-1).astype(np.float32)
    cos = np.cos(ang).astype(np.float32)
    sin = np.sin(ang).astype(np.float32)
    return np.tile(cos, (1, 4)), np.tile(sin, (1, 4))


_i = np.arange(128, dtype=np.float32)
POS_TAB = np.ascontiguousarray(np.stack([
    np.stack([_i + 1, -(_i + 1), 127 - _i, np.full(128, 128.0, np.float32)], -1),
    np.stack([128 - _i, -(128 - _i), _i, np.full(128, 128.0, np.float32)], -1)], 1).astype(np.float32))
MASK_F = (np.arange(128)[None, :] >= np.arange(128)[:, None]).astype(np.float32)
MASK_B = (np.arange(128)[:, None] > np.arange(128)[None, :]).astype(np.float32)


def s5_layouts(inputs):
    out = {}
    def rg(a):
        L_ = a.shape[0]
        return np.ascontiguousarray(a.reshape(L_, 2, 32, 2, 64).transpose(0, 1, 3, 4, 2).reshape(L_, 2, 128, 32))
    out["s5_ar"] = rg(inputs["s5_a_re"]); out["s5_ai"] = rg(inputs["s5_a_im"])
    ls = inputs["s5_log_step"]
    out["s5_ls"] = rg(np.broadcast_to(ls[..., None], ls.shape + (64,)))
    def bz(b):
        L_ = b.shape[0]
        bb = b.reshape(L_, 2, 32, 2, 64, 16)
        z = np.zeros((L_, 2, 2, 64, 32, 2, 16), np.float32)
        for gl in range(2):
            z[:, :, gl, :, :, gl, :] = bb[:, :, :, gl].transpose(0, 1, 3, 2, 4)
        return np.ascontiguousarray(z.reshape(L_, 2, 128, 32, 32))
    out["s5_bzr"] = bz(inputs["s5_b_re"]); out["s5_bzi"] = bz(inputs["s5_b_im"])
    out["s5_czr"] = bz(inputs["s5_c_re"].transpose(0, 1, 2, 4, 3)); out["s5_czi"] = bz(inputs["s5_c_im"].transpose(0, 1, 2, 4, 3))
    out["s5_d"] = np.ascontiguousarray(inputs["s5_d"].reshape(DEPTH, 1024))
    return out


def make_in_maps(inputs, nb=4):
    cos4, sin4 = rope_tables()
    s5l = s5_layouts(inputs)
    rwc = np.concatenate([inputs["router_grp_w"], inputs["router_exp_w"]], -1)
    RW_ = np.ascontiguousarray(rwc.reshape(DEPTH, KT, 128, 36).transpose(0, 2, 1, 3))
    RB_ = np.ascontiguousarray(np.concatenate([inputs["router_grp_b"], inputs["router_exp_b"]], -1))
    maps = []
    for b in range(nb):
        cc = np.stack([inputs["c_ctx"], inputs["c"][b]], 0)
        ccT = np.ascontiguousarray(cc.reshape(2, KT, 128).transpose(2, 1, 0))
        m = {
            "xin": np.ascontiguousarray(inputs["x"][b]),
            "ctxin": np.ascontiguousarray(inputs["ctx"][b]),
            "ccT": ccT,
            "ada_w": inputs["ada_w"], "ada_b": inputs["ada_b"],
            "norm1_g": inputs["norm1_g"], "w_in": inputs["w_in"],
            "ident": np.eye(128, dtype=np.float32),
            "rope_cos": cos4, "rope_sin": sin4,
            "ret_decay": np.ascontiguousarray(inputs["ret_decay"].reshape(DEPTH, 16)),
            "pos": POS_TAB, "maskF": MASK_F, "maskB": MASK_B,
            **s5l,
            "s5_glu_w": inputs["s5_glu_w"], "s5_glu_b": inputs["s5_glu_b"], "w_out": inputs["w_out"],
            "norm2_g": inputs["norm2_g"], "final_g": inputs["final_g"],
            "rw": RW_, "rb": RB_,
            "exp_w_gate": inputs["exp_w_gate"], "exp_w_up": inputs["exp_w_up"], "exp_w_down": inputs["exp_w_down"],
        }
        maps.append(m)
    return maps


def kernel(**inputs):
    inputs = {k_: np.asarray(v) for k_, v in inputs.items()}
    nc = build()
    maps = make_in_maps(inputs)
    res = run_bass_kernel_spmd(nc, maps, core_ids=list(range(4)))
    return np.stack([r["out"] for r in res.results], 0)
```

```python
import numpy as np
import concourse.bass as bass
import concourse.mybir as mybir
from concourse.bass_utils import run_bass_kernel_spmd

F32 = mybir.dt.float32
BF16 = mybir.dt.bfloat16
AF = mybir.ActivationFunctionType
ALU = mybir.AluOpType
AX = mybir.AxisListType

D = 2048
NCTX = 256
NLAT = 2048
NTOK = NCTX + NLAT
NT = NTOK // 128
KT = D // 128
INW = 5120
DEPTH = 2
import os
RLEVEL = int(os.environ.get("RLEVEL", "9"))
RFLAGS = os.environ.get("RFLAGS", "WBE")
_sp = int(os.environ.get("S5SPLIT", "32"))
S5_SPLIT = (("dve", slice(0, _sp), "a"),) + ((("pool", slice(_sp, 32), "b"),) if _sp < 32 else ())
EPOCH = 30000
NDMASEM = 8


class Sched:
    def __init__(self, nc, same_engine_sync=("dve", "act", "pool")):
        self.nc = nc
        self.E = {"pe": nc.tensor, "dve": nc.vector, "act": nc.scalar,
                  "pool": nc.gpsimd, "sp": nc.sync}
        self.same = set(same_engine_sync)
        self.dmaq = {"qsp": "sp", "qpool": "pool", "qact": "act"}
        self.sems = {}
        self.count = {}
        self.waited = {}
        self.res = {}
        self.nwaits = 0

    def _sem(self, stream, idx):
        if stream in self.dmaq:
            key = (stream, (idx - 1) % NDMASEM)
            val = 16 * ((idx - 1) // NDMASEM + 1)
        else:
            key = (stream, (idx - 1) // EPOCH)
            val = (idx - 1) % EPOCH + 1
        if key not in self.sems:
            self.sems[key] = self.nc.alloc_semaphore(f"s_{key[0]}_{key[1]}")
        return self.sems[key], val

    def _is_waited(self, eng, stream, idx):
        w = self.waited.setdefault(eng, {})
        if stream in self.dmaq:
            return idx in w.get(stream, ())
        return w.get(stream, 0) >= idx

    def _wait(self, eng, stream, idx):
        if self._is_waited(eng, stream, idx):
            return
        if stream == eng and eng not in self.same:
            return
        h, v = self._sem(stream, idx)
        self.E[eng].wait_ge(h, v)
        self.nwaits += 1
        w = self.waited[eng]
        if stream in self.dmaq:
            w.setdefault(stream, set()).add(idx)
        else:
            w[stream] = idx

    def _deps(self, eng, reads, writes):
        need = set()
        for k in reads:
            st = self.res.get(k)
            if st and st[0]:
                need.add(st[0])
        for k in writes:
            st = self.res.get(k)
            if st:
                if st[0]:
                    need.add(st[0])
                need.update(st[1])
        mx = {}
        for s, i in need:
            if s in self.dmaq:
                self._wait(eng, s, i)
            else:
                mx[s] = max(mx.get(s, 0), i)
        for s, i in mx.items():
            self._wait(eng, s, i)

    def _record(self, stream, idx, reads, writes):
        for k in reads:
            st = self.res.setdefault(k, [None, []])
            if stream not in self.dmaq:
                st[1] = [r for r in st[1] if r[0] != stream]
            st[1].append((stream, idx))
        for k in writes:
            self.res[k] = [(stream, idx), []]

    def op(self, eng, fn, reads=(), writes=()):
        self._deps(eng, reads, writes)
        idx = self.count.get(eng, 0) + 1
        self.count[eng] = idx
        h, v = self._sem(eng, idx)
        fn(self.E[eng]).then_inc(h, 1)
        self._record(eng, idx, reads, writes)
        return idx

    def dma(self, q, out, in_, reads=(), writes=(), **kw):
        eng = self.dmaq[q]
        self._deps(eng, reads, writes)
        idx = self.count.get(q, 0) + 1
        self.count[q] = idx
        if idx > NDMASEM:
            self._wait(eng, q, idx - NDMASEM)
        h, v = self._sem(q, idx)
        self.E[eng].dma_start(out=out, in_=in_, **kw).then_inc(h, 16)
        self._record(q, idx, reads, writes)
        return idx

    def finish(self, keys, eng="sp"):
        self._deps(eng, list(keys), [])

    def barrier(self):
        for eng in self.E:
            for s_, n in list(self.count.items()):
                if n == 0:
                    continue
                if s_ in self.dmaq:
                    for i in range(max(1, n - NDMASEM + 1), n + 1):
                        self._wait(eng, s_, i)
                elif s_ != eng or eng in self.same:
                    self._wait(eng, s_, n)
        self.res = {}


class Arena:
    def __init__(self, nc, base, limit):
        self.nc, self.off, self.limit = nc, base, limit
        self.n = 0

    def __call__(self, name, shape, dt=F32):
        esz = 2 if dt == BF16 else 4
        size = esz
        for d_ in shape[1:]:
            size *= d_
        off = (self.off + 31) // 32 * 32
        self.off = off + size
        assert self.off <= self.limit, (name, self.off, self.limit)
        return self.nc.alloc_sbuf_tensor_at(name, list(shape), dt, offset=off).ap()


class K:
    pass


def build(stop=None, dbg=()):
    nc = bass.Bass("TRN2", target_bir_lowering=False)
    S = Sched(nc, same_engine_sync=tuple(x for x in os.environ.get("SAMESYNC", "dve,act,pool").split(",") if x))
    k = K()
    k.nc, k.S = nc, S

    def din(name, shape, dt=F32):
        return nc.dram_tensor(name, list(shape), dt, kind="ExternalInput").ap()

    def dscr(name, shape, dt=F32):
        return nc.dram_tensor(name, list(shape), dt, kind="Internal").ap()

    def dout(name, shape, dt=F32):
        return nc.dram_tensor(name, list(shape), dt, kind="ExternalOutput").ap()

    def sb(name, shape, dt=F32):
        return nc.alloc_sbuf_tensor(name, list(shape), dt).ap()

    I = {}
    I["xin"] = din("xin", [NLAT, D])
    I["ctxin"] = din("ctxin", [NCTX, D])
    I["ccT"] = din("ccT", [128, KT, 2])
    I["ada_w"] = din("ada_w", [DEPTH, D, 6 * D])
    I["ada_b"] = din("ada_b", [DEPTH, 6 * D])
    I["norm1_g"] = din("norm1_g", [DEPTH, D])
    I["w_in"] = din("w_in", [DEPTH, D, INW])
    I["ident"] = din("ident", [128, 128])
    I["rope_cos"] = din("rope_cos", [NLAT, 256])
    I["rope_sin"] = din("rope_sin", [NLAT, 256])
    I["ret_decay"] = din("ret_decay", [DEPTH, 16])
    I["pos"] = din("pos", [128, 2, 4])
    I["maskF"] = din("maskF", [128, 128])
    I["maskB"] = din("maskB", [128, 128])
    for nm in ("s5_ar", "s5_ai", "s5_ls"):
        I[nm] = din(nm, [DEPTH, 2, 128, 32])
    for nm in ("s5_bzr", "s5_bzi", "s5_czr", "s5_czi"):
        I[nm] = din(nm, [DEPTH, 2, 128, 32, 32])
    I["s5_d"] = din("s5_d", [DEPTH, 1024])
    I["s5_glu_w"] = din("s5_glu_w", [DEPTH, 1024, 1024])
    I["s5_glu_b"] = din("s5_glu_b", [DEPTH, 1024])
    I["w_out"] = din("w_out", [DEPTH, D, D])
    I["norm2_g"] = din("norm2_g", [DEPTH, D])
    I["final_g"] = din("final_g", [D])
    I["rw"] = din("rw", [DEPTH, 128, KT, 36])
    I["rb"] = din("rb", [DEPTH, 36])
    I["exp_w_gate"] = din("exp_w_gate", [DEPTH, 32, D, 512])
    I["exp_w_up"] = din("exp_w_up", [DEPTH, 32, D, 512])
    I["exp_w_down"] = din("exp_w_down", [DEPTH, 32, 512, D])
    out_final = dout("out", [NLAT, D])

    XR = dscr("XR", [NTOK, D])
    MOD = dscr("MOD", [DEPTH, 2, 6 * D])
    PROJ = dscr("PROJ", [NTOK, INW])
    MIX = dscr("MIX", [NTOK, D])
    Y5 = dscr("Y5", [NTOK, 1024])
    Z5 = dscr("Z5", [NTOK, 1024])
    GATE = dscr("GATE", [NTOK, 32])
    ATd = dscr("ATd", [32, 128, 4, NTOK], BF16)
    dbg_out = {}
    for name, shape in dbg:
        dbg_out[name] = dout("dbg_" + name, shape)

    SLAB = 204 * 1024
    slab = nc.alloc_sbuf_tensor("slab", [128, SLAB // 4], F32)
    SB0 = nc.lookup_mloc(slab).addr
    SB_LIMIT = SB0 + SLAB
    pers = Arena(nc, SB0, SB0 + 12 * 1024)
    ident = pers("ident_sb", [128, 128])
    small = pers("small", [128, 64])
    eps_t = pers("eps_t", [128, 2])
    S.op("dve", lambda e: e.memset(eps_t[:, 0:1], 1e-6), [], ["eps"])
    S.op("dve", lambda e: e.memset(eps_t[:, 1:2], 1e-5), [], ["eps"])
    S.dma("qsp", ident, I["ident"], writes=["ident"])

    ps = [nc.alloc_psum_tensor(f"ps{i}", [128, 512], F32).ap() for i in range(8)]

    S.dma("qsp", XR[0:NCTX, :], I["ctxin"], writes=["XR"])
    S.dma("qpool", XR[NCTX:NTOK, :], I["xin"], writes=["XR"])

    ar = Arena(nc, SB0 + 12 * 1024, SB_LIMIT)
    hT = ar("hT", [128, KT, NTOK], BF16)
    xt = [ar(f"xt{i}", [128, D]) for i in range(2)]
    big = [ar(f"big{i}", [128, KT, 512]) for i in range(2)]
    wbf = [ar(f"wbf{i}", [128, KT, 512], BF16) for i in range(1)]
    wbf.append(wbf[0])
    junk = ar("junk", [128, D])
    bc = [big[0][:, 4 * j:4 * j + 4, :].rearrange("p a b -> p (a b)") for j in range(4)]
    sb = ar

    ccT = sb("ccT_sb", [128, KT, 2])
    sT = sb("sT", [128, KT, 2])
    S.dma("qsp", ccT, I["ccT"], writes=["ccT"])
    S.op("act", lambda e: e.activation(out=sT, in_=ccT, func=AF.Silu), ["ccT"], ["sT"])
    ab = sb("ab", [2, 512])
    mo = sb("mo", [2, 512])
    for l in range(DEPTH):
        for cb in range(24):
            W = big[cb % 2]
            wk = f"big{cb % 2}"
            S.dma("qsp" if cb % 2 == 0 else "qpool", W,
                  I["ada_w"][l, :, cb * 512:(cb + 1) * 512].rearrange("(k p) c -> p k c", p=128),
                  writes=[wk])
            S.dma("qsp", ab, I["ada_b"][l, cb * 512:(cb + 1) * 512].partition_broadcast(2), writes=["ab"])
            for kk in range(KT):
                S.op("pe", lambda e, kk=kk, W=W: e.matmul(ps[0][0:2, :], lhsT=sT[:, kk, :], rhs=W[:, kk, :],
                                                           start=(kk == 0), stop=(kk == KT - 1)),
                     ["sT", wk], ["ps0"])
            S.op("dve", lambda e: e.tensor_tensor(out=mo, in0=ps[0][0:2, :], in1=ab, op=ALU.add),
                 ["ps0", "ab"], ["mo"])
            S.dma("qsp", MOD[l, :, cb * 512:(cb + 1) * 512], mo, reads=["mo"], writes=["MOD"])
    if "MOD" in dbg_out:
        S.dma("qsp", dbg_out["MOD"], MOD, reads=["MOD"], writes=["dbg"])
    if stop == "A":
        S.finish(["dbg"])
        return nc

    def load_bc(l, j, tt, which, qn="qsp"):
        S.dma(qn, bc[j], MOD[l, tt, which * D:(which + 1) * D].partition_broadcast(128),
              reads=["MOD"], writes=[f"bc{j}"])

    small2 = pers("small2", [128, 160])
    rbt = pers("rbt", [128, 36])

    def router_tile(l, t, pr):
        L = small2[:, 0:36]; M = small2[:, 40:72]; m8 = small2[:, 72:80]; G1 = small2[:, 80:112]; G2 = small2[:, 112:144]
        sc = small2[:, 144:160]
        k_ = "rt_"
        S.op("dve", lambda e: e.tensor_tensor(out=L, in0=pr, in1=rbt, op=ALU.add), ["ps7", "rbt"], [k_ + "L"])
        S.op("dve", lambda e: e.tensor_reduce(out=sc[:, 0:1], in_=L[:, 0:4], axis=AX.X, op=ALU.max), [k_ + "L"], [k_ + "gmax"])
        S.op("dve", lambda e: e.tensor_scalar(out=sc[:, 1:2], in0=sc[:, 0:1], scalar1=-1.0, scalar2=None, op0=ALU.mult), [k_ + "gmax"], [k_ + "ngmax"])
        S.op("act", lambda e: e.activation(out=sc[:, 4:8], in_=L[:, 0:4], func=AF.Exp, bias=sc[:, 1:2], accum_out=sc[:, 2:3]),
             [k_ + "L", k_ + "ngmax"], [k_ + "gsum", k_ + "e4"])
        S.op("dve", lambda e: e.reciprocal(out=sc[:, 3:4], in_=sc[:, 2:3]), [k_ + "gsum"], [k_ + "ggate"])
        S.op("dve", lambda e: e.tensor_scalar(out=sc[:, 8:12], in0=L[:, 0:4], scalar1=sc[:, 0:1], scalar2=None, op0=ALU.is_equal), [k_ + "L", k_ + "gmax"], [k_ + "oh"])
        S.op("dve", lambda e: e.tensor_scalar(out=sc[:, 8:12], in0=sc[:, 8:12], scalar1=-1.0, scalar2=1e30, op0=ALU.add, op1=ALU.mult), [k_ + "oh"], [k_ + "oh"])
        for g in range(4):
            S.op("dve", lambda e, g=g: e.tensor_scalar(out=M[:, g * 8:(g + 1) * 8], in0=L[:, 4 + g * 8:12 + g * 8], scalar1=sc[:, 8 + g:9 + g], scalar2=None, op0=ALU.add),
                 [k_ + "L", k_ + "oh"], [k_ + f"M{g}"])
        MALL = [k_ + f"M{g}" for g in range(4)]
        if RLEVEL < 3:
            return
        S.op("dve", lambda e: e.max(out=m8, in_=M), MALL, [k_ + "m8"])
        if RLEVEL < 4:
            return
        S.op("dve", lambda e: e.tensor_tensor(out=sc[:, 12:13], in0=m8[:, 1:2], in1=m8[:, 0:1], op=ALU.subtract), [k_ + "m8"], [k_ + "diff"])
        S.op("act", lambda e: e.activation(out=sc[:, 13:14], in_=sc[:, 12:13], func=AF.Exp), [k_ + "diff"], [k_ + "ed"])
        S.op("dve", lambda e: e.tensor_scalar(out=sc[:, 14:15], in0=sc[:, 13:14], scalar1=1.0, scalar2=None, op0=ALU.add), [k_ + "ed"], [k_ + "w1"])
        S.op("dve", lambda e: e.reciprocal(out=sc[:, 14:15], in_=sc[:, 14:15]), [k_ + "w1"], [k_ + "w1"])
        S.op("dve", lambda e: e.tensor_tensor(out=sc[:, 15:16], in0=sc[:, 13:14], in1=sc[:, 14:15], op=ALU.mult), [k_ + "ed", k_ + "w1"], [k_ + "w2"])
        S.op("dve", lambda e: e.tensor_tensor(out=sc[:, 14:15], in0=sc[:, 14:15], in1=sc[:, 3:4], op=ALU.mult), [k_ + "w1", k_ + "ggate"], [k_ + "w1"])
        S.op("dve", lambda e: e.tensor_tensor(out=sc[:, 15:16], in0=sc[:, 15:16], in1=sc[:, 3:4], op=ALU.mult), [k_ + "w2", k_ + "ggate"], [k_ + "w2"])
        S.op("dve", lambda e: e.tensor_scalar(out=G1, in0=M, scalar1=m8[:, 0:1], scalar2=sc[:, 14:15], op0=ALU.is_equal, op1=ALU.mult), MALL + [k_ + "m8", k_ + "w1"], [k_ + "G1"])
        S.op("dve", lambda e: e.tensor_scalar(out=G2, in0=M, scalar1=m8[:, 1:2], scalar2=sc[:, 15:16], op0=ALU.is_equal, op1=ALU.mult), MALL + [k_ + "m8", k_ + "w2"], [k_ + "G2"])
        S.op("dve", lambda e: e.tensor_tensor(out=G1, in0=G1, in1=G2, op=ALU.add), [k_ + "G1", k_ + "G2"], [k_ + "G1"])
        S.dma("qsp", GATE[t * 128:(t + 1) * 128, :], G1, reads=[k_ + "G1"], writes=["GATE"])

    def norm_mod_T(l, gname, sh_idx, sc_idx, router=False, t_start=0):
        S.dma("qsp", junk, I[gname][l].partition_broadcast(128), writes=["junk"])
        for tt in range(2):
            load_bc(l, 2 * tt, tt, sc_idx)
            load_bc(l, 2 * tt + 1, tt, sh_idx, "qpool")
            S.op("dve", lambda e, tt=tt: e.scalar_tensor_tensor(out=bc[2 * tt], in0=bc[2 * tt], scalar=1.0,
                                                                in1=junk, op0=ALU.add, op1=ALU.mult),
                 [f"bc{2 * tt}", "junk"], [f"bc{2 * tt}"])
        if router:
            h32 = big[1][:, :, 0:128]
            RW = big[1][:, :, 128:164]
            if "W" in RFLAGS:
                S.dma("qsp", RW, I["rw"][l], writes=["RW"])
            if "B" in RFLAGS:
                S.dma("qsp", rbt, I["rb"][l].partition_broadcast(128), writes=["rbt"])
        for t in range(t_start, NT):
            tt = 0 if t < 2 else 1
            x_ = xt[t % 2]
            xk = f"xt{t % 2}"
            S.dma("qsp" if t % 2 == 0 else "qpool", x_, XR[t * 128:(t + 1) * 128, :], reads=["XR"], writes=[xk])
            ss = small[:, 0:1]
            S.op("act", lambda e, x_=x_: e.activation(out=junk, in_=x_, func=AF.Square, accum_out=ss),
                 [xk], ["junk", "ss"])
            S.op("act", lambda e: e.activation(out=small[:, 1:2], in_=ss, func=AF.Sqrt, scale=1.0 / D, bias=eps_t[:, 0:1]),
                 ["ss"], ["rs"])
            S.op("dve", lambda e: e.reciprocal(out=small[:, 2:3], in_=small[:, 1:2]), ["rs"], ["rstd"])
            S.op("dve", lambda e, x_=x_, tt=tt: e.scalar_tensor_tensor(out=x_, in0=x_, scalar=small[:, 2:3],
                                                                       in1=bc[2 * tt], op0=ALU.mult, op1=ALU.mult),
                 [xk, "rstd", f"bc{2 * tt}"], [xk])
            S.op("pool", lambda e, x_=x_, tt=tt: e.tensor_tensor(out=x_, in0=x_, in1=bc[2 * tt + 1], op=ALU.add),
                 [xk, f"bc{2 * tt + 1}"], [xk])
            for kq in range(4):
                p_ = ps[1 + kq % 2]
                pk = f"ps{1 + kq % 2}"
                for j in range(4):
                    kk = kq * 4 + j
                    S.op("pe", lambda e, p_=p_, j=j, kk=kk, x_=x_: e.transpose(p_[:, j * 128:(j + 1) * 128],
                                                                               x_[:, kk * 128:(kk + 1) * 128], ident),
                         [xk, "ident"], [pk])
                S.op("act" if kq % 2 == 0 else "dve",
                     lambda e, p_=p_, kq=kq, t=t: e.activation(out=hT[:, kq * 4:(kq + 1) * 4, t * 128:(t + 1) * 128],
                                                               in_=p_.rearrange("p (j c) -> p j c", j=4), func=AF.Copy)
                     if kq % 2 == 0 else
                     e.tensor_copy(out=hT[:, kq * 4:(kq + 1) * 4, t * 128:(t + 1) * 128],
                                   in_=p_.rearrange("p (j c) -> p j c", j=4)),
                     [pk], [("hT", t)])
                if router and "E" in RFLAGS:
                    S.op("act" if kq % 2 == 0 else "dve",
                         (lambda e, p_=p_, kq=kq: e.tensor_copy(out=h32[:, kq * 4:(kq + 1) * 4, :], in_=p_.rearrange("p (j c) -> p j c", j=4)))
                         if kq % 2 == 1 else
                         (lambda e, p_=p_, kq=kq: e.activation(out=h32[:, kq * 4:(kq + 1) * 4, :], in_=p_.rearrange("p (j c) -> p j c", j=4), func=AF.Copy)),
                         [pk], [("h32", kq)])
            if router and RLEVEL >= 1:
                for kk in range(KT):
                    S.op("pe", lambda e, kk=kk: e.matmul(ps[7][:, 0:36], lhsT=h32[:, kk, :], rhs=RW[:, kk, :], start=(kk == 0), stop=(kk == KT - 1)),
                         [("h32", kk // 4), "RW"], ["ps7"])
                if RLEVEL >= 2:
                    router_tile(l, t, ps[7][:, 0:36])

    HT_ALL = [("hT", t) for t in range(NT)]

    for l in range(DEPTH):
        S.barrier()
        norm_mod_T(l, "norm1_g", 0, 1)
        S.barrier()
        if l == 0 and "hT" in dbg_out:
            S.dma("qsp", dbg_out["hT"].rearrange("(k p) t -> p k t", p=128), hT, reads=HT_ALL, writes=["dbg"])
        if stop == "P1":
            S.finish(["dbg"])
            return nc

        cs = sb("cs", [128, 2, 256]) if l == 0 else cs
        po = [sb(f"po{i}", [128, 512]) for i in range(2)] if l == 0 else po
        rt = [sb(f"rt{i}", [128, 512]) for i in range(2)] if l == 0 else rt
        n_evac = 0
        for cb in range(INW // 512):
            W = big[cb % 2]
            wk = f"big{cb % 2}"
            Wb = wbf[cb % 2]
            wbk = f"wbf{cb % 2}"
            S.dma("qsp" if cb % 2 == 0 else "qpool", W,
                  I["w_in"][l, :, cb * 512:(cb + 1) * 512].rearrange("(k p) c -> p k c", p=128), writes=[wk])
            S.op("act", lambda e, W=W, Wb=Wb: e.activation(out=Wb[:, 0:8, :], in_=W[:, 0:8, :], func=AF.Copy), [wk], [wbk])
            S.op("pool", lambda e, W=W, Wb=Wb: e.tensor_copy(out=Wb[:, 8:16, :], in_=W[:, 8:16, :]), [wk], [wbk])
            for t in range(NT):
                p_ = ps[3 + t % 2]
                pk = f"ps{3 + t % 2}"
                for kk in range(KT):
                    S.op("pe", lambda e, p_=p_, kk=kk, t=t, Wb=Wb: e.matmul(p_, lhsT=hT[:, kk, t * 128:(t + 1) * 128],
                                                                            rhs=Wb[:, kk, :], start=(kk == 0), stop=(kk == KT - 1)),
                         [("hT", t), wbk], [pk])
                o_ = po[n_evac % 2]
                ok = f"po{n_evac % 2}"
                n_evac += 1
                is_qk = cb < 4
                is_k = cb in (2, 3)
                if is_qk and t >= 2:
                    lt = t - 2
                    S.dma("qsp", cs[:, 0, :], I["rope_cos"][lt * 128:(lt + 1) * 128, :], writes=["cs0"])
                    S.dma("qpool", cs[:, 1, :], I["rope_sin"][lt * 128:(lt + 1) * 128, :], writes=["cs1"])
                    pv = p_.rearrange("p (m two) -> p m two", two=2)
                    ov = o_.rearrange("p (m two) -> p m two", two=2)
                    r0 = rt[0].rearrange("p (m two) -> p m two", two=2)
                    r1 = rt[1].rearrange("p (m two) -> p m two", two=2)
                    sc = (128 ** -0.5) if is_k else 1.0
                    S.op("dve", lambda e, pv=pv, r0=r0: e.tensor_tensor(out=r0[:, :, 0], in0=pv[:, :, 0], in1=cs[:, 0, :], op=ALU.mult),
                         [pk, "cs0"], ["rt0a"])
                    S.op("dve", lambda e, pv=pv, r0=r0: e.tensor_tensor(out=r0[:, :, 1], in0=pv[:, :, 1], in1=cs[:, 1, :], op=ALU.mult),
                         [pk, "cs1"], ["rt0b"])
                    S.op("dve", lambda e, pv=pv, r1=r1: e.tensor_tensor(out=r1[:, :, 0], in0=pv[:, :, 0], in1=cs[:, 1, :], op=ALU.mult),
                         [pk, "cs1"], ["rt1a"])
                    S.op("dve", lambda e, pv=pv, r1=r1: e.tensor_tensor(out=r1[:, :, 1], in0=pv[:, :, 1], in1=cs[:, 0, :], op=ALU.mult),
                         [pk, "cs0"], ["rt1b"])
                    S.op("pool", lambda e, ov=ov, r0=r0: e.tensor_tensor(out=ov[:, :, 0], in0=r0[:, :, 0], in1=r0[:, :, 1], op=ALU.subtract),
                         ["rt0a", "rt0b"], [ok])
                    S.op("pool", lambda e, ov=ov, r1=r1: e.tensor_tensor(out=ov[:, :, 1], in0=r1[:, :, 0], in1=r1[:, :, 1], op=ALU.add),
                         ["rt1a", "rt1b"], [ok])
                    if is_k:
                        S.op("act", lambda e, o_=o_, sc=sc: e.activation(out=o_, in_=o_, func=AF.Copy, scale=sc), [ok], [ok])
                elif is_k:
                    S.op("act", lambda e, o_=o_, p_=p_: e.activation(out=o_, in_=p_, func=AF.Copy, scale=128 ** -0.5), [pk], [ok])
                else:
                    S.op("act", lambda e, o_=o_, p_=p_: e.activation(out=o_, in_=p_, func=AF.Copy), [pk], [ok])
                S.dma("qsp" if n_evac % 2 else "qpool", PROJ[t * 128:(t + 1) * 128, cb * 512:(cb + 1) * 512], o_,
                      reads=[ok], writes=["PROJ"])
        if l == 0 and "PROJ" in dbg_out:
            S.dma("qsp", dbg_out["PROJ"], PROJ, reads=["PROJ"], writes=["dbg"])
        if stop == "P2":
            S.finish(["dbg"])
            return nc

        S.barrier()
        a3 = Arena(nc, SB0 + 12 * 1024, SB_LIMIT)
        tg = f"L{l}"
        qf = a3("qf" + tg, [128, NT, 128]); kf = a3("kf" + tg, [128, NT, 128])
        vf = a3("vf" + tg, [128, NT, 128]); gf = a3("gf" + tg, [128, NT, 128])
        qs = a3("qs" + tg, [128, NT, 128]); ks = a3("ks" + tg, [128, NT, 128])
        qsT = a3("qsT" + tg, [128, NT * 128], BF16); ksT = a3("ksT" + tg, [128, NT * 128], BF16)
        kst = a3("kst" + tg, [128, NT, 128], BF16); vb = a3("vb" + tg, [128, NT, 128], BF16)
        RET = a3("RET" + tg, [128, NT, 128]); sq = a3("sq" + tg, [128, NT, 128])
        mF = a3("mF" + tg, [128, 128]); mB = a3("mB" + tg, [128, 128])
        scb = [a3(f"scb{i}" + tg, [128, 128], BF16) for i in range(2)]
        Sf = a3("Sf" + tg, [128, 128]); Sb = a3("Sb" + tg, [128, 128], BF16)
        DEC = a3("DEC" + tg, [128, 2, 8, 4]); lg = a3("lg" + tg, [128, 16]); POS = a3("POS" + tg, [128, 2, 4])
        stat = a3("stat" + tg, [128, 4, NT])
        S.dma("qsp", lg, I["ret_decay"][l].partition_broadcast(128), writes=["lg"])
        S.dma("qsp", POS, I["pos"], writes=["POS"])
        S.dma("qpool", mF, I["maskF"], writes=["mF"])
        S.dma("qpool", mB, I["maskB"], writes=["mB"])
        S.op("act", lambda e: e.activation(out=lg, in_=lg, func=AF.Exp), ["lg"], ["lg"])
        S.op("dve", lambda e: e.tensor_scalar(out=lg, in0=lg, scalar1=-1.0, scalar2=None, op0=ALU.mult), ["lg"], ["lg"])
        for d_ in range(2):
            for h in range(8):
                S.op("act", lambda e, d_=d_, h=h: e.activation(out=DEC[:, d_, h, :], in_=POS[:, d_, :], func=AF.Exp,
                                                               scale=lg[:, d_ * 8 + h:d_ * 8 + h + 1]),
                     ["lg", "POS"], ["DEC"])
        order_f = list(range(NT))
        order_b = [1, 0] + list(range(NT - 1, 1, -1))
        for h in range(8):
            def hv(off):
                return PROJ[:, off + h * 128: off + (h + 1) * 128].rearrange("(t p) c -> p t c", p=128)
            S.dma("qsp", qf, hv(0), reads=["PROJ"], writes=["qf"])
            S.dma("qpool", kf, hv(1024), reads=["PROJ"], writes=["kf"])
            S.dma("qsp", vf, hv(2048), reads=["PROJ"], writes=["vf"])
            S.dma("qpool", gf, hv(3072), reads=["PROJ"], writes=["gf"])
            S.op("pool", lambda e: e.tensor_copy(out=vb, in_=vf), ["vf"], ["vb"])
            for d_ in range(2):
                mk, mkk = (mF, "mF") if d_ == 0 else (mB, "mB")
                S.op("dve", lambda e, d_=d_, h=h: e.tensor_scalar(out=qs, in0=qf, scalar1=DEC[:, d_, h, 0:1], scalar2=None, op0=ALU.mult),
                     ["qf", "DEC"], ["qs"])
                S.op("pool", lambda e, d_=d_, h=h: e.tensor_scalar(out=ks, in0=kf, scalar1=DEC[:, d_, h, 1:2], scalar2=None, op0=ALU.mult),
                     ["kf", "DEC"], ["ks"])
                S.op("act", lambda e, d_=d_, h=h: e.activation(out=kst, in_=kf, func=AF.Copy, scale=DEC[:, d_, h, 2:3]),
                     ["kf", "DEC"], ["kst"])
                for src, srck, dst, dstk, pb in ((qs, "qs", qsT, "qsT", 0), (ks, "ks", ksT, "ksT", 1)):
                    for t0 in range(0, NT, 4):
                        n4 = min(4, NT - t0)
                        for j in range(n4):
                            S.op("pe", lambda e, src=src, t0=t0, j=j, pb=pb: e.transpose(ps[pb][:, j * 128:(j + 1) * 128], src[:, t0 + j, :], ident),
                                 [srck, "ident"], [f"ps{pb}"])
                        if pb == 0:
                            S.op("act", lambda e, dst=dst, t0=t0, n4=n4, pb=pb: e.activation(out=dst[:, t0 * 128:(t0 + n4) * 128], in_=ps[pb][:, 0:n4 * 128], func=AF.Copy),
                                 [f"ps{pb}"], [dstk])
                        else:
                            S.op("dve", lambda e, dst=dst, t0=t0, n4=n4, pb=pb: e.tensor_copy(out=dst[:, t0 * 128:(t0 + n4) * 128], in_=ps[pb][:, 0:n4 * 128]),
                                 [f"ps{pb}"], [dstk])
                S.op("dve", lambda e: e.memset(Sf, 0.0), [], ["Sf"])
                S.op("pool", lambda e: e.memset(Sb, 0.0), [], ["Sb"])
                for ci, t in enumerate(order_f if d_ == 0 else order_b):
                    sl = slice(t * 128, (t + 1) * 128)
                    pS = ps[2 + ci % 2]; pSk = f"ps{2 + ci % 2}"
                    pO = ps[4 + ci % 2]; pOk = f"ps{4 + ci % 2}"
                    sc_ = scb[ci % 2]; sck = f"scb{ci % 2}"
                    S.op("pe", lambda e, pS=pS, sl=sl: e.matmul(pS[:, 0:128], lhsT=ksT[:, sl], rhs=qsT[:, sl], start=True, stop=True),
                         ["ksT", "qsT"], [pSk])
                    S.op("dve", lambda e, pS=pS, sc_=sc_, mk=mk: e.tensor_tensor(out=sc_, in0=pS[:, 0:128], in1=mk, op=ALU.mult),
                         [pSk, mkk], [sck])
                    S.op("pe", lambda e, pO=pO, sc_=sc_, t=t: e.matmul(pO[:, 0:128], lhsT=sc_, rhs=vb[:, t, :], start=True, stop=False),
                         [sck, "vb"], [pOk])
                    S.op("pe", lambda e, pO=pO, sl=sl: e.matmul(pO[:, 0:128], lhsT=qsT[:, sl], rhs=Sb, start=False, stop=True),
                         ["qsT", "Sb"], [pOk])
                    if d_ == 0:
                        S.op("act", lambda e, pO=pO, t=t: e.activation(out=RET[:, t, :], in_=pO[:, 0:128], func=AF.Copy),
                             [pOk], [("RET", t)])
                    else:
                        S.op("dve", lambda e, pO=pO, t=t: e.tensor_tensor(out=RET[:, t, :], in0=pO[:, 0:128], in1=RET[:, t, :], op=ALU.add),
                             [pOk, ("RET", t)], [("RET", t)])
                    S.op("pe", lambda e, t=t: e.matmul(ps[6][:, 0:128], lhsT=kst[:, t, :], rhs=vb[:, t, :], start=True, stop=True),
                         ["kst", "vb"], ["ps6"])
                    S.op("dve", lambda e, d_=d_, h=h: e.scalar_tensor_tensor(out=Sf, in0=Sf, scalar=DEC[:, d_, h, 3:4], in1=ps[6][:, 0:128],
                                                                             op0=ALU.mult, op1=ALU.add),
                         ["Sf", "ps6", "DEC"], ["Sf"])
                    S.op("act", lambda e: e.activation(out=Sb, in_=Sf, func=AF.Copy), ["Sf"], ["Sb"])
            RALL = [("RET", t) for t in range(NT)]
            S.op("dve", lambda e: e.tensor_reduce(out=stat[:, 0, :], in_=RET, axis=AX.X, op=ALU.add), RALL, ["st0"])
            S.op("pool", lambda e: e.tensor_tensor(out=sq, in0=RET, in1=RET, op=ALU.mult), RALL, ["sq"])
            S.op("dve", lambda e: e.tensor_reduce(out=stat[:, 1, :], in_=sq, axis=AX.X, op=ALU.add), ["sq"], ["st1"])
            S.op("dve", lambda e: e.tensor_scalar(out=stat[:, 0, :], in0=stat[:, 0, :], scalar1=1.0 / 128, scalar2=None, op0=ALU.mult), ["st0"], ["st0"])
            S.op("dve", lambda e: e.tensor_tensor(out=stat[:, 2, :], in0=stat[:, 0, :], in1=stat[:, 0, :], op=ALU.mult), ["st0"], ["st2"])
            S.op("dve", lambda e: e.scalar_tensor_tensor(out=stat[:, 1, :], in0=stat[:, 1, :], scalar=1.0 / 128, in1=stat[:, 2, :],
                                                         op0=ALU.mult, op1=ALU.subtract), ["st1", "st2"], ["st1"])
            S.op("act", lambda e: e.activation(out=stat[:, 1, :], in_=stat[:, 1, :], func=AF.Sqrt, bias=eps_t[:, 1:2]), ["st1", "eps"], ["st1"])
            S.op("dve", lambda e: e.reciprocal(out=stat[:, 3, :], in_=stat[:, 1, :]), ["st1"], ["st3"])
            S.op("act", lambda e: e.activation(out=gf, in_=gf, func=AF.Silu), ["gf"], ["gf"])
            for t in range(NT):
                S.op("dve", lambda e, t=t: e.tensor_scalar(out=RET[:, t, :], in0=RET[:, t, :], scalar1=stat[:, 0, t:t + 1],
                                                           scalar2=stat[:, 3, t:t + 1], op0=ALU.subtract, op1=ALU.mult),
                     [("RET", t), "st0", "st3"], [("RET", t)])
            S.op("pool", lambda e: e.tensor_tensor(out=RET, in0=RET, in1=gf, op=ALU.mult), RALL + ["gf"], RALL)
            S.dma("qsp", MIX[:, h * 128:(h + 1) * 128].rearrange("(t p) c -> p t c", p=128), RET, reads=RALL, writes=["MIX"])
        if l == 0 and "MIX" in dbg_out:
            S.dma("qsp", dbg_out["MIX"], MIX, reads=["MIX"], writes=["dbg"])
        if stop == "P3":
            S.finish(["dbg"])
            return nc

        S.barrier()
        a4 = Arena(nc, SB0 + 12 * 1024, SB_LIMIT)
        tg = f"s5L{l}"
        def A4(n, shp, dt=F32):
            return a4(n + tg, shp, dt)
        NJ = 4
        BbT = A4("BbT", [32, 32, NJ, 2, 128], BF16)
        BUX = A4("BUX", [128, 3, 32, 128])
        BUr = BUX[:, 0]; BUi = BUX[:, 1]
        ublk = A4("ublk", [128, 1024]); ysb = A4("ysb", [128, 1024]); dsk = A4("dsk", [128, 1024])
        uTe = A4("uTe", [32, 32, 128 + 3], BF16)
        Hbr = A4("Hbr", [128, 32, 128], BF16); Hbi = A4("Hbi", [128, 32, 128], BF16)
        Cbr = A4("Cbr", [128, 32, 32], BF16); Cbi = A4("Cbi", [128, 32, 32], BF16)
        LR4 = A4("LR4", [128, 2, 32, 4]); LI4 = A4("LI4", [128, 2, 32, 4])
        Hc = A4("Hc", [128, 3, 32, 4]); T1 = A4("T1", [128, 2, 32, 4]); T2 = A4("T2", [128, 2, 32, 4])
        pm = {n: A4(n, [128, 32]) for n in ("ar", "ai", "dt", "xr", "ang", "mag", "c", "s", "lr", "li", "nr", "den",
                                            "fr", "fi", "nfi", "t1", "t2", "t3", "nli", "l4r", "l4i", "nl4i")}
        hpi = A4("hpi", [128, 1])
        BZr = A4("BZr", [128, 32, 32]); BZi = A4("BZi", [128, 32, 32])
        Bbr = A4("Bbr", [128, 32, 32]); Bbi = A4("Bbi", [128, 32, 32]); Bt = A4("Bt", [128, 32, 32])
        CZr = A4("CZr", [128, 32, 32]); CZi = A4("CZi", [128, 32, 32])
        S.op("dve", lambda e: e.memset(hpi, float(np.pi / 2)), [], ["hpi"])
        S.dma("qsp", dsk, I["s5_d"][l].partition_broadcast(128), writes=["dsk"])

        def tt_(eng, out, a, b, op, rk, wk):
            S.op(eng, lambda e: e.tensor_tensor(out=out, in0=a, in1=b, op=op), rk, wk)

        for d_ in range(2):
            S.dma("qsp", pm["ar"], I["s5_ar"][l, d_], writes=["p.ar"])
            S.dma("qpool", pm["ai"], I["s5_ai"][l, d_], writes=["p.ai"])
            S.dma("qsp", pm["dt"], I["s5_ls"][l, d_], writes=["p.dt"])
            S.dma("qsp", BZr, I["s5_bzr"][l, d_], writes=["BZr"])
            S.dma("qpool", BZi, I["s5_bzi"][l, d_], writes=["BZi"])
            S.dma("qsp", CZr, I["s5_czr"][l, d_], writes=["CZr"])
            S.dma("qpool", CZi, I["s5_czi"][l, d_], writes=["CZi"])
            P = pm
            S.op("act", lambda e: e.activation(out=P["dt"], in_=P["dt"], func=AF.Exp), ["p.dt"], ["p.dt"])
            tt_("dve", P["xr"], P["ar"], P["dt"], ALU.mult, ["p.ar", "p.dt"], ["p.xr"])
            tt_("dve", P["ang"], P["ai"], P["dt"], ALU.mult, ["p.ai", "p.dt"], ["p.ang"])
            S.op("act", lambda e: e.activation(out=P["mag"], in_=P["xr"], func=AF.Exp), ["p.xr"], ["p.mag"])
            S.op("act", lambda e: e.activation(out=P["s"], in_=P["ang"], func=AF.Sin, scale=1.0 / 16), ["p.ang"], ["p.s"])
            S.op("act", lambda e: e.activation(out=P["c"], in_=P["ang"], func=AF.Sin, scale=-1.0 / 16, bias=hpi[:, 0:1]), ["p.ang", "hpi"], ["p.c"])
            for _ in range(4):
                tt_("dve", P["t1"], P["c"], P["c"], ALU.mult, ["p.c"], ["p.t1"])
                tt_("dve", P["t2"], P["s"], P["s"], ALU.mult, ["p.s"], ["p.t2"])
                tt_("dve", P["t3"], P["c"], P["s"], ALU.mult, ["p.c", "p.s"], ["p.t3"])
                tt_("dve", P["c"], P["t1"], P["t2"], ALU.subtract, ["p.t1", "p.t2"], ["p.c"])
                tt_("dve", P["s"], P["t3"], P["t3"], ALU.add, ["p.t3"], ["p.s"])
            tt_("dve", P["lr"], P["mag"], P["c"], ALU.mult, ["p.mag", "p.c"], ["p.lr"])
            tt_("dve", P["li"], P["mag"], P["s"], ALU.mult, ["p.mag", "p.s"], ["p.li"])
            S.op("dve", lambda e: e.tensor_scalar(out=P["nli"], in0=P["li"], scalar1=-1.0, scalar2=None, op0=ALU.mult), ["p.li"], ["p.nli"])
            S.op("dve", lambda e: e.tensor_scalar(out=P["nr"], in0=P["lr"], scalar1=-1.0, scalar2=None, op0=ALU.add), ["p.lr"], ["p.nr"])
            tt_("dve", P["t1"], P["lr"], P["lr"], ALU.mult, ["p.lr"], ["p.t1"])
            tt_("dve", P["t2"], P["li"], P["li"], ALU.mult, ["p.li"], ["p.t2"])
            tt_("dve", P["t3"], P["lr"], P["li"], ALU.mult, ["p.lr", "p.li"], ["p.t3"])
            tt_("dve", P["l4r"], P["t1"], P["t2"], ALU.subtract, ["p.t1", "p.t2"], ["p.l4r"])
            tt_("dve", P["l4i"], P["t3"], P["t3"], ALU.add, ["p.t3"], ["p.l4i"])
            tt_("dve", P["t1"], P["l4r"], P["l4r"], ALU.mult, ["p.l4r"], ["p.t1"])
            tt_("dve", P["t2"], P["l4i"], P["l4i"], ALU.mult, ["p.l4i"], ["p.t2"])
            tt_("dve", P["t3"], P["l4r"], P["l4i"], ALU.mult, ["p.l4r", "p.l4i"], ["p.t3"])
            tt_("dve", P["l4r"], P["t1"], P["t2"], ALU.subtract, ["p.t1", "p.t2"], ["p.l4r"])
            tt_("dve", P["l4i"], P["t3"], P["t3"], ALU.add, ["p.t3"], ["p.l4i"])
            S.op("dve", lambda e: e.tensor_scalar(out=P["nl4i"], in0=P["l4i"], scalar1=-1.0, scalar2=None, op0=ALU.mult), ["p.l4i"], ["p.nl4i"])
            for r_ in range(4):
                for hh in range(2):
                    S.op("dve", lambda e, hh=hh, r_=r_: e.tensor_copy(out=LR4[:, hh, :, r_], in_=P["l4r"]), ["p.l4r"], ["LR4"])
                S.op("dve", lambda e, r_=r_: e.tensor_copy(out=LI4[:, 0, :, r_], in_=P["nl4i"]), ["p.nl4i"], ["LI4"])
                S.op("dve", lambda e, r_=r_: e.tensor_copy(out=LI4[:, 1, :, r_], in_=P["l4i"]), ["p.l4i"], ["LI4"])
            tt_("dve", P["t1"], P["ar"], P["ar"], ALU.mult, ["p.ar"], ["p.t1"])
            tt_("dve", P["t2"], P["ai"], P["ai"], ALU.mult, ["p.ai"], ["p.t2"])
            tt_("dve", P["den"], P["t1"], P["t2"], ALU.add, ["p.t1", "p.t2"], ["p.den"])
            S.op("dve", lambda e: e.reciprocal(out=P["den"], in_=P["den"]), ["p.den"], ["p.den"])
            tt_("dve", P["t1"], P["nr"], P["ar"], ALU.mult, ["p.nr", "p.ar"], ["p.t1"])
            tt_("dve", P["t2"], P["li"], P["ai"], ALU.mult, ["p.li", "p.ai"], ["p.t2"])
            tt_("dve", P["t1"], P["t1"], P["t2"], ALU.add, ["p.t1", "p.t2"], ["p.t1"])
            tt_("dve", P["fr"], P["t1"], P["den"], ALU.mult, ["p.t1", "p.den"], ["p.fr"])
            tt_("dve", P["t1"], P["li"], P["ar"], ALU.mult, ["p.li", "p.ar"], ["p.t1"])
            tt_("dve", P["t2"], P["nr"], P["ai"], ALU.mult, ["p.nr", "p.ai"], ["p.t2"])
            tt_("dve", P["t1"], P["t1"], P["t2"], ALU.subtract, ["p.t1", "p.t2"], ["p.t1"])
            tt_("dve", P["fi"], P["t1"], P["den"], ALU.mult, ["p.t1", "p.den"], ["p.fi"])
            S.op("dve", lambda e: e.tensor_scalar(out=P["nfi"], in0=P["fi"], scalar1=-1.0, scalar2=None, op0=ALU.mult), ["p.fi"], ["p.nfi"])
            for gh in range(32):
                S.op("dve", lambda e, gh=gh: e.tensor_scalar(out=Bbr[:, gh, :], in0=BZr[:, gh, :], scalar1=P["fr"][:, gh:gh + 1], scalar2=None, op0=ALU.mult),
                     ["BZr", "p.fr"], [("Bbr", gh)])
                S.op("dve", lambda e, gh=gh: e.scalar_tensor_tensor(out=Bbr[:, gh, :], in0=BZi[:, gh, :], scalar=P["nfi"][:, gh:gh + 1], in1=Bbr[:, gh, :],
                                                                    op0=ALU.mult, op1=ALU.add), ["BZi", "p.nfi", ("Bbr", gh)], [("Bbr", gh)])
                S.op("dve", lambda e, gh=gh: e.tensor_scalar(out=Bbi[:, gh, :], in0=BZi[:, gh, :], scalar1=P["fr"][:, gh:gh + 1], scalar2=None, op0=ALU.mult),
                     ["BZi", "p.fr"], [("Bbi", gh)])
                S.op("dve", lambda e, gh=gh: e.scalar_tensor_tensor(out=Bbi[:, gh, :], in0=BZr[:, gh, :], scalar=P["fi"][:, gh:gh + 1], in1=Bbi[:, gh, :],
                                                                     op0=ALU.mult, op1=ALU.add), ["BZr", "p.fi", ("Bbi", gh)], [("Bbi", gh)])
            for j_ in range(NJ):
                if j_ > 0:
                    for gh in range(32):
                        S.op("dve", lambda e, gh=gh: e.tensor_scalar(out=Bt[:, gh, :], in0=Bbr[:, gh, :], scalar1=P["li"][:, gh:gh + 1], scalar2=None, op0=ALU.mult),
                             [("Bbr", gh), "p.li"], [("Bt", gh)])
                        S.op("dve", lambda e, gh=gh: e.tensor_scalar(out=Bbr[:, gh, :], in0=Bbr[:, gh, :], scalar1=P["lr"][:, gh:gh + 1], scalar2=None, op0=ALU.mult),
                             [("Bbr", gh), "p.lr"], [("Bbr", gh)])
                        S.op("dve", lambda e, gh=gh: e.scalar_tensor_tensor(out=Bbr[:, gh, :], in0=Bbi[:, gh, :], scalar=P["nli"][:, gh:gh + 1], in1=Bbr[:, gh, :],
                                                                            op0=ALU.mult, op1=ALU.add), [("Bbi", gh), "p.nli", ("Bbr", gh)], [("Bbr", gh)])
                        S.op("dve", lambda e, gh=gh: e.scalar_tensor_tensor(out=Bbi[:, gh, :], in0=Bbi[:, gh, :], scalar=P["lr"][:, gh:gh + 1], in1=Bt[:, gh, :],
                                                                            op0=ALU.mult, op1=ALU.add), [("Bbi", gh), "p.lr", ("Bt", gh)], [("Bbi", gh)])
                for ri, src in enumerate((Bbr, Bbi)):
                    srck = "Bbr" if ri == 0 else "Bbi"
                    for g0 in range(0, 32, 4):
                        for j in range(4):
                            S.op("pe", lambda e, src=src, g0=g0, j=j: e.transpose(ps[0][0:32, j * 128:(j + 1) * 128], src[:, g0 + j, :], ident),
                                 [(srck, g0 + j), "ident"], ["ps0"])
                        S.op("act", lambda e, g0=g0, ri=ri, j_=j_: e.activation(out=BbT[:, g0:g0 + 4, j_, ri, :], in_=ps[0][0:32, :].rearrange("p (j c) -> p j c", j=4), func=AF.Copy),
                             ["ps0"], ["BbT"])
            S.op("act", lambda e: e.activation(out=Cbr, in_=CZr, func=AF.Copy), ["CZr"], ["Cbr"])
            S.op("act", lambda e: e.activation(out=Cbi, in_=CZi, func=AF.Copy, scale=-1.0), ["CZi"], ["Cbi"])
            S.op("dve", lambda e: e.memset(Hc, 0.0), [], ["Hca", "Hcb"])
            S.op("pool", lambda e: e.memset(uTe, 0.0), [], ["uTe"])
            order = list(range(NT)) if d_ == 0 else [1, 0] + list(range(NT - 1, 1, -1))
            c0u = 3 if d_ == 0 else 0
            for bi, t in enumerate(order):
                S.dma("qsp", ublk, PROJ[t * 128:(t + 1) * 128, 4096:5120], reads=["PROJ"], writes=["ublk"])
                if bi > 0:
                    if d_ == 0:
                        S.op("pool", lambda e: e.tensor_copy(out=uTe[:, :, 0:3], in_=uTe[:, :, 128:131]), ["uTe"], ["uTe"])
                    else:
                        S.op("pool", lambda e: e.tensor_copy(out=uTe[:, :, 128:131], in_=uTe[:, :, 0:3]), ["uTe"], ["uTe"])
                for g0 in range(0, 32, 4):
                    for j in range(4):
                        S.op("pe", lambda e, g0=g0, j=j: e.transpose(ps[1][0:32, j * 128:(j + 1) * 128], ublk[:, (g0 + j) * 32:(g0 + j + 1) * 32], ident),
                             ["ublk", "ident"], ["ps1"])
                    S.op("act", lambda e, g0=g0: e.activation(out=uTe[:, g0:g0 + 4, c0u:c0u + 128], in_=ps[1][0:32, :].rearrange("p (j c) -> p j c", j=4), func=AF.Copy),
                         ["ps1"], ["uTe"])
                for ri, dst in ((0, BUr), (1, BUi)):
                    for g0 in range(0, 32, 4):
                        pb = 2 + (g0 // 4) % 2
                        for j in range(4):
                            for j_ in range(NJ):
                                cj = (c0u - j_) if d_ == 0 else (c0u + j_)
                                S.op("pe", lambda e, pb=pb, g0=g0, j=j, ri=ri, j_=j_, cj=cj: e.matmul(ps[pb][:, j * 128:(j + 1) * 128], lhsT=BbT[:, g0 + j, j_, ri, :],
                                                                                                     rhs=uTe[:, g0 + j, cj:cj + 128], start=(j_ == 0), stop=(j_ == NJ - 1)),
                                     ["BbT", "uTe"], [f"ps{pb}"])
                        S.op("act", lambda e, pb=pb, dst=dst, g0=g0: e.activation(out=dst[:, g0:g0 + 4, :], in_=ps[pb].rearrange("p (j c) -> p j c", j=4), func=AF.Copy),
                             [f"ps{pb}"], ["BUXa", "BUXb"])
                ks = range(32) if d_ == 0 else range(31, -1, -1)
                for eng, gs, sfx in S5_SPLIT:
                    prev = Hc[:, :, gs, :]
                    pk_ = "Hc" + sfx
                    bk_ = "BUX" + sfx
                    for k_ in ks:
                        cur = BUX[:, :, gs, 4 * k_:4 * k_ + 4]
                        S.op(eng, lambda e, prev=prev, gs=gs: e.tensor_tensor(out=T1[:, :, gs, :], in0=LR4[:, :, gs, :], in1=prev[:, 0:2], op=ALU.mult),
                             [pk_, bk_, "LR4"], ["T1" + sfx])
                        S.op(eng, lambda e, prev=prev, gs=gs: e.tensor_tensor(out=T2[:, :, gs, :], in0=LI4[:, :, gs, :], in1=prev[:, 1:3], op=ALU.mult),
                             [pk_, bk_, "LI4"], ["T2" + sfx])
                        S.op(eng, lambda e, cur=cur, gs=gs: e.tensor_tensor(out=cur[:, 0:2], in0=cur[:, 0:2], in1=T1[:, :, gs, :], op=ALU.add),
                             [bk_, "T1" + sfx], [bk_])
                        S.op(eng, lambda e, cur=cur, gs=gs: e.tensor_tensor(out=cur[:, 0:2], in0=cur[:, 0:2], in1=T2[:, :, gs, :], op=ALU.add),
                             [bk_, "T2" + sfx], [bk_])
                        S.op(eng, lambda e, cur=cur: e.tensor_copy(out=cur[:, 2], in_=cur[:, 0]), [bk_], [bk_])
                        prev = cur
                    S.op(eng, lambda e, prev=prev, gs=gs: e.tensor_copy(out=Hc[:, :, gs, :], in_=prev), [bk_], [pk_])
                S.op("act", lambda e: e.activation(out=Hbr, in_=BUr, func=AF.Copy), ["BUXa", "BUXb"], ["Hbr"])
                S.op("act", lambda e: e.activation(out=Hbi, in_=BUi, func=AF.Copy), ["BUXa", "BUXb"], ["Hbi"])
                for gh in range(32):
                    pb = 4 + gh // 16
                    c0 = (gh % 16) * 32
                    S.op("pe", lambda e, pb=pb, c0=c0, gh=gh: e.matmul(ps[pb][:, c0:c0 + 32], lhsT=Hbr[:, gh, :], rhs=Cbr[:, gh, :], start=True, stop=False),
                         ["Hbr", "Cbr"], [f"ps{pb}"])
                    S.op("pe", lambda e, pb=pb, c0=c0, gh=gh: e.matmul(ps[pb][:, c0:c0 + 32], lhsT=Hbi[:, gh, :], rhs=Cbi[:, gh, :], start=False, stop=True),
                         ["Hbi", "Cbi"], [f"ps{pb}"])
                if d_ == 0:
                    S.op("pool", lambda e: e.tensor_tensor(out=ysb, in0=ublk, in1=dsk, op=ALU.mult), ["ublk", "dsk"], ["ysb"])
                else:
                    S.dma("qpool", ysb, Y5[t * 128:(t + 1) * 128, :], reads=["Y5"], writes=["ysb"])
                for hb in range(2):
                    S.op("dve", lambda e, hb=hb: e.tensor_tensor(out=ysb[:, hb * 512:(hb + 1) * 512], in0=ps[4 + hb], in1=ysb[:, hb * 512:(hb + 1) * 512], op=ALU.add),
                         [f"ps{4 + hb}", "ysb"], ["ysb"])
                S.dma("qsp", Y5[t * 128:(t + 1) * 128, :], ysb, reads=["ysb"], writes=["Y5"])
        if l == 0 and "Y5" in dbg_out:
            S.dma("qsp", dbg_out["Y5"], Y5, reads=["Y5"], writes=["dbg"])
        if stop == "P4":
            S.finish(["dbg"])
            return nc

        S.barrier()
        last = (l == DEPTH - 1)
        T0 = 2 if last else 0
        for t in range(T0, NT):
            x_ = xt[t % 2]; xk = f"xt{t % 2}"
            y_ = x_[:, 0:1024]; w_ = x_[:, 1024:2048]
            S.dma("qsp", y_, Y5[t * 128:(t + 1) * 128, :], reads=["Y5"], writes=[xk])
            S.op("dve", lambda e, y_=y_, w_=w_: e.tensor_tensor(out=w_, in0=y_, in1=y_, op=ALU.mult), [xk], [xk])
            S.op("dve", lambda e, w_=w_: e.tensor_scalar(out=w_, in0=w_, scalar1=0.044715, scalar2=1.0, op0=ALU.mult, op1=ALU.add), [xk], [xk])
            S.op("dve", lambda e, y_=y_, w_=w_: e.tensor_tensor(out=w_, in0=w_, in1=y_, op=ALU.mult), [xk], [xk])
            S.op("act", lambda e, w_=w_: e.activation(out=w_, in_=w_, func=AF.Tanh, scale=float(np.sqrt(2.0 / np.pi))), [xk], [xk])
            S.op("dve", lambda e, w_=w_: e.tensor_scalar(out=w_, in0=w_, scalar1=0.5, scalar2=0.5, op0=ALU.mult, op1=ALU.add), [xk], [xk])
            S.op("dve", lambda e, y_=y_, w_=w_: e.tensor_tensor(out=y_, in0=w_, in1=y_, op=ALU.mult), [xk], [xk])
            S.dma("qpool", Z5[t * 128:(t + 1) * 128, :], y_, reads=[xk], writes=["Z5"])
            for kq in range(2):
                p_ = ps[1 + kq]; pk = f"ps{1 + kq}"
                for j in range(4):
                    kk = kq * 4 + j
                    S.op("pe", lambda e, p_=p_, j=j, kk=kk, y_=y_: e.transpose(p_[:, j * 128:(j + 1) * 128], y_[:, kk * 128:(kk + 1) * 128], ident),
                         [xk, "ident"], [pk])
                S.op("act", lambda e, p_=p_, kq=kq, t=t: e.activation(out=hT[:, kq * 4:(kq + 1) * 4, t * 128:(t + 1) * 128],
                                                                       in_=p_.rearrange("p (j c) -> p j c", j=4), func=AF.Copy), [pk], [("hT", t)])
        S.dma("qsp", junk[:, 0:1024], I["s5_glu_b"][l].partition_broadcast(128), writes=["junk"])
        for cb in range(2):
            W = big[cb % 2]; wk = f"big{cb % 2}"; Wb = wbf[0]; wbk = "wbf0"
            S.dma("qsp", W[:, 0:8, :], I["s5_glu_w"][l, :, cb * 512:(cb + 1) * 512].rearrange("(k p) c -> p k c", p=128), writes=[wk])
            S.op("act", lambda e, W=W, Wb=Wb: e.activation(out=Wb[:, 0:8, :], in_=W[:, 0:8, :], func=AF.Copy), [wk], [wbk])
            for t in range(T0, NT):
                p_ = ps[3 + t % 2]; pk = f"ps{3 + t % 2}"
                for kk in range(8):
                    S.op("pe", lambda e, p_=p_, kk=kk, t=t, Wb=Wb: e.matmul(p_, lhsT=hT[:, kk, t * 128:(t + 1) * 128], rhs=Wb[:, kk, :],
                                                                            start=(kk == 0), stop=(kk == 7)), [("hT", t), wbk], [pk])
                o_ = po[t % 2]; ok = f"po{t % 2}"; z_ = rt[t % 2]; zk = f"rt{t % 2}"
                S.dma("qpool", z_, Z5[t * 128:(t + 1) * 128, cb * 512:(cb + 1) * 512], reads=["Z5"], writes=[zk])
                S.op("dve", lambda e, o_=o_, p_=p_, cb=cb: e.tensor_tensor(out=o_, in0=p_, in1=junk[:, cb * 512:(cb + 1) * 512], op=ALU.add), [pk, "junk"], [ok])
                S.op("act", lambda e, o_=o_: e.activation(out=o_, in_=o_, func=AF.Sigmoid), [ok], [ok])
                S.op("pool", lambda e, o_=o_, z_=z_: e.tensor_tensor(out=o_, in0=o_, in1=z_, op=ALU.mult), [ok, zk], [ok])
                S.dma("qsp", MIX[t * 128:(t + 1) * 128, 1024 + cb * 512:1024 + (cb + 1) * 512], o_, reads=[ok], writes=["MIX"])

        S.barrier()
        for t in range(T0, NT):
            x_ = xt[t % 2]; xk = f"xt{t % 2}"
            S.dma("qsp" if t % 2 == 0 else "qpool", x_, MIX[t * 128:(t + 1) * 128, :], reads=["MIX"], writes=[xk])
            for kq in range(4):
                p_ = ps[1 + kq % 2]; pk = f"ps{1 + kq % 2}"
                for j in range(4):
                    kk = kq * 4 + j
                    S.op("pe", lambda e, p_=p_, j=j, kk=kk, x_=x_: e.transpose(p_[:, j * 128:(j + 1) * 128], x_[:, kk * 128:(kk + 1) * 128], ident),
                         [xk, "ident"], [pk])
                S.op("act" if kq % 2 == 0 else "dve",
                     (lambda e, p_=p_, kq=kq, t=t: e.activation(out=hT[:, kq * 4:(kq + 1) * 4, t * 128:(t + 1) * 128], in_=p_.rearrange("p (j c) -> p j c", j=4), func=AF.Copy))
                     if kq % 2 == 0 else
                     (lambda e, p_=p_, kq=kq, t=t: e.tensor_copy(out=hT[:, kq * 4:(kq + 1) * 4, t * 128:(t + 1) * 128], in_=p_.rearrange("p (j c) -> p j c", j=4))),
                     [pk], [("hT", t)])
        S.barrier()
        for tt in range(2):
            S.dma("qsp", xt[tt], MOD[l, tt, 2 * D:3 * D].partition_broadcast(128), reads=["MOD"], writes=[f"xt{tt}"])
        for cb in range(4):
            W = big[cb % 2]; wk = f"big{cb % 2}"; Wb = wbf[0]; wbk = "wbf0"
            S.dma("qsp" if cb % 2 == 0 else "qpool", W, I["w_out"][l, :, cb * 512:(cb + 1) * 512].rearrange("(k p) c -> p k c", p=128), writes=[wk])
            S.op("act", lambda e, W=W, Wb=Wb: e.activation(out=Wb[:, 0:8, :], in_=W[:, 0:8, :], func=AF.Copy), [wk], [wbk])
            S.op("pool", lambda e, W=W, Wb=Wb: e.tensor_copy(out=Wb[:, 8:16, :], in_=W[:, 8:16, :]), [wk], [wbk])
            for t in range(T0, NT):
                tt = 0 if t < 2 else 1
                p_ = ps[3 + t % 2]; pk = f"ps{3 + t % 2}"
                for kk in range(KT):
                    S.op("pe", lambda e, p_=p_, kk=kk, t=t, Wb=Wb: e.matmul(p_, lhsT=hT[:, kk, t * 128:(t + 1) * 128], rhs=Wb[:, kk, :],
                                                                            start=(kk == 0), stop=(kk == KT - 1)), [("hT", t), wbk], [pk])
                o_ = po[t % 2]; ok = f"po{t % 2}"; z_ = rt[t % 2]; zk = f"rt{t % 2}"
                S.dma("qpool", z_, XR[t * 128:(t + 1) * 128, cb * 512:(cb + 1) * 512], reads=["XR"], writes=[zk])
                S.op("dve", lambda e, o_=o_, p_=p_, cb=cb, tt=tt: e.tensor_tensor(out=o_, in0=p_, in1=xt[tt][:, cb * 512:(cb + 1) * 512], op=ALU.mult), [pk, f"xt{tt}"], [ok])
                S.op("pool", lambda e, o_=o_, z_=z_: e.tensor_tensor(out=o_, in0=o_, in1=z_, op=ALU.add), [ok, zk], [ok])
                S.dma("qsp", XR[t * 128:(t + 1) * 128, cb * 512:(cb + 1) * 512], o_, reads=[ok], writes=["XR"])
        if l == 0 and "X1" in dbg_out:
            S.dma("qsp", dbg_out["X1"], XR, reads=["XR"], writes=["dbg"])
        if stop == "P5":
            S.finish(["dbg"])
            return nc

        S.barrier()
        norm_mod_T(l, "norm2_g", 3, 4, router=(RLEVEL >= 0), t_start=T0)
        if l == 0 and "GATE" in dbg_out:
            S.dma("qsp", dbg_out["GATE"], GATE, reads=["GATE"], writes=["dbg"])
        if "HT2" in dbg_out:
            S.dma("qpool", dbg_out["HT2"].rearrange("(k p) t -> p k t", p=128), hT, reads=HT_ALL, writes=["dbg"])
        if stop == "P6":
            S.finish(["dbg"])
            print("counts", S.count, "waits", S.nwaits)
            return nc

        S.barrier()
        ARB = SB0 + 12 * 1024
        wbf2 = nc.alloc_sbuf_tensor_at(f"wbf2L{l}", [128, KT, 512], BF16, offset=ARB + KT * NTOK * 2).ap()
        GT = pers(f"GT{l}", [128, NT, 32])
        S.dma("qsp", GT, GATE.rearrange("(t p) e -> p t e", p=128), reads=["GATE"], writes=["GT"])
        aTt = [rt[0].bitcast(BF16), rt[1].bitcast(BF16)]
        pending = None
        for ex in range(32):
            S.dma("qsp", big[0], I["exp_w_gate"][l, ex].rearrange("(k p) c -> p k c", p=128), writes=["big0"])
            S.dma("qpool", big[1], I["exp_w_up"][l, ex].rearrange("(k p) c -> p k c", p=128), writes=["big1"])
            S.op("act", lambda e: e.activation(out=wbf[0], in_=big[0], func=AF.Copy), ["big0"], ["wbf0"])
            S.op("dve", lambda e: e.tensor_copy(out=wbf2, in_=big[1]), ["big1"], ["wbf2"])
            for t in range(T0, NT):
                pG = ps[2 + t % 2]; pGk = f"ps{2 + t % 2}"; pU = ps[4 + t % 2]; pUk = f"ps{4 + t % 2}"
                for kk in range(KT):
                    S.op("pe", lambda e, pG=pG, kk=kk, t=t: e.matmul(pG, lhsT=hT[:, kk, t * 128:(t + 1) * 128], rhs=wbf[0][:, kk, :],
                                                                     start=(kk == 0), stop=(kk == KT - 1)), [("hT", t), "wbf0"], [pGk])
                for kk in range(KT):
                    S.op("pe", lambda e, pU=pU, kk=kk, t=t: e.matmul(pU, lhsT=hT[:, kk, t * 128:(t + 1) * 128], rhs=wbf2[:, kk, :],
                                                                     start=(kk == 0), stop=(kk == KT - 1)), [("hT", t), "wbf2"], [pUk])
                o_ = po[t % 2]; ok = f"po{t % 2}"
                S.op("act", lambda e, o_=o_, pG=pG: e.activation(out=o_, in_=pG, func=AF.Silu), [pGk], [ok])
                S.op("dve", lambda e, o_=o_, pU=pU, t=t, ex=ex: e.scalar_tensor_tensor(out=o_, in0=o_, scalar=GT[:, t, ex:ex + 1], in1=pU,
                                                                                       op0=ALU.mult, op1=ALU.mult), [ok, pUk, "GT"], [ok])
                def tail(t=t, o_=o_, ok=ok, ex=ex):
                    for j in range(4):
                        S.op("pe", lambda e, o_=o_, j=j: e.transpose(ps[6][:, j * 128:(j + 1) * 128], o_[:, j * 128:(j + 1) * 128], ident), [ok, "ident"], ["ps6"])
                    a_ = aTt[t % 2][:, 0:512]; ak = f"rt{t % 2}"
                    S.op("act", lambda e, a_=a_: e.activation(out=a_, in_=ps[6], func=AF.Copy), ["ps6"], [ak])
                    S.dma("qsp" if t % 2 == 0 else "qpool", ATd[ex, :, :, t * 128:(t + 1) * 128], a_.rearrange("p (k c) -> p k c", k=4), reads=[ak], writes=["ATd"])
                if pending is not None:
                    pending()
                pending = tail
            if pending is not None:
                pending()
                pending = None
        S.barrier()
        acc = nc.alloc_sbuf_tensor_at(f"accL{l}", [128, NT, 2, 512], F32, offset=ARB).ap()
        RB0 = ARB + KT * NTOK * 2 + 2 * D * 4
        ATe = [nc.alloc_sbuf_tensor_at(f"ATe{i}L{l}", [128, 4, NTOK], BF16, offset=RB0 + i * 18432).ap() for i in range(2)]
        Wd32 = [nc.alloc_sbuf_tensor_at(f"Wd32_{i}L{l}", [128, 4, 1024], F32, offset=RB0 + 36864 + i * 16384).ap() for i in range(2)]
        Wd16 = [nc.alloc_sbuf_tensor_at(f"Wd16_{i}L{l}", [128, 4, 1024], BF16, offset=RB0 + 36864 + 32768 + i * 8192).ap() for i in range(2)]
        for tt in range(2):
            S.dma("qsp", xt[tt], MOD[l, tt, 5 * D:6 * D].partition_broadcast(128), reads=["MOD"], writes=[f"xt{tt}"])
        for dg in range(2):
            S.op("dve", lambda e: e.memset(acc, 0.0), [], [("acc", t) for t in range(NT)])
            for ex in range(32):
                bi_ = ex % 2
                S.dma("qsp", Wd32[bi_], I["exp_w_down"][l, ex, :, dg * 1024:(dg + 1) * 1024].rearrange("(k p) c -> p k c", p=128), writes=[f"Wd32_{bi_}"])
                S.op("act", lambda e, bi_=bi_: e.activation(out=Wd16[bi_], in_=Wd32[bi_], func=AF.Copy), [f"Wd32_{bi_}"], [f"Wd16_{bi_}"])
                S.dma("qpool", ATe[bi_], ATd[ex], reads=["ATd"], writes=[f"ATe{bi_}"])
                for t in range(T0, NT):
                    for hb in range(2):
                        p_ = ps[2 + (2 * t + hb) % 4]; pk = f"ps{2 + (2 * t + hb) % 4}"
                        for kk in range(4):
                            S.op("pe", lambda e, p_=p_, kk=kk, t=t, hb=hb, bi_=bi_: e.matmul(p_, lhsT=ATe[bi_][:, kk, t * 128:(t + 1) * 128], rhs=Wd16[bi_][:, kk, hb * 512:(hb + 1) * 512],
                                                                                         start=(kk == 0), stop=(kk == 3)), [f"ATe{bi_}", f"Wd16_{bi_}"], [pk])
                        S.op("dve", lambda e, p_=p_, t=t, hb=hb: e.tensor_tensor(out=acc[:, t, hb, :], in0=p_, in1=acc[:, t, hb, :], op=ALU.add), [pk, ("acc", t)], [("acc", t)])
            for t in range(T0, NT):
                tt = 0 if t < 2 else 1
                for hb in range(2):
                    dc = dg * 2 + hb
                    o_ = po[hb]; ok = f"po{hb}"
                    S.dma("qpool", o_, XR[t * 128:(t + 1) * 128, dc * 512:(dc + 1) * 512], reads=["XR"], writes=[ok])
                    S.op("dve", lambda e, t=t, tt=tt, dc=dc, hb=hb: e.tensor_tensor(out=acc[:, t, hb, :], in0=acc[:, t, hb, :], in1=xt[tt][:, dc * 512:(dc + 1) * 512], op=ALU.mult),
                         [("acc", t), f"xt{tt}"], [("acc", t)])
                    S.op("pool", lambda e, o_=o_, t=t, hb=hb: e.tensor_tensor(out=o_, in0=o_, in1=acc[:, t, hb, :], op=ALU.add), [ok, ("acc", t)], [ok])
                    S.dma("qsp", XR[t * 128:(t + 1) * 128, dc * 512:(dc + 1) * 512], o_, reads=[ok], writes=["XR"])
        if l == 0 and "X2" in dbg_out:
            S.dma("qsp", dbg_out["X2"], XR, reads=["XR"], writes=["dbg"])
        if stop == "P7":
            S.finish(["dbg"])
            return nc

    S.barrier()
    S.dma("qsp", junk, I["final_g"].partition_broadcast(128), writes=["junk"])
    for t in range(2, NT):
        x_ = xt[t % 2]; xk = f"xt{t % 2}"
        S.dma("qsp" if t % 2 == 0 else "qpool", x_, XR[t * 128:(t + 1) * 128, :], reads=["XR"], writes=[xk])
        ss = small[:, 0:1]
        S.op("act", lambda e, x_=x_: e.activation(out=big[1][:, 0:4, :].rearrange("p a b -> p (a b)"), in_=x_, func=AF.Square, accum_out=ss), [xk], ["sqj", "ss"])
        S.op("act", lambda e: e.activation(out=small[:, 1:2], in_=ss, func=AF.Sqrt, scale=1.0 / D, bias=eps_t[:, 0:1]), ["ss"], ["rs"])
        S.op("dve", lambda e: e.reciprocal(out=small[:, 2:3], in_=small[:, 1:2]), ["rs"], ["rstd"])
        S.op("dve", lambda e, x_=x_: e.scalar_tensor_tensor(out=x_, in0=x_, scalar=small[:, 2:3], in1=junk, op0=ALU.mult, op1=ALU.mult), [xk, "rstd", "junk"], [xk])
        S.dma("qsp", out_final[(t - 2) * 128:(t - 1) * 128, :], x_, reads=[xk], writes=["OUT"])
    S.finish(["OUT"])
    return nc


def rope_tables():
    rows = NLAT // 64
    row = np.repeat(np.arange(rows, dtype=np.float32), 64)
    col = np.tile(np.arange(64, dtype=np.float32), rows)
    n_freq = 32
    inv = (np.float32(10000.0) ** (-np.arange(n_freq, dtype=np.float32) / n_freq)).astype(np.float32)
    ang = np.concatenate([row[:, None] * inv, col[:, None] * inv], axis=-1).astype(np.float32)
    cos = np.cos(ang).astype(np.float32)
    sin = np.sin(ang).astype(np.float32)
    return np.tile(cos, (1, 4)), np.tile(sin, (1, 4))


_i = np.arange(128, dtype=np.float32)
POS_TAB = np.ascontiguousarray(np.stack([
    np.stack([_i + 1, -(_i + 1), 127 - _i, np.full(128, 128.0, np.float32)], -1),
    np.stack([128 - _i, -(128 - _i), _i, np.full(128, 128.0, np.float32)], -1)], 1).astype(np.float32))
MASK_F = (np.arange(128)[None, :] >= np.arange(128)[:, None]).astype(np.float32)
MASK_B = (np.arange(128)[:, None] > np.arange(128)[None, :]).astype(np.float32)


def s5_layouts(inputs):
    out = {}
    def rg(a):
        L_ = a.shape[0]
        return np.ascontiguousarray(a.reshape(L_, 2, 32, 2, 64).transpose(0, 1, 3, 4, 2).reshape(L_, 2, 128, 32))
    out["s5_ar"] = rg(inputs["s5_a_re"]); out["s5_ai"] = rg(inputs["s5_a_im"])
    ls = inputs["s5_log_step"]
    out["s5_ls"] = rg(np.broadcast_to(ls[..., None], ls.shape + (64,)))
    def bz(b):
        L_ = b.shape[0]
        bb = b.reshape(L_, 2, 32, 2, 64, 16)
        z = np.zeros((L_, 2, 2, 64, 32, 2, 16), np.float32)
        for gl in range(2):
            z[:, :, gl, :, :, gl, :] = bb[:, :, :, gl].transpose(0, 1, 3, 2, 4)
        return np.ascontiguousarray(z.reshape(L_, 2, 128, 32, 32))
    out["s5_bzr"] = bz(inputs["s5_b_re"]); out["s5_bzi"] = bz(inputs["s5_b_im"])
    out["s5_czr"] = bz(inputs["s5_c_re"].transpose(0, 1, 2, 4, 3)); out["s5_czi"] = bz(inputs["s5_c_im"].transpose(0, 1, 2, 4, 3))
    out["s5_d"] = np.ascontiguousarray(inputs["s5_d"].reshape(DEPTH, 1024))
    return out


def make_in_maps(inputs, nb=4):
    cos4, sin4 = rope_tables()
    s5l = s5_layouts(inputs)
    rwc = np.concatenate([inputs["router_grp_w"], inputs["router_exp_w"]], -1)
    RW_ = np.ascontiguousarray(rwc.reshape(DEPTH, KT, 128, 36).transpose(0, 2, 1, 3))
    RB_ = np.ascontiguousarray(np.concatenate([inputs["router_grp_b"], inputs["router_exp_b"]], -1))
    maps = []
    for b in range(nb):
        cc = np.stack([inputs["c_ctx"], inputs["c"][b]], 0)
        ccT = np.ascontiguousarray(cc.reshape(2, KT, 128).transpose(2, 1, 0))
        m = {
            "xin": np.ascontiguousarray(inputs["x"][b]),
            "ctxin": np.ascontiguousarray(inputs["ctx"][b]),
            "ccT": ccT,
            "ada_w": inputs["ada_w"], "ada_b": inputs["ada_b"],
            "norm1_g": inputs["norm1_g"], "w_in": inputs["w_in"],
            "ident": np.eye(128, dtype=np.float32),
            "rope_cos": cos4, "rope_sin": sin4,
            "ret_decay": np.ascontiguousarray(inputs["ret_decay"].reshape(DEPTH, 16)),
            "pos": POS_TAB, "maskF": MASK_F, "maskB": MASK_B,
            **s5l,
            "s5_glu_w": inputs["s5_glu_w"], "s5_glu_b": inputs["s5_glu_b"], "w_out": inputs["w_out"],
            "norm2_g": inputs["norm2_g"], "final_g": inputs["final_g"],
            "rw": RW_, "rb": RB_,
            "exp_w_gate": inputs["exp_w_gate"], "exp_w_up": inputs["exp_w_up"], "exp_w_down": inputs["exp_w_down"],
        }
        maps.append(m)
    return maps


def kernel(**inputs):
    inputs = {k_: np.asarray(v) for k_, v in inputs.items()}
    nc = build()
    maps = make_in_maps(inputs)
    res = run_bass_kernel_spmd(nc, maps, core_ids=list(range(4)))
    return np.stack([r["out"] for r in res.results], 0)
```
